# Optimizing a Trainium2 kernel written in Bass

```python
import math
import jax
import jax.numpy as jnp
from jax import lax
import numpy as np

D_MODEL = 1024
BATCH = 8
SEQ = 4096
DEPTH = 2

N_META = 16
CHUNK = 128
PAD = CHUNK - N_META
WINDOW = 128
D_MIX = D_MODEL
CONV_K = 4
EPS = 1e-6
NEG = -1e30

DN_H = 4
DN_DK = 64
DN_DV = 64
DF_H = 4
DF_DQK = 32
DF_DV = 2 * DF_DQK
SW_HQ = 4
SW_HKV = 2
SW_REP = SW_HQ // SW_HKV
SW_D = 64
SSD_H = 4
SSD_P = 64
SSD_N = 128
SSD_G = 2
D_SSM = SSD_H * SSD_P

D_FF = 2816
N_EXP = 8
TOP_K = 2
D_FF_E = 3584
MOE_BLOCK = 128
N_DENSE = (DEPTH + 1) // 2
N_MOE = DEPTH // 2

COL_WIDTHS = (DN_H * DN_DK, DN_H * DN_DK, DN_H * DN_DV, DN_H * DN_DV, DN_H, DN_H,
              2 * DF_H * DF_DQK, 2 * DF_H * DF_DQK, DF_H * DF_DV,
              SW_HQ * SW_D, SW_HKV * SW_D, SW_HKV * SW_D,
              D_SSM, D_SSM + 2 * SSD_G * SSD_N, SSD_H)
N_COLS = sum(COL_WIDTHS)

kernel_name = "hymba_parallel_hybrid_trunk"


def rmsnorm(x, w):
    xf = x.astype(jnp.float32)
    y = xf * lax.rsqrt(jnp.mean(xf * xf, axis=-1, keepdims=True) + EPS)
    return (y * w.astype(jnp.float32)).astype(x.dtype)


def l2norm(x):
    return x * lax.rsqrt(jnp.sum(x * x, axis=-1, keepdims=True) + EPS)


def causal_conv(x, w):
    return lax.conv_general_dilated(x, w[:, None, :], window_strides=(1,),
                                    padding=[(w.shape[0] - 1, 0)],
                                    dimension_numbers=("NWC", "WIO", "NWC"),
                                    feature_group_count=x.shape[-1])


def front_pad(t):
    return jnp.pad(t, [(0, 0), (PAD, 0)] + [(0, 0)] * (t.ndim - 2))


def to_chunks(t):
    t = front_pad(t)
    bsz, lp = t.shape[:2]
    t = t.reshape((bsz, lp // CHUNK, CHUNK) + t.shape[2:])
    return jnp.moveaxis(t, 2, 3)


def from_chunks(t):
    t = jnp.moveaxis(t, 3, 2)
    bsz, nc = t.shape[:2]
    return t.reshape((bsz, nc * CHUNK) + t.shape[3:])[:, PAD:]


def split_points(widths):
    pts, acc = [], 0
    for w in widths[:-1]:
        acc += w
        pts.append(acc)
    return pts


def gated_deltanet(q_in, k_in, v_in, z_in, b_in, a_in, conv_w, a_log, dt_bias, norm_w):
    bsz, L, _ = q_in.shape
    qkv = jax.nn.silu(causal_conv(jnp.concatenate([q_in, k_in, v_in], -1), conv_w))
    q, k, v = jnp.split(qkv, 3, axis=-1)
    q = l2norm(q.reshape(bsz, L, DN_H, DN_DK)) * (DN_DK ** -0.5)
    k = l2norm(k.reshape(bsz, L, DN_H, DN_DK))
    v = v.reshape(bsz, L, DN_H, DN_DV)
    beta = jax.nn.sigmoid(b_in)
    g = -jnp.exp(a_log) * jax.nn.softplus(a_in + dt_bias)
    q, k, v, beta, g = (to_chunks(t) for t in (q, k, v, beta, g))
    gc = jnp.cumsum(g, axis=-1)
    causal = jnp.tril(jnp.ones((CHUNK, CHUNK), bool))
    strict = jnp.tril(jnp.ones((CHUNK, CHUNK), bool), -1)
    decay = jnp.exp(jnp.where(causal, gc[..., :, None] - gc[..., None, :], -jnp.inf))
    kb = k * beta[..., None]
    m = jnp.where(strict, jnp.einsum("bnhtd,bnhsd->bnhts", kb, k) * decay, 0.0)
    rhs = jnp.concatenate([v * beta[..., None], kb * jnp.exp(gc)[..., None]], axis=-1)
    sol = lax.linalg.triangular_solve(m, rhs, left_side=True, lower=True, unit_diagonal=True)
    u, w = sol[..., :DN_DV], sol[..., DN_DV:]
    attn = jnp.einsum("bnhtd,bnhsd->bnhts", q, k) * decay
    qg = q * jnp.exp(gc)[..., None]
    g_last = gc[..., -1]
    kdec = k * jnp.exp(g_last[..., None] - gc)[..., None]

    def step(S, xs):
        qg_n, w_n, u_n, attn_n, kdec_n, gl_n = xs
        v_new = u_n - jnp.einsum("bhtd,bhde->bhte", w_n, S)
        o_n = jnp.einsum("bhtd,bhde->bhte", qg_n, S) + jnp.einsum("bhts,bhse->bhte", attn_n, v_new)
        S = S * jnp.exp(gl_n)[..., None, None] + jnp.einsum("bhsd,bhse->bhde", kdec_n, v_new)
        return S, o_n

    xs = tuple(jnp.moveaxis(t, 1, 0) for t in (qg, w, u, attn, kdec, g_last))
    s0 = jnp.zeros((bsz, DN_H, DN_DK, DN_DV), jnp.float32)
    _, o = lax.scan(step, s0, xs)
    o = from_chunks(jnp.moveaxis(o, 0, 1))
    o = rmsnorm(o, norm_w) * jax.nn.silu(z_in.reshape(bsz, L, DN_H, DN_DV))
    return o.reshape(bsz, L, DN_H * DN_DV)


def diff_attention(q_in, k_in, v_in, lam_p, norm_w, lambda_init):
    bsz, L, _ = q_in.shape
    q = front_pad(q_in.reshape(bsz, L, DF_H, 2, DF_DQK))
    k = front_pad(k_in.reshape(bsz, L, DF_H, 2, DF_DQK))
    v = front_pad(v_in.reshape(bsz, L, DF_H, DF_DV))
    lp = q.shape[1]
    nb = lp // CHUNK
    lam = jnp.exp(jnp.sum(lam_p[0] * lam_p[1])) - jnp.exp(jnp.sum(lam_p[2] * lam_p[3])) + lambda_init
    kpos = jnp.arange(lp)
    qb = jnp.moveaxis(q.reshape(bsz, nb, CHUNK, DF_H, 2, DF_DQK), 1, 0)
    scale = DF_DQK ** -0.5

    def block(args):
        q_blk, i = args
        qpos = i * CHUNK + jnp.arange(CHUNK)
        s = jnp.einsum("bthmd,bshmd->bhmts", q_blk, k) * scale
        mask = (kpos[None, :] <= qpos[:, None]) & (kpos[None, :] >= PAD)
        p = jax.nn.softmax(jnp.where(mask, s, NEG), axis=-1)
        a = p[:, :, 0] - lam * p[:, :, 1]
        return jnp.einsum("bhts,bshe->bthe", a, v)

    o = lax.map(block, (qb, jnp.arange(nb)))
    o = jnp.moveaxis(o, 0, 1).reshape(bsz, lp, DF_H, DF_DV)[:, PAD:]
    o = rmsnorm(o, norm_w) * (1.0 - lambda_init)
    return o.reshape(bsz, L, DF_H * DF_DV)


def swa_sinks(q_in, k_in, v_in, sinks):
    bsz, L, _ = q_in.shape
    q = front_pad(q_in.reshape(bsz, L, SW_HKV, SW_REP, SW_D))
    k = front_pad(k_in.reshape(bsz, L, SW_HKV, SW_D))
    v = front_pad(v_in.reshape(bsz, L, SW_HKV, SW_D))
    lp = q.shape[1]
    nb = lp // CHUNK
    qb = q.reshape(bsz, nb, CHUNK, SW_HKV, SW_REP, SW_D)
    kb = k.reshape(bsz, nb, CHUNK, SW_HKV, SW_D)
    vb = v.reshape(bsz, nb, CHUNK, SW_HKV, SW_D)
    kk = jnp.concatenate([jnp.concatenate([jnp.zeros_like(kb[:, :1]), kb[:, :-1]], 1), kb], 2)
    vv = jnp.concatenate([jnp.concatenate([jnp.zeros_like(vb[:, :1]), vb[:, :-1]], 1), vb], 2)
    blk = jnp.arange(nb)[:, None]
    qpos = blk * CHUNK + jnp.arange(CHUNK)
    kpos = blk * CHUNK - CHUNK + jnp.arange(2 * CHUNK)
    rel = qpos[:, :, None] - kpos[:, None, :]
    mask = (rel >= 0) & (rel < WINDOW) & (kpos[:, None, :] >= PAD)
    s = jnp.einsum("bntgrd,bnsgd->bngrts", qb, kk) * (SW_D ** -0.5)
    s = jnp.where(mask[None, :, None, None], s, NEG)
    sink = jnp.broadcast_to(sinks.reshape(1, 1, SW_HKV, SW_REP, 1, 1), s.shape[:-1] + (1,))
    p = jax.nn.softmax(jnp.concatenate([s, sink], axis=-1), axis=-1)[..., :-1]
    o = jnp.einsum("bngrts,bnsgd->bntgrd", p, vv)
    return o.reshape(bsz, lp, SW_HQ * SW_D)[:, PAD:]


def mamba2_ssd(z_in, xbc_in, dt_in, conv_w, conv_b, a_log, dt_bias, d_skip, norm_w):
    bsz, L, _ = xbc_in.shape
    xbc = jax.nn.silu(causal_conv(xbc_in, conv_w) + conv_b)
    x, bm, cm = jnp.split(xbc, [D_SSM, D_SSM + SSD_G * SSD_N], axis=-1)
    x = x.reshape(bsz, L, SSD_H, SSD_P)
    rep = SSD_H // SSD_G
    bm = jnp.repeat(bm.reshape(bsz, L, SSD_G, SSD_N), rep, axis=2)
    cm = jnp.repeat(cm.reshape(bsz, L, SSD_G, SSD_N), rep, axis=2)
    dt = jax.nn.softplus(dt_in + dt_bias)
    a = -jnp.exp(a_log)
    xc, bc, cc, dtc = (to_chunks(t) for t in (x * dt[..., None], bm, cm, dt))
    acum = jnp.cumsum(dtc * a[:, None], axis=-1)
    causal = jnp.tril(jnp.ones((CHUNK, CHUNK), bool))
    lmat = jnp.exp(jnp.where(causal, acum[..., :, None] - acum[..., None, :], -jnp.inf))
    y_diag = jnp.einsum("bnhts,bnhsp->bnhtp", jnp.einsum("bnhtc,bnhsc->bnhts", cc, bc) * lmat, xc)
    states = jnp.einsum("bnhsc,bnhs,bnhsp->bnhpc", bc, jnp.exp(acum[..., -1:] - acum), xc)
    chunk_decay = jnp.exp(acum[..., -1])

    def step(hs, xs):
        st_n, dec_n = xs
        return hs * dec_n[..., None, None] + st_n, hs

    h0 = jnp.zeros((bsz, SSD_H, SSD_P, SSD_N), jnp.float32)
    _, h_prev = lax.scan(step, h0, (jnp.moveaxis(states, 1, 0), jnp.moveaxis(chunk_decay, 1, 0)))
    h_prev = jnp.moveaxis(h_prev, 0, 1)
    y_off = jnp.einsum("bnhtc,bnhpc,bnht->bnhtp", cc, h_prev, jnp.exp(acum))
    y = from_chunks(y_diag + y_off) + d_skip[:, None] * x
    y = y.reshape(bsz, L, D_SSM) * jax.nn.silu(z_in)
    y = rmsnorm(y.reshape(bsz, L, SSD_G, D_SSM // SSD_G), norm_w.reshape(SSD_G, -1))
    return y.reshape(bsz, L, D_SSM)


def swiglu(x, w_gate, w_up, w_down):
    return (jax.nn.silu(x @ w_gate) * (x @ w_up)) @ w_down


def moe_swiglu(u, router, e_gate, e_up, e_down):
    bsz, L, d = u.shape
    xt = u.reshape(-1, d)
    T = xt.shape[0]
    logits = jnp.einsum("td,de->te", xt, router).astype(jnp.float32)
    top_v, top_i = lax.top_k(logits, TOP_K)
    gates = jax.nn.softmax(top_v, axis=-1)
    n_assign = T * TOP_K
    exp_flat = top_i.reshape(-1)
    tok_flat = jnp.repeat(jnp.arange(T), TOP_K)
    gate_flat = gates.reshape(-1)
    order = jnp.argsort(exp_flat * n_assign + jnp.arange(n_assign))
    exp_s, tok_s, gate_s = exp_flat[order], tok_flat[order], gate_flat[order]
    counts = jnp.bincount(exp_flat, length=N_EXP)
    padded = ((counts + MOE_BLOCK - 1) // MOE_BLOCK) * MOE_BLOCK
    start_s = jnp.cumsum(counts) - counts
    ends_p = jnp.cumsum(padded)
    start_p = ends_p - padded
    dest = start_p[exp_s] + (jnp.arange(n_assign) - start_s[exp_s])
    cap = (-(-n_assign // MOE_BLOCK) + N_EXP) * MOE_BLOCK
    buf_tok = jnp.zeros((cap,), jnp.int32).at[dest].set(tok_s)
    buf_gate = jnp.zeros((cap,), jnp.float32).at[dest].set(gate_s)
    nblk = cap // MOE_BLOCK
    blk_start = jnp.arange(nblk) * MOE_BLOCK
    blk_exp = jnp.minimum(jnp.sum(ends_p[None, :] <= blk_start[:, None], axis=-1), N_EXP - 1)
    xb = xt[buf_tok].reshape(nblk, MOE_BLOCK, d)

    def expert_block(args):
        x_blk, e = args
        return swiglu(x_blk, e_gate[e], e_up[e], e_down[e])

    yb = lax.map(expert_block, (xb, blk_exp)).reshape(cap, d)
    y = jnp.zeros((T, d), u.dtype).at[buf_tok].add((yb * buf_gate[:, None]).astype(u.dtype))
    return y.reshape(bsz, L, d)


def setup_inputs(seed: int = 0) -> dict:
    key = jax.random.key(seed)
    ks = iter(jax.random.split(key, 40))

    def nrm(shape, scale=1.0):
        return jax.random.normal(next(ks), shape, jnp.float32) * scale

    def gain(shape):
        return 1.0 + 0.01 * jax.random.normal(next(ks), shape, jnp.float32)

    def a_log(shape):
        return jnp.log(jax.random.uniform(next(ks), shape, jnp.float32, 1.0, 16.0))

    def dt_bias(shape):
        lo, hi = math.log(1e-3), math.log(1e-1)
        dt = jnp.exp(jax.random.uniform(next(ks), shape, jnp.float32) * (hi - lo) + lo)
        return dt + jnp.log(-jnp.expm1(-dt))

    conv_w_dn = 3 * DN_H * DN_DK
    conv_w_ssd = D_SSM + 2 * SSD_G * SSD_N
    return {
        "x": nrm((BATCH, SEQ, D_MODEL)),
        "meta_tokens": nrm((N_META, D_MODEL)),
        "norm_mix": gain((DEPTH, D_MODEL)),
        "w_in": nrm((DEPTH, D_MODEL, N_COLS), D_MODEL ** -0.5),
        "dn_conv_w": nrm((DEPTH, CONV_K, conv_w_dn), CONV_K ** -0.5),
        "dn_a_log": a_log((DEPTH, DN_H)),
        "dn_dt_bias": dt_bias((DEPTH, DN_H)),
        "dn_norm_w": gain((DEPTH, DN_DV)),
        "df_lambda": nrm((DEPTH, 4, DF_DQK), 0.1),
        "df_norm_w": gain((DEPTH, DF_DV)),
        "sw_sinks": nrm((DEPTH, SW_HQ), 0.5),
        "ssd_conv_w": nrm((DEPTH, CONV_K, conv_w_ssd), CONV_K ** -0.5),
        "ssd_conv_b": nrm((DEPTH, conv_w_ssd), 0.01),
        "ssd_a_log": a_log((DEPTH, SSD_H)),
        "ssd_dt_bias": dt_bias((DEPTH, SSD_H)),
        "ssd_d": gain((DEPTH, SSD_H)),
        "ssd_norm_w": gain((DEPTH, D_SSM)),
        "w_out": nrm((DEPTH, D_MIX, D_MODEL), D_MIX ** -0.5),
        "norm_ffn": gain((DEPTH, D_MODEL)),
        "ffn_w_gate": nrm((N_DENSE, D_MODEL, D_FF), D_MODEL ** -0.5),
        "ffn_w_up": nrm((N_DENSE, D_MODEL, D_FF), D_MODEL ** -0.5),
        "ffn_w_down": nrm((N_DENSE, D_FF, D_MODEL), D_FF ** -0.5),
        "moe_router": nrm((N_MOE, D_MODEL, N_EXP), D_MODEL ** -0.5),
        "moe_w_gate": nrm((N_MOE, N_EXP, D_MODEL, D_FF_E), D_MODEL ** -0.5),
        "moe_w_up": nrm((N_MOE, N_EXP, D_MODEL, D_FF_E), D_MODEL ** -0.5),
        "moe_w_down": nrm((N_MOE, N_EXP, D_FF_E, D_MODEL), D_FF_E ** -0.5),
        "norm_final": gain((D_MODEL,)),
    }


def reference(x, meta_tokens, norm_mix, w_in, dn_conv_w, dn_a_log, dn_dt_bias, dn_norm_w,
              df_lambda, df_norm_w, sw_sinks, ssd_conv_w, ssd_conv_b, ssd_a_log, ssd_dt_bias,
              ssd_d, ssd_norm_w, w_out, norm_ffn, ffn_w_gate, ffn_w_up, ffn_w_down,
              moe_router, moe_w_gate, moe_w_up, moe_w_down, norm_final):
    bsz = x.shape[0]
    f32 = jnp.float32
    meta = jnp.broadcast_to(meta_tokens[None].astype(x.dtype), (bsz, N_META, D_MODEL))
    h = jnp.concatenate([meta, x], axis=1)
    pts = split_points(COL_WIDTHS)
    for l in range(DEPTH):
        u = rmsnorm(h, norm_mix[l])
        proj = jnp.einsum("bld,dc->blc", u, w_in[l]).astype(f32)
        (dn_q, dn_k, dn_v, dn_z, dn_b, dn_a, df_q, df_k, df_v,
         sw_q, sw_k, sw_v, ssd_z, ssd_xbc, ssd_dt) = jnp.split(proj, pts, axis=-1)
        y_dn = gated_deltanet(dn_q, dn_k, dn_v, dn_z, dn_b, dn_a, dn_conv_w[l].astype(f32),
                              dn_a_log[l].astype(f32), dn_dt_bias[l].astype(f32), dn_norm_w[l])
        lambda_init = 0.8 - 0.6 * math.exp(-0.3 * l)
        y_df = diff_attention(df_q, df_k, df_v, df_lambda[l].astype(f32), df_norm_w[l], lambda_init)
        y_sw = swa_sinks(sw_q, sw_k, sw_v, sw_sinks[l].astype(f32))
        y_ssd = mamba2_ssd(ssd_z, ssd_xbc, ssd_dt, ssd_conv_w[l].astype(f32), ssd_conv_b[l].astype(f32),
                           ssd_a_log[l].astype(f32), ssd_dt_bias[l].astype(f32),
                           ssd_d[l].astype(f32), ssd_norm_w[l])
        mix = jnp.concatenate([y_dn, y_df, y_sw, y_ssd], axis=-1).astype(h.dtype)
        h = h + jnp.einsum("blc,cd->bld", mix, w_out[l])
        u = rmsnorm(h, norm_ffn[l])
        if l % 2 == 0:
            i = l // 2
            h = h + swiglu(u, ffn_w_gate[i], ffn_w_up[i], ffn_w_down[i])
        else:
            i = l // 2
            h = h + moe_swiglu(u, moe_router[i], moe_w_gate[i], moe_w_up[i], moe_w_down[i])
    y = rmsnorm(h, norm_final)
    return y[:, N_META:]
```

```python
import numpy as np
from contextlib import ExitStack
import concourse.bass as bass
import concourse.mybir as mybir

F32 = mybir.dt.float32
BF16 = mybir.dt.bfloat16
AF = mybir.ActivationFunctionType
ALU = mybir.AluOpType
AX = mybir.AxisListType

ENGS = ("pe", "act", "dve", "pool", "sp")
N_DMA_SEMS = 6


class Buf:
    __slots__ = ("name", "w", "r")

    def __init__(self, name):
        self.name = name
        self.w = None
        self.r = {}


class Sched:
    def __init__(self, nc, stack):
        self.nc = nc
        self.sem = {e: stack.enter_context(nc.semaphore("s_" + e)) for e in ENGS}
        self.dsem = {}
        for q in ("sp", "act", "pool"):
            for i in range(N_DMA_SEMS):
                self.dsem[(q, i)] = stack.enter_context(nc.semaphore(f"d_{q}{i}"))
        self.cnt = {e: 0 for e in ENGS}
        self.dcnt = {k: 0 for k in self.dsem}
        self.drr = {"sp": 0, "act": 0, "pool": 0}
        self.seen = {e: {} for e in ENGS}
        self.ops = {e: [] for e in ENGS}
        self.nops = 0
        import os
        self.limit = int(os.environ.get("MK_LIMIT", "1000000000"))

    def _semh(self, key):
        return self.sem[key] if isinstance(key, str) else self.dsem[key]

    def _need(self, eng, tok, waits):
        if tok is None:
            return
        key, val, teng = tok
        if eng == "pe" and teng == "pe":
            return
        if self.seen[eng].get(key, 0) >= val:
            return
        self.seen[eng][key] = val
        waits[key] = max(waits.get(key, 0), val)

    def _deps(self, eng, reads, writes, waits):
        for b in reads:
            self._need(eng, b.w, waits)
        for b in writes:
            self._need(eng, b.w, waits)
            for k, (v, te) in b.r.items():
                self._need(eng, (k, v, te), waits)

    def _commit(self, tok, reads, writes):
        for b in reads:
            o = b.r.get(tok[0])
            if o is None or o[0] < tok[1]:
                b.r[tok[0]] = (tok[1], tok[2])
        for b in writes:
            b.w = tok
            b.r = {}

    def op(self, eng, fn, reads=(), writes=()):
        if self.nops >= self.limit:
            return None
        waits = {}
        self._deps(eng, reads, writes, waits)
        self.cnt[eng] += 1
        tok = (eng, self.cnt[eng], eng)
        self.ops[eng].append((fn, waits, (eng, 1)))
        self._commit(tok, reads, writes)
        self.nops += 1
        return tok

    def dma(self, q, fn, reads=(), writes=()):
        if self.nops >= self.limit:
            return None
        waits = {}
        self._deps(q, reads, writes, waits)
        i = self.drr[q]
        self.drr[q] = (i + 1) % N_DMA_SEMS
        key = (q, i)
        if self.dcnt[key] > 0:
            self._need(q, (key, self.dcnt[key], "dma"), waits)
        self.dcnt[key] += 16
        tok = (key, self.dcnt[key], "dma")
        self.ops[q].append((fn, waits, (key, 16)))
        self._commit(tok, reads, writes)
        self.nops += 1
        return tok

    def barrier(self):
        for e in ENGS:
            waits = {}
            for k in ENGS:
                if k == "pe" and e == "pe":
                    continue
                if self.cnt[k] > 0 and self.seen[e].get(k, 0) < self.cnt[k]:
                    self.seen[e][k] = self.cnt[k]
                    waits[k] = self.cnt[k]
            for k, v in self.dcnt.items():
                if v > 0 and self.seen[e].get(k, 0) < v:
                    self.seen[e][k] = v
                    waits[k] = v
            if waits:
                self.ops[e].append((None, waits, None))

    def emit(self, block):
        names = {"pe": "tensor", "act": "scalar", "dve": "vector", "pool": "gpsimd", "sp": "sync"}
        for e in ENGS:
            ops = self.ops[e]
            self.ops[e] = []

            def body(engine, ops=ops):
                for fn, waits, inc in ops:
                    for k, v in waits.items():
                        engine.wait_ge(self._semh(k), v)
                    if fn is not None:
                        fn(engine).then_inc(self._semh(inc[0]), inc[1])
            getattr(block, names[e])(body)


D = 1024
NCOLS = 3340
C_DNQ, C_DNK, C_DNV, C_DNZ, C_DNB, C_DNA = 0, 256, 512, 768, 1024, 1028
C_DFQ, C_DFK, C_DFV = 1032, 1288, 1544
C_SWQ, C_SWK, C_SWV = 1800, 2056, 2184
C_SSZ, C_SSX, C_SSB, C_SSC, C_SSDT = 2312, 2568, 2824, 3080, 3336
EPS = 1e-6
DFF = 2816
NEXP = 8
DFFE = 3584
BIG = 1e30


class KB:
    def __init__(self, NT, depth=2, dbg=()):
        self.NT = NT
        self.T = NT * 128
        self.SEQ = self.T - 128
        self.depth = depth
        self.dbg = dbg
        self.nc = bass.Bass("TRN2", target_bir_lowering=False)
        self.bufs = {}
        self.uid = 0
        self.oplog = []
        self.psum_names = set()

    def din(self, name, shape):
        return self.nc.dram_tensor(name, list(shape), F32, kind="ExternalInput").ap()

    def dout(self, name, shape):
        return self.nc.dram_tensor(name, list(shape), F32, kind="ExternalOutput").ap()

    def dscr(self, name, shape, dt=F32):
        return self.nc.dram_tensor(name, list(shape), dt, kind="Internal").ap()

    def sb(self, st, name, shape, dt=F32):
        self.uid += 1
        return st.enter_context(self.nc.sbuf_tensor(f"{name}_{self.uid}", list(shape), dt))

    def ps(self, st, name, shape, dt=F32):
        self.uid += 1
        nm = f"{name}_{self.uid}"
        self.psum_names.add(nm)
        return st.enter_context(self.nc.psum_tensor(nm, list(shape), dt))

    def buf(self, x):
        if isinstance(x, tuple):
            key = x[1]
        else:
            key = x.name
        b = self.bufs.get(key)
        if b is None:
            b = self.bufs[key] = Buf(str(key))
        return b

    @staticmethod
    def ap(x):
        return x[0] if isinstance(x, tuple) else x

    def _op(self, eng, fn, outs, ins):
        rd = [self.buf(i) for i in ins if i is not None and not isinstance(i, (int, float))]
        wr = [self.buf(o) for o in outs]
        wr += [b for b in rd if b.name in self.psum_names and b not in wr]
        import sys
        self.oplog.append((self.S.nops, eng, sys._getframe(1).f_code.co_name, [b.name for b in wr], [b.name for b in rd]))
        self.S.op(eng, fn, reads=rd, writes=wr)

    def mm(self, out, lhsT, rhs, start=True, stop=True, acc=False, **kw):
        o, l, r = self.ap(out), self.ap(lhsT), self.ap(rhs)
        self._op("pe", lambda e: e.matmul(o, lhsT=l, rhs=r, start=start, stop=stop, skip_group_check=True, **kw),
                 [out], [lhsT, rhs] + ([out] if (acc or not start) else []))

    def tr(self, out, in_, ident):
        o, i, d = self.ap(out), self.ap(in_), self.ap(ident)
        self._op("pe", lambda e: e.transpose(o, i, d), [out], [in_, ident])

    def act(self, out, in_, func, bias=0.0, scale=1.0, accum_out=None, eng="act"):
        o, i = self.ap(out), self.ap(in_)
        b = self.ap(bias) if not isinstance(bias, (int, float)) else bias
        s = self.ap(scale) if not isinstance(scale, (int, float)) else scale
        if accum_out is not None:
            a = self.ap(accum_out)
            self._op("act", lambda e: e.activation(o, i, func, bias=b, scale=s, accum_out=a), [out, accum_out], [in_, bias, scale, accum_out])
        else:
            self._op("act", lambda e: e.activation(o, i, func, bias=b, scale=s), [out], [in_, bias, scale])

    def tt(self, eng, out, in0, in1, op):
        o, a, b = self.ap(out), self.ap(in0), self.ap(in1)
        self._op(eng, lambda e: e.tensor_tensor(o, a, b, op), [out], [in0, in1])

    def ts(self, eng, out, in0, s1, op0, s2=None, op1=None):
        o, a = self.ap(out), self.ap(in0)
        v1 = self.ap(s1) if not isinstance(s1, (int, float)) else s1
        v2 = self.ap(s2) if (s2 is not None and not isinstance(s2, (int, float))) else s2
        if op1 is None:
            self._op(eng, lambda e: e.tensor_scalar(o, a, v1, None, op0), [out], [in0, s1])
        else:
            self._op(eng, lambda e: e.tensor_scalar(o, a, v1, v2, op0, op1), [out], [in0, s1, s2])

    def stt(self, eng, out, in0, scalar, in1, op0, op1):
        o, a, b = self.ap(out), self.ap(in0), self.ap(in1)
        sc = self.ap(scalar) if not isinstance(scalar, (int, float)) else scalar
        self._op(eng, lambda e: e.scalar_tensor_tensor(o, a, sc, b, op0, op1), [out], [in0, scalar, in1])

    def cp(self, eng, out, in_):
        o, i = self.ap(out), self.ap(in_)
        if eng == "act":
            self._op(eng, lambda e: e.copy(o, i), [out], [in_])
        else:
            self._op(eng, lambda e: e.tensor_copy(o, i), [out], [in_])

    def memset(self, eng, out, val):
        o = self.ap(out)
        self._op(eng, lambda e: e.memset(o, val), [out], [])

    def red(self, eng, out, in_, op, axis=AX.X):
        o, i = self.ap(out), self.ap(in_)
        self._op(eng, lambda e: e.tensor_reduce(o, i, axis, op), [out], [in_])

    def recip(self, out, in_):
        o, i = self.ap(out), self.ap(in_)
        self._op("dve", lambda e: e.reciprocal(o, i), [out], [in_])

    def asel(self, out, in_, pattern, cmp, fill, base, cm):
        o, i = self.ap(out), self.ap(in_)
        self._op("pool", lambda e: e.affine_select(o, i, pattern=pattern, compare_op=cmp, fill=fill, base=base, channel_multiplier=cm), [out], [in_])

    def dma(self, q, out, in_, slow=False):
        o, i = self.ap(out), self.ap(in_)
        rd = [self.buf(in_)]
        wr = [self.buf(out)]
        self.oplog.append((self.S.nops, q, "dma", [b.name for b in wr], [b.name for b in rd]))
        if slow:
            self.S.dma(q, lambda e: e.dma_start(out=o, in_=i, allow_slow_non_contiguous=True), reads=rd, writes=wr)
        else:
            self.S.dma(q, lambda e: e.dma_start(out=o, in_=i), reads=rd, writes=wr)

    def flush(self):
        self.S.barrier()
        with self.nc.Block() as blk:
            self.S.emit(blk)


class Model(KB):
    def declare_io(self):
        d = self.din
        self.x = d("x", [self.SEQ, D])
        self.meta = d("meta_tokens", [16, D])
        self.norm_mix = d("norm_mix", [2, D])
        self.w_in = d("w_in", [2, D, NCOLS])
        self.dn_conv_w = d("dn_conv_w", [2, 4, 768])
        self.dn_a_log = d("dn_a_log", [2, 4])
        self.dn_dt_bias = d("dn_dt_bias", [2, 4])
        self.dn_norm_w = d("dn_norm_w", [2, 64])
        self.df_lambda = d("df_lambda", [2, 4, 32])
        self.df_norm_w = d("df_norm_w", [2, 64])
        self.sw_sinks = d("sw_sinks", [2, 4])
        self.ssd_conv_w = d("ssd_conv_w", [2, 4, 768])
        self.ssd_conv_b = d("ssd_conv_b", [2, 768])
        self.ssd_a_log = d("ssd_a_log", [2, 4])
        self.ssd_dt_bias = d("ssd_dt_bias", [2, 4])
        self.ssd_d = d("ssd_d", [2, 4])
        self.ssd_norm_w = d("ssd_norm_w", [2, 256])
        self.w_out = d("w_out", [2, D, D])
        self.norm_ffn = d("norm_ffn", [2, D])
        self.ffn_w_gate = d("ffn_w_gate", [1, D, DFF])
        self.ffn_w_up = d("ffn_w_up", [1, D, DFF])
        self.ffn_w_down = d("ffn_w_down", [1, DFF, D])
        self.moe_router = d("moe_router", [1, D, NEXP])
        self.moe_w_gate = d("moe_w_gate", [1, NEXP, D, DFFE])
        self.moe_w_up = d("moe_w_up", [1, NEXP, D, DFFE])
        self.moe_w_down = d("moe_w_down", [1, NEXP, DFFE, D])
        self.norm_final = d("norm_final", [D])
        self.out = self.dout("out", [self.SEQ, D])
        T = self.T
        s = self.dscr
        self.H = s("H", [T, D])
        self.DN_TOK = s("DN_TOK", [T, 1024])
        self.TOK2 = s("TOK2", [T, 384])
        self.SSD_TOK = s("SSD_TOK", [T, 768])
        self.SSD_FT = s("SSD_FT", [4, 128, T])
        self.DF_QT = s("DF_QT", [2, 128, T], BF16)
        self.DF_KT = s("DF_KT", [2, 128, T], BF16)
        self.SW_QT = s("SW_QT", [2, 128, T], BF16)
        self.SW_KT = s("SW_KT", [128, T], BF16)
        self.MIX = s("MIX", [T, D])
        self.U2T = s("U2T", [8, 128, T], BF16)
        self.GATES = s("GATES", [NEXP, T])

    def build(self):
        nc = self.nc
        self.declare_io()
        with ExitStack() as gs:
            self.gs = gs
            self.S = Sched(nc, gs)
            self.consts()
            for l in range(self.depth):
                self.phase1(l)
                if "p1" in self.dbg:
                    break
                self.phase_sw(l)
                self.phase_df(l)
                self.phase_ssd(l)
                self.phase_dn(l)
                if "mix" in self.dbg:
                    break
                self.phase3(l)
            if not self.dbg:
                self.final()
        return nc

    def consts(self):
        gs = self.gs
        NT = self.NT
        sb = lambda n, s, dt=F32: self.sb(gs, n, s, dt)
        self.identf = sb("identf", [128, 128])
        self.identb = sb("identb", [128, 128], BF16)
        self.onesf = sb("onesf", [128, 128])
        self.Umat = sb("Umat", [128, 128])
        self.E127 = sb("E127", [128, 128])
        self.mstrict = sb("mstrict", [128, 128])
        self.mTincl = sb("mTincl", [128, 128])
        self.c01T = sb("c01T", [128, 128], BF16)
        self.p01T = sb("p01T", [128, 128], BF16)
        self.bind4 = sb("bind4", [128, 4])
        self.bind2 = sb("bind2", [128, 2])
        self.BETA = sb("BETA", [128, NT, 4])
        self.G = sb("G", [128, NT, 4])
        self.DT = sb("DT", [128, NT, 4])
        self.BGraw = sb("BGraw", [128, NT, 8])
        self.DTraw = sb("DTraw", [128, NT, 4])
        self.stats = sb("stats", [4, 8])
        self.mbias = sb("mbias", [128, 2])
        m = self.memset
        a = self.asel
        self.epsb = sb("epsb", [128, 1])
        m("pool", self.epsb[:], EPS)
        m("pool", self.identf[:], 1.0)
        a(self.identf[:], self.identf[:], [[-1, 128]], ALU.is_equal, 0.0, 0, 1)
        self.cp("dve", self.identb[:], self.identf[:])
        m("pool", self.onesf[:], 1.0)
        m("pool", self.Umat[:], 1.0)
        a(self.Umat[:], self.Umat[:], [[1, 128]], ALU.is_ge, 0.0, 0, -1)
        self.cp("dve", self.c01T[:], self.Umat[:])
        m("pool", self.E127[:], 1.0)
        a(self.E127[:], self.E127[:], [[0, 128]], ALU.is_equal, 0.0, -127, 1)
        m("pool", self.mstrict[:], 0.0)
        a(self.mstrict[:], self.mstrict[:], [[-1, 128]], ALU.is_gt, BIG, 0, 1)
        m("pool", self.mTincl[:], 0.0)
        a(self.mTincl[:], self.mTincl[:], [[1, 128]], ALU.is_ge, -BIG, 0, -1)
        m("pool", self.p01T[:], 1.0)
        a(self.p01T[:], self.p01T[:], [[-1, 128]], ALU.is_gt, 0.0, 0, 1)
        m("pool", self.bind4[:], 1.0)
        a(self.bind4[:], self.bind4[:], [[-32, 4]], ALU.is_ge, 0.0, 0, 1)
        a(self.bind4[:], self.bind4[:], [[32, 4]], ALU.is_ge, 0.0, 31, -1)
        m("pool", self.bind2[:], 1.0)
        a(self.bind2[:], self.bind2[:], [[-64, 2]], ALU.is_ge, 0.0, 0, 1)
        a(self.bind2[:], self.bind2[:], [[64, 2]], ALU.is_ge, 0.0, 63, -1)

    def groups(self):
        NT = self.NT
        g = []
        n = 0
        while n < NT:
            tg = min(4, NT - n)
            g.append((n, tg))
            n += tg
        return g

    def load_h_group(self, l, hs, n0, TG):
        if l == 0:
            j0 = 0
            if n0 == 0:
                self.memset("pool", hs[:, 0, :], 0.0)
                self.dma("sp", hs[112:128, 0, :], self.meta[:, :])
                j0 = 1
            if TG - j0 > 0:
                xt = self.x.rearrange("(n p) d -> p n d", p=128)
                self.dma("sp", hs[:, j0:TG, :], xt[:, n0 + j0 - 1:n0 + TG - 1, :])
        else:
            ht = self.H.rearrange("(n p) d -> p n d", p=128)
            self.dma("sp", hs[:, 0:TG, :], ht[:, n0:n0 + TG, :])

    def rmsnorm_T(self, st_tiles, hs, TG, normw, n0, uT, tp, zero_pad=True):
        junk, ssq, rstd, ub = st_tiles
        for j in range(TG):
            self.tt("pool", junk[:], hs[:, j, :], hs[:, j, :], ALU.mult)
            self.red("dve", ssq[:, j:j + 1], junk[:], ALU.add)
        self.act(rstd[:, 0:TG], ssq[:, 0:TG], AF.Sqrt, bias=self.epsb[:, 0:1], scale=1.0 / D)
        self.recip(rstd[:, 0:TG], rstd[:, 0:TG])
        for j in range(TG):
            self.stt("dve", ub[:, j, :], hs[:, j, :], rstd[:, j:j + 1], normw[:], ALU.mult, ALU.mult)
        if zero_pad and n0 == 0:
            self.memset("pool", ub[0:112, 0, :], 0.0)
        for j in range(TG):
            for c in range(8):
                self.tr(tp[:, c, :], ub[:, j, c * 128:(c + 1) * 128], self.identb[:])
            self.cp("act", uT[:, :, j * 128:(j + 1) * 128], tp[:, :, :])

    def phase1(self, l):
        NT = self.NT
        with ExitStack() as st:
            sb = lambda n, s, dt=F32: self.sb(st, n, s, dt)
            ps = lambda n, s, dt=F32: self.ps(st, n, s, dt)
            win = sb("win", [128, 8, NCOLS], BF16)
            for c in range(8):
                self.dma("pool", win[:, c, :], self.w_in[l, c * 128:(c + 1) * 128, :])
            wswq = sb("wswq", [128, 8, 256], BF16)
            for i_, h_ in enumerate((0, 2, 1, 3)):
                self.cp("pool", wswq[:, :, i_ * 64:(i_ + 1) * 64], win[:, :, C_SWQ + h_ * 64:C_SWQ + (h_ + 1) * 64])
            normw = sb("normw", [128, D])
            self.dma("sp", normw[:], self.norm_mix[l].partition_broadcast(128))
            dncw = sb("dncw", [128, 6, 4])
            for cc in range(6):
                self.dma("sp", dncw[:, cc, :], self.dn_conv_w[l, :, cc * 128:(cc + 1) * 128].rearrange("j p -> p j"), slow=True)
            sscw = sb("sscw", [128, 6, 4])
            for cc in range(6):
                self.dma("sp", sscw[:, cc, :], self.ssd_conv_w[l, :, cc * 128:(cc + 1) * 128].rearrange("j p -> p j"), slow=True)
            sscb = sb("sscb", [128, 6])
            self.dma("sp", sscb[:], self.ssd_conv_b[l].rearrange("(c p) -> p c", p=128), slow=True)
            hp = sb("hp", [128, 16])
            self.dma("sp", hp[:, 0:4], self.dn_a_log[l].partition_broadcast(128))
            self.dma("sp", hp[:, 4:8], self.dn_dt_bias[l].partition_broadcast(128))
            self.dma("sp", hp[:, 8:12], self.ssd_dt_bias[l].partition_broadcast(128))

            hs = sb("hs", [128, 4, D])
            junk = sb("junk", [128, D])
            ssq = sb("ssq", [128, 4])
            rstd = sb("rstd", [128, 4])
            ub = sb("ub", [128, 4, D], BF16)
            uT = sb("uT", [128, 8, 512], BF16)
            xin_dn = sb("xin_dn", [128, 6, 515])
            xin_ss = sb("xin_ss", [128, 6, 515])
            cacc = sb("cacc", [128, 512])
            cs = [sb("cs0", [128, 512]), sb("cs1", [128, 512])]
            dnst = sb("dnst", [128, 4, 1024])
            ssdst = sb("ssdst", [128, 4, 768])
            tok2st = sb("tok2st", [128, 4, 384])
            fst = [sb("fst0", [128, 512], BF16), sb("fst1", [128, 512], BF16)]
            sqf = sb("sqf", [128, 512])
            sqt = sb("sqt", [128, 512])
            ssn = sb("ssn", [128, 8])
            stmp = sb("stmp", [4, 1])

            tp = ps("tp", [128, 8, 128], BF16)
            pfm = [ps("pfm0", [128, 512]), ps("pfm1", [128, 512])]
            pA = ps("pA", [128, 512])
            pB = ps("pB", [128, 512])
            pC = ps("pC", [128, 512])
            t32 = ps("t32", [128, 4, 128])
            pstat = ps("pstat", [4, 512])

            self.memset("pool", xin_dn[:], 0.0)
            self.memset("pool", xin_ss[:], 0.0)
            self.memset("pool", self.stats[:], 0.0)
            fmi = [0]
            csi = [0]
            fsi = [0]

            for (n0, TG) in self.groups():
                TW = TG * 128
                self.load_h_group(l, hs, n0, TG)
                self.rmsnorm_T((junk, ssq, rstd, ub), hs, TG, normw, n0, uT, tp)

                def fm(lhs_fn):
                    pf = pfm[fmi[0] % 2]
                    fmi[0] += 1
                    for c in range(8):
                        self.mm(pf[:, 0:TW], lhs_fn(c), uT[:, c, 0:TW], start=(c == 0), stop=(c == 7))
                    return pf

                def conv_chunk(pf, xin, cc, cw, bias=None):
                    self.cp("act", xin[:, cc, 3:3 + TW], pf[:, 0:TW])
                    self.ts("dve", cacc[:, 0:TW], xin[:, cc, 0:TW], cw[:, cc, 0:1], ALU.mult)
                    for j in range(1, 4):
                        self.stt("dve", cacc[:, 0:TW], xin[:, cc, j:j + TW], cw[:, cc, j:j + 1], cacc[:, 0:TW], ALU.mult, ALU.add)
                    o = cs[csi[0] % 2]
                    csi[0] += 1
                    if bias is None:
                        self.act(o[:, 0:TW], cacc[:, 0:TW], AF.Silu)
                    else:
                        self.act(o[:, 0:TW], cacc[:, 0:TW], AF.Silu, bias=bias)
                    self.cp("pool", xin[:, cc, 0:3], xin[:, cc, TW:TW + 3])
                    return o

                def to_tok(o, dst, col0):
                    for j in range(TG):
                        self.tr(t32[:, j, :], o[:, j * 128:(j + 1) * 128], self.identf[:])
                    self.cp("dve", dst[:, 0:TG, col0:col0 + 128], t32[:, 0:TG, :])

                for cc in range(6):
                    pf = fm(lambda c, cc=cc: win[:, c, C_DNQ + cc * 128:C_DNQ + (cc + 1) * 128])
                    o = conv_chunk(pf, xin_dn, cc, dncw)
                    to_tok(o, dnst, cc * 128)
                for cc in range(6):
                    pf = fm(lambda c, cc=cc: win[:, c, C_SSX + cc * 128:C_SSX + (cc + 1) * 128])
                    o = conv_chunk(pf, xin_ss, cc, sscw, bias=sscb[:, cc:cc + 1])
                    if cc < 4:
                        to_tok(o, ssdst, cc * 128)
                    if cc >= 2:
                        self.dma("sp", self.SSD_FT[cc - 2, :, n0 * 128:n0 * 128 + TW], o[:, 0:TW])
                def qk_chunk(lhs_fn, scale, dst_ap, stat_col, bind, nb):
                    pf = fm(lhs_fn)
                    f = fst[fsi[0] % 2]
                    fsi[0] += 1
                    self.act(f[:, 0:TW], pf[:, 0:TW], AF.Identity, scale=scale)
                    self.act(sqf[:, 0:TW], pf[:, 0:TW], AF.Square, scale=scale)
                    self.mm(pstat[0:nb, 0:TW], bind[:, 0:nb], sqf[:, 0:TW])
                    self.red("dve", stmp[0:nb, 0:1], pstat[0:nb, 0:TW], ALU.max)
                    self.tt("dve", self.stats[0:nb, stat_col:stat_col + 1], self.stats[0:nb, stat_col:stat_col + 1], stmp[0:nb, 0:1], ALU.max)
                    self.dma("sp", dst_ap, f[:, 0:TW])

                sl = slice(n0 * 128, n0 * 128 + TW)
                for c2 in range(2):
                    qk_chunk(lambda c, c2=c2: win[:, c, C_DFQ + c2 * 128:C_DFQ + (c2 + 1) * 128], 32 ** -0.5, self.DF_QT[c2, :, sl], 0, self.bind4, 4)
                    qk_chunk(lambda c, c2=c2: win[:, c, C_DFK + c2 * 128:C_DFK + (c2 + 1) * 128], 1.0, self.DF_KT[c2, :, sl], 1, self.bind4, 4)
                for c2 in range(2):
                    qk_chunk(lambda c, c2=c2: wswq[:, c, c2 * 128:(c2 + 1) * 128], 64 ** -0.5, self.SW_QT[c2, :, sl], 2, self.bind2, 2)
                qk_chunk(lambda c: win[:, c, C_SWK:C_SWK + 128], 1.0, self.SW_KT[:, sl], 3, self.bind2, 2)

                for j in range(TG):
                    n = n0 + j
                    lt = lambda c: uT[:, c, j * 128:(j + 1) * 128]
                    for c in range(8):
                        self.mm(pA[:, 0:264], lt(c), win[:, c, C_DNZ:C_DNZ + 264], start=(c == 0), stop=(c == 7))
                    for c in range(8):
                        self.mm(pA[:, 264:268], lt(c), win[:, c, C_SSDT:C_SSDT + 4], start=(c == 0), stop=(c == 7))
                    for c in range(8):
                        self.mm(pB[:, 0:256], lt(c), win[:, c, C_DFV:C_DFV + 256], start=(c == 0), stop=(c == 7))
                    for c in range(8):
                        self.mm(pC[:, 0:384], lt(c), win[:, c, C_SWV:C_SWV + 384], start=(c == 0), stop=(c == 7))
                    self.cp("act", dnst[:, j, 768:1024], pA[:, 0:256])
                    self.cp("dve", self.BGraw[:, n, :], pA[:, 256:264])
                    self.cp("dve", self.DTraw[:, n, :], pA[:, 264:268])
                    self.cp("act", tok2st[:, j, 0:256], pB[:, 0:256])
                    self.cp("dve", tok2st[:, j, 256:384], pC[:, 0:128])
                    self.cp("act", ssdst[:, j, 512:768], pC[:, 128:384])
                for j in range(TG):
                    qk = dnst[:, j, 0:512].rearrange("p (h d) -> p h d", d=64)
                    self.tt("pool", sqt[:], dnst[:, j, 0:512], dnst[:, j, 0:512], ALU.mult)
                    self.red("dve", ssn[:], sqt[:].rearrange("p (h d) -> p h d", d=64), ALU.add)
                    self.act(ssn[:], ssn[:], AF.Sqrt, bias=self.epsb[:, 0:1])
                    self.recip(ssn[:], ssn[:])
                    self.ts("dve", ssn[:, 0:4], ssn[:, 0:4], 0.125, ALU.mult)
                    self.tt("dve", qk, qk, ssn[:].unsqueeze(2).to_broadcast([128, 8, 64]), ALU.mult)
                tsl = slice(n0, n0 + TG)
                self.dma("sp", self.DN_TOK.rearrange("(n p) c -> p n c", p=128)[:, tsl, :], dnst[:, 0:TG, :])
                self.dma("sp", self.SSD_TOK.rearrange("(n p) c -> p n c", p=128)[:, tsl, :], ssdst[:, 0:TG, :])
                self.dma("sp", self.TOK2.rearrange("(n p) c -> p n c", p=128)[:, tsl, :], tok2st[:, 0:TG, :])

            tmp = sb("gtmp", [128, NT, 4])
            tmp2 = sb("gtmp2", [128, NT, 4])
            negA = sb("negA", [128, 4])
            bc = lambda ap: ap.unsqueeze(1).to_broadcast([128, NT, 4])
            self.act(self.BETA[:], self.BGraw[:, :, 0:4], AF.Sigmoid)

            def softplus(dst, src_raw, bias_ap):
                self.tt("dve", tmp[:], src_raw, bc(bias_ap), ALU.add)
                self.act(tmp2[:], tmp[:], AF.Abs)
                self.act(tmp2[:], tmp2[:], AF.Exp, scale=-1.0)
                self.act(tmp2[:], tmp2[:], AF.Ln, bias=1.0)
                self.stt("dve", dst, tmp[:], 0.0, tmp2[:], ALU.max, ALU.add)

            softplus(self.G[:], self.BGraw[:, :, 4:8], hp[:, 4:8])
            self.act(negA[:], hp[:, 0:4], AF.Exp)
            self.ts("dve", negA[:], negA[:], -1.0, ALU.mult)
            self.tt("dve", self.G[:], self.G[:], bc(negA[:]), ALU.mult)
            softplus(self.DT[:], self.DTraw[:], hp[:, 8:12])
            self.memset("pool", self.G[0:112, 0, :], 0.0)
            self.memset("pool", self.BETA[0:112, 0, :], 0.0)
            self.memset("pool", self.DT[0:112, 0, :], 0.0)

            s4 = sb("s4", [4, 4])
            pt1 = pstat
            r14 = sb("r14", [1, 4])
            m11 = sb("m11", [1, 2])
            pbc = pA
            for i in range(4):
                self.tr(pt1[0:1, 0:4], self.stats[0:4, i:i + 1], self.identf[0:4, 0:4])
                self.red("dve", r14[0:1, i:i + 1], pt1[0:1, 0:4], ALU.max)
            self.tt("dve", m11[0:1, 0:1], r14[0:1, 0:1], r14[0:1, 1:2], ALU.mult)
            self.tt("dve", m11[0:1, 1:2], r14[0:1, 2:3], r14[0:1, 3:4], ALU.mult)
            self.act(m11[:], m11[:], AF.Sqrt)
            self.ts("dve", m11[:], m11[:], -1.0, ALU.mult)
            self.mm(pbc[:, 0:2], self.onesf[0:1, :], m11[0:1, 0:2])
            self.cp("dve", self.mbias[:], pbc[:, 0:2])
            self.flush()

    def phase_sw(self, l):
        NT, T = self.NT, self.T
        with ExitStack() as st:
            sb = lambda n, s, dt=F32: self.sb(st, n, s, dt)
            ps = lambda n, s, dt=F32: self.ps(st, n, s, dt)
            QT = sb("swQT", [128, 2, T], BF16)
            KT = sb("swKT", [128, T], BF16)
            for c in range(2):
                self.dma("sp", QT[:, c, :], self.SW_QT[c, :, :])
            self.dma("sp", KT[:], self.SW_KT[:, :])
            QTp = sb("swQTp", [128, 4, T], BF16)
            self.memset("pool", QTp[:], 0.0)
            for h in range(4):
                kv = h // 2
                self.cp("pool", QTp[64 * kv:64 * kv + 64, h, :], QT[64 * kv:64 * kv + 64, h % 2, :])
            Vst = sb("swVst", [128, NT, 128])
            self.dma("sp", Vst[:], self.TOK2.rearrange("(n p) c -> p n c", p=128)[:, :, 256:384])
            Vaug = sb("swVaug", [128, NT, 2, 66], BF16)
            self.cp("dve", Vaug[:, :, :, 0:64], Vst[:].rearrange("p n (g d) -> p n g d", d=64))
            self.memset("pool", Vaug[:, :, :, 64:66], 0.0)
            self.memset("pool", Vaug[:, :, :, 64:65], 1.0)
            self.memset("pool", Vaug[0:112, 0, :, :], 0.0)
            sk = sb("swsk", [128, 4])
            self.dma("sp", sk[:], self.sw_sinks[l].partition_broadcast(128))
            esink = sb("esink", [128, 4])
            self.act(esink[:], sk[:], AF.Exp, bias=self.mbias[:, 1:2])
            PTp = sb("PTp", [128, 4, 128], BF16)
            PTo = sb("PTo", [128, 4, 128], BF16)
            zt = sb("swz", [128, 4])
            ysw = [sb("ysw0", [128, 4, 64]), sb("ysw1", [128, 4, 64])]
            pSp = ps("pSp", [128, 4, 128])
            pSo = ps("pSo", [128, 4, 128])
            pO = [ps("pO0", [128, 512])[:, 0:264].rearrange("p (h d) -> p h d", d=66), ps("pO1", [128, 512])[:, 0:264].rearrange("p (h d) -> p h d", d=66)]
            mixt = self.MIX.rearrange("(n p) c -> p n c", p=128)
            for i in range(NT):
                qs = slice(i * 128, (i + 1) * 128)
                blks = ([(i - 1, pSp, PTp, self.p01T)] if i > 0 else []) + [(i, pSo, PTo, self.c01T)]
                for (j, pS, PT, msk) in blks:
                    ks = slice(j * 128, (j + 1) * 128)
                    for h in range(4):
                        self.mm(pS[:, h, :], KT[:, ks], QTp[:, h, qs])
                    self.act(PT[:], pS[:], AF.Exp, bias=self.mbias[:, 1:2])
                    self.tt("pool", PT[:], PT[:], msk[:].unsqueeze(1).to_broadcast([128, 4, 128]), ALU.mult)
                po = pO[i % 2]
                for h in range(4):
                    kv = h // 2
                    for bi, (j, pS, PT, msk) in enumerate(blks):
                        self.mm(po[:, h, :], PT[:, h, :], Vaug[:, j, kv, :], start=(bi == 0), stop=(bi == len(blks) - 1))
                self.tt("dve", zt[:], po[:, :, 64], esink[:], ALU.add)
                self.recip(zt[:], zt[:])
                y = ysw[i % 2]
                self.tt("dve", y[:], po[:, :, 0:64], zt[:].unsqueeze(2).to_broadcast([128, 4, 64]), ALU.mult)
                self.dma("sp", mixt[:, i, 512:768], y[:].rearrange("p h d -> p (h d)"))
            self.flush()

    def phase_df(self, l):
        NT, T = self.NT, self.T
        import math
        lam_init = 0.8 - 0.6 * math.exp(-0.3 * l)
        with ExitStack() as st:
            sb = lambda n, s, dt=F32: self.sb(st, n, s, dt)
            ps = lambda n, s, dt=F32: self.ps(st, n, s, dt)
            QT = sb("dfQT", [128, 2, T], BF16)
            KT = sb("dfKT", [128, 2, T], BF16)
            for c in range(2):
                self.dma("sp", QT[:, c, :], self.DF_QT[c, :, :])
                self.dma("sp", KT[:, c, :], self.DF_KT[c, :, :])
            QTp = sb("dfQTp", [128, 8, T], BF16)
            self.memset("pool", QTp[:], 0.0)
            for hm in range(8):
                pb = 32 * (hm % 4)
                self.cp("pool" if hm % 2 else "dve", QTp[pb:pb + 32, hm, :], QT[pb:pb + 32, hm // 4, :])
            Vst = sb("dfVst", [128, NT, 256])
            self.dma("sp", Vst[:], self.TOK2.rearrange("(n p) c -> p n c", p=128)[:, :, 0:256])
            Vaug = sb("dfVaug", [128, NT, 4, 66], BF16)
            self.cp("dve", Vaug[:, :, :, 0:64], Vst[:].rearrange("p n (g d) -> p n g d", d=64))
            self.memset("pool", Vaug[:, :, :, 64:66], 0.0)
            self.memset("pool", Vaug[:, :, :, 64:65], 1.0)
            self.memset("pool", Vaug[0:112, 0, :, :], 0.0)
            lt = sb("dflam", [128, 4, 32])
            self.dma("sp", lt[:].rearrange("p a b -> p (a b)"), self.df_lambda[l].rearrange("a b -> (a b)").partition_broadcast(128))
            lp = sb("dflp", [128, 2, 32])
            l2 = sb("dfl2", [128, 2])
            neglam = sb("neglam", [128, 1])
            self.tt("dve", lp[:, 0, :], lt[:, 0, :], lt[:, 1, :], ALU.mult)
            self.tt("dve", lp[:, 1, :], lt[:, 2, :], lt[:, 3, :], ALU.mult)
            self.red("dve", l2[:], lp[:], ALU.add)
            self.act(l2[:], l2[:], AF.Exp)
            self.tt("dve", neglam[:], l2[:, 1:2], l2[:, 0:1], ALU.subtract)
            self.ts("dve", neglam[:], neglam[:], -lam_init, ALU.add)
            nw = sb("dfnw", [128, 64])
            self.dma("sp", nw[:], self.df_norm_w[l].partition_broadcast(128))
            self.ts("dve", nw[:], nw[:], 1.0 - lam_init, ALU.mult)

            PT = [[sb(f"dfPT{m}{k}", [128, 512], BF16) for k in range(2)] for m in range(2)]
            pS = [[ps(f"dfpS{m}{k}", [128, 512]) for k in range(2)] for m in range(2)]
            pO = [[ps(f"dfpO{m}{k}", [128, 512])[:, 0:264].rearrange("p (h d) -> p h d", d=66) for k in range(2)] for m in range(2)]
            r0 = sb("dfr0", [128, 4])
            r1 = sb("dfr1", [128, 4])
            t0 = sb("dft0", [128, 4, 64])
            t1 = sb("dft1", [128, 4, 64])
            sq = sb("dfsq", [128, 4, 64])
            ss = sb("dfss", [128, 4])
            ydf = [sb("ydf0", [128, 4, 64]), sb("ydf1", [128, 4, 64])]
            mixt = self.MIX.rearrange("(n p) c -> p n c", p=128)
            it = 0
            for (b0, TG) in self.groups():
                for h in range(4):
                    po = [pO[0][it % 2], pO[1][it % 2]]
                    first = [True, True]
                    jj = 0
                    for j in range(0, b0 + TG):
                        qlo = max(j, b0)
                        ncol = (b0 + TG - qlo) * 128
                        qcols = slice(qlo * 128, (b0 + TG) * 128)
                        ks = slice(j * 128, (j + 1) * 128)
                        for m in range(2):
                            hm = 2 * h + m
                            c = hm // 4
                            pb = 32 * (hm % 4)
                            pst = pS[m][jj % 2]
                            ptt = PT[m][jj % 2]
                            self.mm(pst[:, 0:ncol], KT[:, c, ks], QTp[:, hm, qcols])
                            self.act(ptt[:, 0:ncol], pst[:, 0:ncol], AF.Exp, bias=self.mbias[:, 0:1])
                            if j >= b0:
                                self.tt("pool", ptt[:, 0:128], ptt[:, 0:128], self.c01T[:], ALU.mult)
                            for bi in range(qlo, b0 + TG):
                                col = (bi - qlo) * 128
                                self.mm(po[m][:, bi - b0, :], ptt[:, col:col + 128], Vaug[:, j, h, :], start=first[m], stop=False, acc=True)
                                first[m] = False
                        jj += 1
                    if b0 == 0:
                        self.ts("dve", r0[:, 0:TG], po[0][:, 0:TG, 64], 1e-30, ALU.max)
                        self.ts("dve", r1[:, 0:TG], po[1][:, 0:TG, 64], 1e-30, ALU.max)
                        self.recip(r0[:, 0:TG], r0[:, 0:TG])
                        self.recip(r1[:, 0:TG], r1[:, 0:TG])
                    else:
                        self.recip(r0[:, 0:TG], po[0][:, 0:TG, 64])
                        self.recip(r1[:, 0:TG], po[1][:, 0:TG, 64])
                    self.ts("dve", r1[:, 0:TG], r1[:, 0:TG], neglam[:, 0:1], ALU.mult)
                    self.tt("dve", t0[:, 0:TG, :], po[0][:, 0:TG, 0:64], r0[:, 0:TG].unsqueeze(2).to_broadcast([128, TG, 64]), ALU.mult)
                    self.tt("dve", t1[:, 0:TG, :], po[1][:, 0:TG, 0:64], r1[:, 0:TG].unsqueeze(2).to_broadcast([128, TG, 64]), ALU.mult)
                    self.tt("pool", t0[:, 0:TG, :], t0[:, 0:TG, :], t1[:, 0:TG, :], ALU.add)
                    self.tt("pool", sq[:, 0:TG, :], t0[:, 0:TG, :], t0[:, 0:TG, :], ALU.mult)
                    self.red("dve", ss[:, 0:TG], sq[:, 0:TG, :], ALU.add)
                    self.act(ss[:, 0:TG], ss[:, 0:TG], AF.Sqrt, bias=self.epsb[:, 0:1], scale=1.0 / 64)
                    self.recip(ss[:, 0:TG], ss[:, 0:TG])
                    y = ydf[it % 2]
                    self.tt("dve", y[:, 0:TG, :], t0[:, 0:TG, :], ss[:, 0:TG].unsqueeze(2).to_broadcast([128, TG, 64]), ALU.mult)
                    self.tt("pool", y[:, 0:TG, :], y[:, 0:TG, :], nw[:].unsqueeze(1).to_broadcast([128, TG, 64]), ALU.mult)
                    self.dma("sp", mixt[:, b0:b0 + TG, 256 + h * 64:256 + (h + 1) * 64], y[:, 0:TG, :])
                    it += 1
            self.flush()

    def chunk_cumsum(self, st, src, name):
        NT = self.NT
        cum = self.sb(st, name + "cum", [128, NT, 4])
        last = self.sb(st, name + "last", [128, NT, 4])
        pc = self.ps(st, name + "pc", [128, 512])
        n4 = NT * 4
        self.mm(pc[:, 0:n4], self.Umat[:], src[:].rearrange("p n h -> p (n h)"))
        self.cp("dve", cum[:].rearrange("p n h -> p (n h)"), pc[:, 0:n4])
        self.mm(pc[:, 0:n4], self.E127[:], cum[:].rearrange("p n h -> p (n h)"))
        self.cp("dve", last[:].rearrange("p n h -> p (n h)"), pc[:, 0:n4])
        return cum, last, pc

    def phase_ssd(self, l):
        NT, T = self.NT, self.T
        with ExitStack() as st:
            sb = lambda n, s, dt=F32: self.sb(st, n, s, dt)
            ps = lambda n, s, dt=F32: self.ps(st, n, s, dt)
            hp = sb("sshp", [128, 8])
            self.dma("sp", hp[:, 0:4], self.ssd_a_log[l].partition_broadcast(128))
            self.dma("sp", hp[:, 4:8], self.ssd_d[l].partition_broadcast(128))
            nw = sb("ssnw", [128, 256])
            self.dma("sp", nw[:], self.ssd_norm_w[l].partition_broadcast(128))
            negA = sb("ssnegA", [128, 4])
            self.act(negA[:], hp[:, 0:4], AF.Exp)
            self.ts("dve", negA[:], negA[:], -1.0, ALU.mult)
            DA = sb("ssDA", [128, NT, 4])
            self.tt("dve", DA[:], self.DT[:], negA[:].unsqueeze(1).to_broadcast([128, NT, 4]), ALU.mult)
            ACUM, AL, pc = self.chunk_cumsum(st, DA, "ss")
            EAC = sb("ssEAC", [128, NT, 4])
            EDEC = sb("ssEDEC", [128, NT, 4])
            ECH = sb("ssECH", [128, NT, 4])
            self.act(EAC[:], ACUM[:], AF.Exp)
            self.tt("dve", EDEC[:], AL[:], ACUM[:], ALU.subtract)
            self.act(EDEC[:], EDEC[:], AF.Exp)
            self.act(ECH[:], AL[:], AF.Exp)
            hT = sb("sshT", [128, 4, 64])
            self.memset("pool", hT[:], 0.0)
            tok = sb("sstok", [128, 768])
            ft = sb("ssft", [128, 4, 128])
            diag = sb("ssdiag", [128, 4, 128])
            X = sb("ssX", [128, 4, 128])
            lmT = sb("sslmT", [128, 4, 128])
            WT = sb("ssWT", [128, 4, 128])
            xc = sb("ssxc", [128, 4, 64])
            xdec = sb("ssxdec", [128, 4, 64])
            t1 = sb("sst1", [128, 4, 64])
            y = sb("ssy", [128, 256])
            sz = sb("sssz", [128, 256])
            sq = sb("sssq", [128, 256])
            ss = sb("ssss", [128, 2])
            pBr = ps("sspBr", [128, 4, 128])
            pBC = ps("sspBC", [128, 4, 128])
            pY = ps("sspY", [128, 4, 128])
            pSt = ps("sspSt", [128, 4, 128])
            mixt = self.MIX.rearrange("(n p) c -> p n c", p=128)
            stok = self.SSD_TOK.rearrange("(n p) c -> p n c", p=128)
            bc64 = lambda ap: ap.unsqueeze(2).to_broadcast([128, 4, 64])
            bc128 = lambda ap: ap.unsqueeze(2).to_broadcast([128, 4, 128])
            for n in range(NT):
                self.dma("sp", tok[:], stok[:, n, :])
                self.dma("sp", ft[:], self.SSD_FT[:, :, n * 128:(n + 1) * 128].rearrange("c p t -> p c t"))
                x3 = tok[:, 0:256].rearrange("p (h d) -> p h d", d=64)
                self.tt("pool", diag[:], self.identf[:].unsqueeze(1).to_broadcast([128, 4, 128]), bc128(ACUM[:, n, :]), ALU.mult)
                for h in range(4):
                    self.mm(pBr[:, h, :], self.onesf[:], diag[:, h, :])
                self.tt("dve", X[:], pBr[:], bc128(ACUM[:, n, :]), ALU.subtract)
                self.tt("dve", X[:], X[:], self.mTincl[:].unsqueeze(1).to_broadcast([128, 4, 128]), ALU.min)
                self.act(lmT[:], X[:], AF.Exp)
                for g in range(2):
                    self.mm(pBC[:, g, :], ft[:, g, :], ft[:, 2 + g, :])
                for g in range(2):
                    self.tt("dve", WT[:, 2 * g:2 * g + 2, :], pBC[:, g, :].unsqueeze(1).to_broadcast([128, 2, 128]), lmT[:, 2 * g:2 * g + 2, :], ALU.mult)
                self.tt("pool", xc[:], x3, bc64(self.DT[:, n, :]), ALU.mult)
                for h in range(4):
                    self.mm(pY[:, h, 0:64], WT[:, h, :], xc[:, h, :])
                for h in range(4):
                    self.mm(pY[:, h, 64:128], ft[:, 2 + h // 2, :], hT[:, h, :])
                self.tt("dve", t1[:], pY[:, :, 64:128], bc64(EAC[:, n, :]), ALU.mult)
                self.tt("dve", t1[:], t1[:], pY[:, :, 0:64], ALU.add)
                y3 = y[:].rearrange("p (h d) -> p h d", d=64)
                self.tt("pool", y3, x3, bc64(hp[:, 4:8]), ALU.mult)
                self.tt("pool", y3, y3, t1[:], ALU.add)
                self.tt("pool", xdec[:], xc[:], bc64(EDEC[:, n, :]), ALU.mult)
                for h in range(4):
                    g = h // 2
                    self.mm(pSt[:, h, 0:64], tok[:, 256 + 128 * g:256 + 128 * (g + 1)], xdec[:, h, :])
                self.tt("pool", hT[:], hT[:], bc64(ECH[:, n, :]), ALU.mult)
                self.tt("dve", hT[:], hT[:], pSt[:, :, 0:64], ALU.add)
                self.act(sz[:], tok[:, 512:768], AF.Silu)
                self.tt("dve", y[:], y[:], sz[:], ALU.mult)
                self.tt("pool", sq[:], y[:], y[:], ALU.mult)
                self.red("dve", ss[:], sq[:].rearrange("p (g d) -> p g d", d=128), ALU.add)
                self.act(ss[:], ss[:], AF.Sqrt, bias=self.epsb[:, 0:1], scale=1.0 / 128)
                self.recip(ss[:], ss[:])
                yg = y[:].rearrange("p (g d) -> p g d", d=128)
                self.tt("dve", yg, yg, ss[:].unsqueeze(2).to_broadcast([128, 2, 128]), ALU.mult)
                self.tt("pool", y[:], y[:], nw[:], ALU.mult)
                self.dma("sp", mixt[:, n, 768:1024], y[:])
            self.flush()

    def phase_dn(self, l):
        NT, T = self.NT, self.T
        with ExitStack() as st:
            sb = lambda n, s, dt=F32: self.sb(st, n, s, dt)
            ps = lambda n, s, dt=F32: self.ps(st, n, s, dt)
            nw = sb("dnnw", [128, 64])
            self.dma("sp", nw[:], self.dn_norm_w[l].partition_broadcast(128))
            GC, GL, pc = self.chunk_cumsum(st, self.G, "dn")
            EGC = sb("dnEGC", [128, NT, 4])
            EKD = sb("dnEKD", [128, NT, 4])
            EGL = sb("dnEGL", [128, NT, 4])
            BGE = sb("dnBGE", [128, NT, 4])
            NEGB = sb("dnNEGB", [128, NT, 4])
            EGLS = sb("dnEGLS", [128, NT, 2])
            self.act(EGC[:], GC[:], AF.Exp)
            self.tt("dve", EKD[:], GL[:], GC[:], ALU.subtract)
            self.act(EKD[:], EKD[:], AF.Exp)
            self.act(EGL[:], GL[:], AF.Exp)
            self.tt("dve", BGE[:], self.BETA[:], EGC[:], ALU.mult)
            self.ts("dve", NEGB[:], self.BETA[:], -1.0, ALU.mult)
            self.cp("dve", EGLS[0:64, :, :], EGL[0:64, :, 0:4:2])
            self.cp("dve", EGLS[64:128, :, :], EGL[64:128, :, 1:4:2])
            Sst = sb("dnS", [128, 2, 64])
            self.memset("pool", Sst[:], 0.0)
            tok = sb("dntok", [128, 1024])
            qkT = sb("dnqkT", [128, 4, 128])
            qpad = sb("dnqpad", [128, 2, 2, 128])
            kpad = sb("dnkpad", [128, 2, 2, 128])
            self.memset("pool", qpad[:], 0.0)
            self.memset("pool", kpad[:], 0.0)
            diag = sb("dndiag", [128, 4, 128])
            X = sb("dnX", [128, 4, 128])
            dS = sb("dndS", [128, 4, 128])
            dT = sb("dndT", [128, 4, 128])
            attnT = sb("dnattnT", [128, 4, 128])
            Pb = [sb("dnP0", [128, 4, 128]), sb("dnP1", [128, 4, 128])]
            Qb = [sb("dnQ0", [128, 4, 128]), sb("dnQ1", [128, 4, 128])]
            W = sb("dnW", [128, 4, 128])
            vb = sb("dnvb", [128, 4, 64])
            kdec = sb("dnkdec", [128, 4, 64])
            Rt = sb("dnR", [128, 4, 64])
            vnew = sb("dnvnew", [128, 4, 64])
            o = sb("dno", [128, 4, 64])
            sq = sb("dnsq", [128, 4, 64])
            ss = sb("dnss", [128, 4])
            sz = sb("dnsz", [128, 256])
            B = [ps(f"dnB{i}", [128, 4, 128]) for i in range(6)]
            mixt = self.MIX.rearrange("(n p) c -> p n c", p=128)
            dtok = self.DN_TOK.rearrange("(n p) c -> p n c", p=128)
            bc64 = lambda ap: ap.unsqueeze(2).to_broadcast([128, 4, 64])
            bc128 = lambda ap: ap.unsqueeze(2).to_broadcast([128, 4, 128])
            mb = lambda m_: m_[:].unsqueeze(1).to_broadcast([128, 4, 128])
            hsl = lambda h: slice(64 * (h % 2), 64 * (h % 2) + 64)
            for n in range(NT):
                self.dma("sp", tok[:], dtok[:, n, :])
                q3 = tok[:, 0:256].rearrange("p (h d) -> p h d", d=64)
                k3 = tok[:, 256:512].rearrange("p (h d) -> p h d", d=64)
                v3 = tok[:, 512:768].rearrange("p (h d) -> p h d", d=64)
                pT = B[0]
                for i in range(4):
                    self.tr(pT[:, i, :], tok[:, i * 128:(i + 1) * 128], self.identf[:])
                self.cp("act", qkT[:], pT[:])
                self.cp("act", qpad[0:64, 0, :, :], pT[0:64, 0:2, :])
                self.cp("dve", qpad[64:128, 1, :, :], pT[64:128, 0:2, :])
                self.cp("act", kpad[0:64, 0, :, :], pT[0:64, 2:4, :])
                self.cp("dve", kpad[64:128, 1, :, :], pT[64:128, 2:4, :])
                qT = lambda h: qpad[:, h % 2, h // 2, :]
                kT = lambda h: kpad[:, h % 2, h // 2, :]
                pKK, pKQ, pBr = B[1], B[2], B[3]
                for h in range(4):
                    self.mm(pKK[:, h, :], kT(h), qkT[:, 2 + h // 2, :])
                for h in range(4):
                    self.mm(pKQ[:, h, :], kT(h), qkT[:, h // 2, :])
                self.tt("pool", diag[:], self.identf[:].unsqueeze(1).to_broadcast([128, 4, 128]), bc128(GC[:, n, :]), ALU.mult)
                for h in range(4):
                    self.mm(pBr[:, h, :], self.onesf[:], diag[:, h, :])
                self.tt("dve", X[:], pBr[:], bc128(GC[:, n, :]), ALU.subtract)
                self.tt("dve", dS[:], X[:], mb(self.mstrict), ALU.max)
                self.act(dS[:], dS[:], AF.Exp, scale=-1.0)
                self.tt("dve", dT[:], X[:], mb(self.mTincl), ALU.min)
                self.act(dT[:], dT[:], AF.Exp)
                P, Q = Pb[0], Qb[0]
                self.tt("dve", P[:], pKK[:], dS[:], ALU.mult)
                self.tt("pool", P[:], P[:], bc128(NEGB[:, n, :]), ALU.mult)
                self.tt("dve", attnT[:], pKQ[:], dT[:], ALU.mult)
                pQ = B[0]
                for h in range(4):
                    self.tr(pQ[:, h, :], P[:, h, :], self.identf[:])
                self.cp("act", Q[:], pQ[:])
                self.tt("pool", W[:], Q[:], self.identf[:].unsqueeze(1).to_broadcast([128, 4, 128]), ALU.add)
                pP, pW = B[1], B[2]
                for k in range(6):
                    P, Q = Pb[k % 2], Qb[k % 2]
                    Pn, Qn = Pb[(k + 1) % 2], Qb[(k + 1) % 2]
                    for h in range(4):
                        self.mm(pP[:, h, :], Q[:, h, :], P[:, h, :])
                    if k < 5:
                        for h in range(4):
                            self.mm(pQ[:, h, :], P[:, h, :], Q[:, h, :])
                    self.cp("act", Pn[:], pP[:])
                    if k < 5:
                        self.cp("dve", Qn[:], pQ[:])
                    for h in range(4):
                        self.mm(pW[:, h, :], Pn[:, h, :], W[:, h, :])
                    self.tt("dve", W[:], W[:], pW[:], ALU.add)
                self.tt("pool", vb[:], v3, bc64(self.BETA[:, n, :]), ALU.mult)
                self.tt("pool", kdec[:], k3, bc64(EKD[:, n, :]), ALU.mult)
                pA, pV, pSU = B[3], B[4], B[5]
                for h in range(4):
                    self.mm(pA[:, h, 0:64], kT(h), Sst[:, h // 2, :])
                for h in range(4):
                    self.mm(pA[:, h, 64:128], qT(h), Sst[:, h // 2, :])
                self.tt("dve", Rt[:], pA[:, :, 0:64], bc64(BGE[:, n, :]), ALU.mult)
                self.tt("dve", Rt[:], vb[:], Rt[:], ALU.subtract)
                for h in range(4):
                    self.mm(pV[:, h, 0:64], W[:, h, :], Rt[:, h, :])
                self.cp("act", vnew[:], pV[:, :, 0:64])
                for h in range(4):
                    self.mm(pV[:, h, 64:128], attnT[:, h, :], vnew[:, h, :])
                for pr_ in range(2):
                    self.mm(pSU[:, pr_, :], kdec[:, 2 * pr_:2 * pr_ + 2, :].rearrange("p h d -> p (h d)"),
                            vnew[:, 2 * pr_:2 * pr_ + 2, :].rearrange("p h d -> p (h d)"))
                self.tt("dve", o[:], pA[:, :, 64:128], bc64(EGC[:, n, :]), ALU.mult)
                self.tt("dve", o[:], o[:], pV[:, :, 64:128], ALU.add)
                self.tt("pool", Sst[:], Sst[:], EGLS[:, n, :].unsqueeze(2).to_broadcast([128, 2, 64]), ALU.mult)
                self.tt("dve", Sst[0:64, :, :], Sst[0:64, :, :], pSU[0:64, 0:2, 0:64], ALU.add)
                self.tt("dve", Sst[64:128, :, :], Sst[64:128, :, :], pSU[64:128, 0:2, 64:128], ALU.add)
                self.tt("pool", sq[:], o[:], o[:], ALU.mult)
                self.red("dve", ss[:], sq[:], ALU.add)
                self.act(ss[:], ss[:], AF.Sqrt, bias=self.epsb[:, 0:1], scale=1.0 / 64)
                self.recip(ss[:], ss[:])
                self.tt("dve", o[:], o[:], bc64(ss[:]), ALU.mult)
                self.tt("pool", o[:], o[:], nw[:].unsqueeze(1).to_broadcast([128, 4, 64]), ALU.mult)
                self.act(sz[:], tok[:, 768:1024], AF.Silu)
                self.tt("dve", o[:], o[:], sz[:].rearrange("p (h d) -> p h d", d=64), ALU.mult)
                self.dma("sp", mixt[:, n, 0:256], o[:].rearrange("p h d -> p (h d)"))
            self.flush()

    def phase3(self, l):
        self.phase3a(l)
        if l % 2 == 0:
            i = l // 2
            self.ffn(1, DFF, lambda e: self.ffn_w_gate[i], lambda e: self.ffn_w_up[i], lambda e: self.ffn_w_down[i], gated=False)
        else:
            i = l // 2
            self.ffn(NEXP, DFFE, lambda e: self.moe_w_gate[i, e], lambda e: self.moe_w_up[i, e], lambda e: self.moe_w_down[i, e], gated=True)

    def phase3a(self, l):
        NT, T = self.NT, self.T
        moe = (l % 2 == 1)
        with ExitStack() as st:
            sb = lambda n, s, dt=F32: self.sb(st, n, s, dt)
            ps = lambda n, s, dt=F32: self.ps(st, n, s, dt)
            wout = sb("wout", [128, 8, D], BF16)
            self.dma("pool", wout[:], self.w_out[l].rearrange("(c p) d -> p c d", p=128))
            normw = sb("normw2", [128, D])
            self.dma("sp", normw[:], self.norm_ffn[l].partition_broadcast(128))
            hs = sb("hs3", [128, 4, D])
            mx = sb("mx3", [128, 4, D])
            mxb = sb("mxb3", [128, 4, D], BF16)
            mixT = sb("mixT", [128, 8, 512], BF16)
            junk = sb("junk3", [128, D])
            ssq = sb("ssq3", [128, 4])
            rstd = sb("rstd3", [128, 4])
            ub = sb("ub3", [128, 4, D], BF16)
            uT = sb("uT3", [128, 8, 512], BF16)
            tp = ps("tp3", [128, 8, 128], BF16)
            pH = [ps("pH0", [128, 512]), ps("pH1", [128, 512])]
            if moe:
                rt = sb("router", [128, 8, NEXP])
                self.dma("sp", rt[:], self.moe_router[l // 2].rearrange("(c p) e -> p c e", p=128))
                uf = sb("uf3", [128, D])
                ufT = sb("ufT3", [128, 8, 128])
                ptf = ps("ptf3", [128, 4, 128])
                pL = ps("pL3", [128, 512])
                lg = sb("lg3", [128, NEXP])
                m1 = sb("m13", [128, 1])
                msk = sb("msk3", [128, NEXP])
                l2 = sb("l23", [128, NEXP])
                ex = sb("ex3", [128, NEXP])
                den = sb("den3", [128, 1])
                gT = sb("gT3", [NEXP, 512])
            mixt = self.MIX.rearrange("(n p) c -> p n c", p=128)
            ht = self.H.rearrange("(n p) d -> p n d", p=128)
            for (n0, TG) in self.groups():
                TW = TG * 128
                self.load_h_group(l, hs, n0, TG)
                self.dma("sp", mx[:, 0:TG, :], mixt[:, n0:n0 + TG, :])
                for j in range(TG):
                    self.cp("pool", mxb[:, j, :], mx[:, j, :])
                if n0 == 0:
                    self.memset("pool", mxb[0:112, 0, :], 0.0)
                for j in range(TG):
                    for c in range(8):
                        self.tr(tp[:, c, :], mxb[:, j, c * 128:(c + 1) * 128], self.identb[:])
                    self.cp("act", mixT[:, :, j * 128:(j + 1) * 128], tp[:, :, :])
                for j in range(TG):
                    for hf in range(2):
                        for c in range(8):
                            self.mm(pH[hf][:], mixT[:, c, j * 128:(j + 1) * 128], wout[:, c, hf * 512:(hf + 1) * 512], start=(c == 0), stop=(c == 7))
                        self.tt("dve", hs[:, j, hf * 512:(hf + 1) * 512], hs[:, j, hf * 512:(hf + 1) * 512], pH[hf][:], ALU.add)
                self.dma("sp", ht[:, n0:n0 + TG, :], hs[:, 0:TG, :])
                for j in range(TG):
                    self.tt("pool", junk[:], hs[:, j, :], hs[:, j, :], ALU.mult)
                    self.red("dve", ssq[:, j:j + 1], junk[:], ALU.add)
                self.act(rstd[:, 0:TG], ssq[:, 0:TG], AF.Sqrt, bias=self.epsb[:, 0:1], scale=1.0 / D)
                self.recip(rstd[:, 0:TG], rstd[:, 0:TG])
                for j in range(TG):
                    if moe:
                        self.stt("dve", uf[:], hs[:, j, :], rstd[:, j:j + 1], normw[:], ALU.mult, ALU.mult)
                        self.cp("pool", ub[:, j, :], uf[:])
                        for c4 in range(2):
                            for c in range(4):
                                self.tr(ptf[:, c, :], uf[:, (c4 * 4 + c) * 128:(c4 * 4 + c + 1) * 128], self.identf[:])
                            self.cp("act", ufT[:, c4 * 4:c4 * 4 + 4, :], ptf[:])
                        for c in range(8):
                            self.mm(pL[:, 0:NEXP], ufT[:, c, :], rt[:, c, :], start=(c == 0), stop=(c == 7))
                        self.cp("dve", lg[:], pL[:, 0:NEXP])
                        self.red("dve", m1[:], lg[:], ALU.max)
                        self.ts("dve", msk[:], lg[:], m1[:, 0:1], ALU.is_equal)
                        self.stt("dve", l2[:], msk[:], -BIG, lg[:], ALU.mult, ALU.add)
                        self.red("dve", den[:], l2[:], ALU.max)
                        self.ts("dve", msk[:], lg[:], den[:, 0:1], ALU.is_ge)
                        self.ts("dve", m1[:], m1[:], -1.0, ALU.mult)
                        self.act(ex[:], lg[:], AF.Exp, bias=m1[:, 0:1])
                        self.tt("dve", ex[:], ex[:], msk[:], ALU.mult)
                        self.red("dve", den[:], ex[:], ALU.add)
                        self.recip(den[:], den[:])
                        self.ts("dve", ex[:], ex[:], den[:, 0:1], ALU.mult)
                        self.tr(pL[0:NEXP, 128:256], ex[:], self.identf[:])
                        self.cp("dve", gT[:, j * 128:(j + 1) * 128], pL[0:NEXP, 128:256])
                    else:
                        self.stt("dve", ub[:, j, :], hs[:, j, :], rstd[:, j:j + 1], normw[:], ALU.mult, ALU.mult)
                for j in range(TG):
                    for c in range(8):
                        self.tr(tp[:, c, :], ub[:, j, c * 128:(c + 1) * 128], self.identb[:])
                    self.cp("act", uT[:, :, j * 128:(j + 1) * 128], tp[:, :, :])
                self.dma("sp", self.U2T[:, :, n0 * 128:n0 * 128 + TW].rearrange("c p t -> p c t"), uT[:, :, 0:TW])
                if moe:
                    self.dma("sp", self.GATES[:, n0 * 128:n0 * 128 + TW], gT[:, 0:TW])
            self.flush()

    def ffn(self, NE, FF, wg_of, wu_of, wd_of, gated):
        NT, T = self.NT, self.T
        nfc = FF // 128
        blocks = []
        f = 0
        while f < nfc:
            b = min(4, nfc - f)
            blocks.append((f, b))
            f += b
        halves = []
        n = 0
        HMAX = getattr(self, "HMAX", 17)
        while n < NT:
            k = min(HMAX, NT - n)
            halves.append((n, k))
            n += k
        with ExitStack() as st:
            sb = lambda n, s, dt=F32: self.sb(st, n, s, dt)
            ps = lambda n, s, dt=F32: self.ps(st, n, s, dt)
            hmax = max(k for _, k in halves)
            u2T = sb("fu2T", [128, 8, hmax * 128], BF16)
            yacc = sb("fyacc", [128, hmax, D])
            wg = [sb(f"fwg{i}", [128, 8, 512], BF16) for i in range(2)]
            wu = [sb(f"fwu{i}", [128, 8, 512], BF16) for i in range(2)]
            wd = [sb(f"fwd{i}", [128, 4, D], BF16) for i in range(2)]
            sg = [sb(f"fsg{i}", [128, 512]) for i in range(2)]
            actT = [sb(f"fact{i}", [128, 4, 512], BF16) for i in range(2)]
            hb = [sb(f"fhb{i}", [128, D]) for i in range(2)]
            if gated:
                gbc = sb("fgbc", [128, hmax * 128])
            pG = [ps(f"fpG{i}", [128, 512]) for i in range(2)]
            pU = [ps(f"fpU{i}", [128, 512]) for i in range(2)]
            pD = [[ps(f"fpD{i}{hf}", [128, 512]) for hf in range(2)] for i in range(2)]
            ht = self.H.rearrange("(n p) d -> p n d", p=128)
            wi = 0
            gi = 0
            ai = 0
            di = 0
            for (h0, hk) in halves:
                TH = hk * 128
                self.dma("sp", u2T[:, :, 0:TH], self.U2T[:, :, h0 * 128:h0 * 128 + TH].rearrange("c p t -> p c t"))
                self.memset("pool", yacc[:, 0:hk, :], 0.0)
                groups = []
                j = 0
                while j < hk:
                    tg = min(4, hk - j)
                    groups.append((j, tg))
                    j += tg
                for e in range(NE):
                    if gated:
                        self.dma("sp", gbc[:, 0:TH], self.GATES[e, h0 * 128:h0 * 128 + TH].partition_broadcast(128))
                    for (f0, fb) in blocks:
                        FB = fb * 128
                        g_, u_, d_ = wg[wi % 2], wu[wi % 2], wd[wi % 2]
                        wi += 1
                        self.dma("pool", g_[:, :, 0:FB], wg_of(e)[:, f0 * 128:f0 * 128 + FB].rearrange("(c p) f -> p c f", p=128))
                        self.dma("pool", u_[:, :, 0:FB], wu_of(e)[:, f0 * 128:f0 * 128 + FB].rearrange("(c p) f -> p c f", p=128))
                        self.dma("pool", d_[:, 0:fb, :], wd_of(e)[f0 * 128:f0 * 128 + FB, :].rearrange("(c p) d -> p c d", p=128))
                        for (j0, tg) in groups:
                            TW = tg * 128
                            cs_ = slice(j0 * 128, j0 * 128 + TW)
                            a_ = actT[ai % 2]
                            ai += 1
                            for fc in range(fb):
                                pg, pu, s_ = pG[gi % 2], pU[gi % 2], sg[gi % 2]
                                gi += 1
                                for c in range(8):
                                    self.mm(pg[:, 0:TW], g_[:, c, fc * 128:(fc + 1) * 128], u2T[:, c, cs_], start=(c == 0), stop=(c == 7))
                                for c in range(8):
                                    self.mm(pu[:, 0:TW], u_[:, c, fc * 128:(fc + 1) * 128], u2T[:, c, cs_], start=(c == 0), stop=(c == 7))
                                self.act(s_[:, 0:TW], pg[:, 0:TW], AF.Silu)
                                if gated:
                                    self.tt("dve", s_[:, 0:TW], s_[:, 0:TW], pu[:, 0:TW], ALU.mult)
                                    self.tt("pool", a_[:, fc, 0:TW], s_[:, 0:TW], gbc[:, cs_], ALU.mult)
                                else:
                                    self.tt("dve", a_[:, fc, 0:TW], s_[:, 0:TW], pu[:, 0:TW], ALU.mult)
                            for j in range(tg):
                                pd = pD[di % 2]
                                di += 1
                                for hf in range(2):
                                    for fc in range(fb):
                                        self.mm(pd[hf][:], a_[:, fc, j * 128:(j + 1) * 128], d_[:, fc, hf * 512:(hf + 1) * 512], start=(fc == 0), stop=(fc == fb - 1))
                                    ya = yacc[:, j0 + j, hf * 512:(hf + 1) * 512]
                                    self.tt("dve", ya, ya, pd[hf][:], ALU.add)
                for j in range(hk):
                    b_ = hb[j % 2]
                    self.dma("sp", b_[:], ht[:, h0 + j, :])
                    self.tt("pool", b_[:], b_[:], yacc[:, j, :], ALU.add)
                    self.dma("sp", ht[:, h0 + j, :], b_[:])
            self.flush()

    def final(self):
        NT = self.NT
        with ExitStack() as st:
            sb = lambda n, s, dt=F32: self.sb(st, n, s, dt)
            normw = sb("normwf", [128, D])
            self.dma("sp", normw[:], self.norm_final.partition_broadcast(128))
            hs = [sb("hsf0", [128, D]), sb("hsf1", [128, D])]
            junk = sb("junkf", [128, D])
            ssq = sb("ssqf", [128, 2])
            ht = self.H.rearrange("(n p) d -> p n d", p=128)
            ot = self.out.rearrange("(n p) d -> p n d", p=128)
            for n in range(1, NT):
                h = hs[n % 2]
                s_ = ssq[:, (n % 2):(n % 2) + 1]
                self.dma("sp", h[:], ht[:, n, :])
                self.tt("pool", junk[:], h[:], h[:], ALU.mult)
                self.red("dve", s_, junk[:], ALU.add)
                self.act(s_, s_, AF.Sqrt, bias=self.epsb[:, 0:1], scale=1.0 / D)
                self.recip(s_, s_)
                self.stt("dve", h[:], h[:], s_, normw[:], ALU.mult, ALU.mult)
                self.dma("sp", ot[:, n - 1, :], h[:])
            self.flush()


_PARAMS = ["meta_tokens", "norm_mix", "w_in", "dn_conv_w", "dn_a_log", "dn_dt_bias", "dn_norm_w", "df_lambda",
           "df_norm_w", "sw_sinks", "ssd_conv_w", "ssd_conv_b", "ssd_a_log", "ssd_dt_bias", "ssd_d", "ssd_norm_w",
           "w_out", "norm_ffn", "ffn_w_gate", "ffn_w_up", "ffn_w_down", "moe_router", "moe_w_gate", "moe_w_up",
           "moe_w_down", "norm_final"]


def kernel(**inputs):
    from concourse.bass_utils import run_bass_kernel_spmd
    x = np.asarray(inputs["x"], dtype=np.float32)
    B, SEQ, _ = x.shape
    NT = SEQ // 128 + 1
    m = Model(NT, depth=2)
    nc = m.build()
    params = {k: np.ascontiguousarray(np.asarray(inputs[k], dtype=np.float32)) for k in _PARAMS}
    in_maps = []
    for b in range(B):
        d = dict(params)
        d["x"] = np.ascontiguousarray(x[b])
        in_maps.append(d)
    res = run_bass_kernel_spmd(nc, in_maps, core_ids=list(range(B)))
    return np.stack([np.asarray(r["out"], dtype=np.float32) for r in res.results], axis=0)
```

```python
import numpy as np
from contextlib import ExitStack
import concourse.bass as bass
import concourse.mybir as mybir

F32 = mybir.dt.float32
BF16 = mybir.dt.bfloat16
AF = mybir.ActivationFunctionType
ALU = mybir.AluOpType
AX = mybir.AxisListType

ENGS = ("pe", "act", "dve", "pool", "sp")
N_DMA_SEMS = 6


class Buf:
    __slots__ = ("name", "w", "r")

    def __init__(self, name):
        self.name = name
        self.w = None
        self.r = {}


class Sched:
    def __init__(self, nc, stack):
        self.nc = nc
        self.sem = {e: stack.enter_context(nc.semaphore("s_" + e)) for e in ENGS}
        self.dsem = {}
        for q in ("sp", "act", "pool"):
            for i in range(N_DMA_SEMS):
                self.dsem[(q, i)] = stack.enter_context(nc.semaphore(f"d_{q}{i}"))
        self.cnt = {e: 0 for e in ENGS}
        self.dcnt = {k: 0 for k in self.dsem}
        self.drr = {"sp": 0, "act": 0, "pool": 0}
        self.seen = {e: {} for e in ENGS}
        self.ops = {e: [] for e in ENGS}
        self.nops = 0
        import os
        self.limit = int(os.environ.get("MK_LIMIT", "1000000000"))

    def _semh(self, key):
        return self.sem[key] if isinstance(key, str) else self.dsem[key]

    def _need(self, eng, tok, waits):
        if tok is None:
            return
        key, val, teng = tok
        if eng == "pe" and teng == "pe":
            return
        if self.seen[eng].get(key, 0) >= val:
            return
        self.seen[eng][key] = val
        waits[key] = max(waits.get(key, 0), val)

    def _deps(self, eng, reads, writes, waits):
        for b in reads:
            self._need(eng, b.w, waits)
        for b in writes:
            self._need(eng, b.w, waits)
            for k, (v, te) in b.r.items():
                self._need(eng, (k, v, te), waits)

    def _commit(self, tok, reads, writes):
        for b in reads:
            o = b.r.get(tok[0])
            if o is None or o[0] < tok[1]:
                b.r[tok[0]] = (tok[1], tok[2])
        for b in writes:
            b.w = tok
            b.r = {}

    def op(self, eng, fn, reads=(), writes=()):
        if self.nops >= self.limit:
            return None
        waits = {}
        self._deps(eng, reads, writes, waits)
        self.cnt[eng] += 1
        tok = (eng, self.cnt[eng], eng)
        self.ops[eng].append((fn, waits, (eng, 1)))
        self._commit(tok, reads, writes)
        self.nops += 1
        return tok

    def dma(self, q, fn, reads=(), writes=()):
        if self.nops >= self.limit:
            return None
        waits = {}
        self._deps(q, reads, writes, waits)
        i = self.drr[q]
        self.drr[q] = (i + 1) % N_DMA_SEMS
        key = (q, i)
        if self.dcnt[key] > 0:
            self._need(q, (key, self.dcnt[key], "dma"), waits)
        self.dcnt[key] += 16
        tok = (key, self.dcnt[key], "dma")
        self.ops[q].append((fn, waits, (key, 16)))
        self._commit(tok, reads, writes)
        self.nops += 1
        return tok

    def barrier(self):
        for e in ENGS:
            waits = {}
            for k in ENGS:
                if k == "pe" and e == "pe":
                    continue
                if self.cnt[k] > 0 and self.seen[e].get(k, 0) < self.cnt[k]:
                    self.seen[e][k] = self.cnt[k]
                    waits[k] = self.cnt[k]
            for k, v in self.dcnt.items():
                if v > 0 and self.seen[e].get(k, 0) < v:
                    self.seen[e][k] = v
                    waits[k] = v
            if waits:
                self.ops[e].append((None, waits, None))

    def emit(self, block):
        names = {"pe": "tensor", "act": "scalar", "dve": "vector", "pool": "gpsimd", "sp": "sync"}
        for e in ENGS:
            ops = self.ops[e]
            self.ops[e] = []

            def body(engine, ops=ops):
                for fn, waits, inc in ops:
                    for k, v in waits.items():
                        engine.wait_ge(self._semh(k), v)
                    if fn is not None:
                        fn(engine).then_inc(self._semh(inc[0]), inc[1])
            getattr(block, names[e])(body)


D = 1024
NCOLS = 3340
C_DNQ, C_DNK, C_DNV, C_DNZ, C_DNB, C_DNA = 0, 256, 512, 768, 1024, 1028
C_DFQ, C_DFK, C_DFV = 1032, 1288, 1544
C_SWQ, C_SWK, C_SWV = 1800, 2056, 2184
C_SSZ, C_SSX, C_SSB, C_SSC, C_SSDT = 2312, 2568, 2824, 3080, 3336
EPS = 1e-6
DFF = 2816
NEXP = 8
DFFE = 3584
BIG = 1e30


class KB:
    def __init__(self, NT, depth=2, dbg=()):
        self.NT = NT
        self.T = NT * 128
        self.SEQ = self.T - 128
        self.depth = depth
        self.dbg = dbg
        self.nc = bass.Bass("TRN2", target_bir_lowering=False)
        self.bufs = {}
        self.uid = 0
        self.oplog = []
        self.psum_names = set()

    def din(self, name, shape):
        return self.nc.dram_tensor(name, list(shape), F32, kind="ExternalInput").ap()

    def dout(self, name, shape):
        return self.nc.dram_tensor(name, list(shape), F32, kind="ExternalOutput").ap()

    def dscr(self, name, shape, dt=F32):
        return self.nc.dram_tensor(name, list(shape), dt, kind="Internal").ap()

    def sb(self, st, name, shape, dt=F32):
        self.uid += 1
        return st.enter_context(self.nc.sbuf_tensor(f"{name}_{self.uid}", list(shape), dt))

    def ps(self, st, name, shape, dt=F32):
        self.uid += 1
        nm = f"{name}_{self.uid}"
        self.psum_names.add(nm)
        return st.enter_context(self.nc.psum_tensor(nm, list(shape), dt))

    def buf(self, x):
        if isinstance(x, tuple):
            key = x[1]
        else:
            key = x.name
        b = self.bufs.get(key)
        if b is None:
            b = self.bufs[key] = Buf(str(key))
        return b

    @staticmethod
    def ap(x):
        return x[0] if isinstance(x, tuple) else x

    def _op(self, eng, fn, outs, ins):
        rd = [self.buf(i) for i in ins if i is not None and not isinstance(i, (int, float))]
        wr = [self.buf(o) for o in outs]
        wr += [b for b in rd if b.name in self.psum_names and b not in wr]
        import sys
        self.oplog.append((self.S.nops, eng, sys._getframe(1).f_code.co_name, [b.name for b in wr], [b.name for b in rd]))
        self.S.op(eng, fn, reads=rd, writes=wr)

    def mm(self, out, lhsT, rhs, start=True, stop=True, acc=False, **kw):
        o, l, r = self.ap(out), self.ap(lhsT), self.ap(rhs)
        self._op("pe", lambda e: e.matmul(o, lhsT=l, rhs=r, start=start, stop=stop, skip_group_check=True, **kw),
                 [out], [lhsT, rhs] + ([out] if (acc or not start) else []))

    def tr(self, out, in_, ident):
        o, i, d = self.ap(out), self.ap(in_), self.ap(ident)
        self._op("pe", lambda e: e.transpose(o, i, d), [out], [in_, ident])

    def act(self, out, in_, func, bias=0.0, scale=1.0, accum_out=None, eng="act"):
        o, i = self.ap(out), self.ap(in_)
        b = self.ap(bias) if not isinstance(bias, (int, float)) else bias
        s = self.ap(scale) if not isinstance(scale, (int, float)) else scale
        if accum_out is not None:
            a = self.ap(accum_out)
            self._op("act", lambda e: e.activation(o, i, func, bias=b, scale=s, accum_out=a), [out, accum_out], [in_, bias, scale, accum_out])
        else:
            self._op("act", lambda e: e.activation(o, i, func, bias=b, scale=s), [out], [in_, bias, scale])

    def tt(self, eng, out, in0, in1, op):
        o, a, b = self.ap(out), self.ap(in0), self.ap(in1)
        self._op(eng, lambda e: e.tensor_tensor(o, a, b, op), [out], [in0, in1])

    def ts(self, eng, out, in0, s1, op0, s2=None, op1=None):
        o, a = self.ap(out), self.ap(in0)
        v1 = self.ap(s1) if not isinstance(s1, (int, float)) else s1
        v2 = self.ap(s2) if (s2 is not None and not isinstance(s2, (int, float))) else s2
        if op1 is None:
            self._op(eng, lambda e: e.tensor_scalar(o, a, v1, None, op0), [out], [in0, s1])
        else:
            self._op(eng, lambda e: e.tensor_scalar(o, a, v1, v2, op0, op1), [out], [in0, s1, s2])

    def stt(self, eng, out, in0, scalar, in1, op0, op1):
        o, a, b = self.ap(out), self.ap(in0), self.ap(in1)
        sc = self.ap(scalar) if not isinstance(scalar, (int, float)) else scalar
        self._op(eng, lambda e: e.scalar_tensor_tensor(o, a, sc, b, op0, op1), [out], [in0, scalar, in1])

    def cp(self, eng, out, in_):
        o, i = self.ap(out), self.ap(in_)
        if eng == "act":
            self._op(eng, lambda e: e.copy(o, i), [out], [in_])
        else:
            self._op(eng, lambda e: e.tensor_copy(o, i), [out], [in_])

    def memset(self, eng, out, val):
        o = self.ap(out)
        self._op(eng, lambda e: e.memset(o, val), [out], [])

    def red(self, eng, out, in_, op, axis=AX.X):
        o, i = self.ap(out), self.ap(in_)
        self._op(eng, lambda e: e.tensor_reduce(o, i, axis, op), [out], [in_])

    def recip(self, out, in_):
        o, i = self.ap(out), self.ap(in_)
        self._op("dve", lambda e: e.reciprocal(o, i), [out], [in_])

    def asel(self, out, in_, pattern, cmp, fill, base, cm):
        o, i = self.ap(out), self.ap(in_)
        self._op("pool", lambda e: e.affine_select(o, i, pattern=pattern, compare_op=cmp, fill=fill, base=base, channel_multiplier=cm), [out], [in_])

    def dma(self, q, out, in_, slow=False):
        o, i = self.ap(out), self.ap(in_)
        rd = [self.buf(in_)]
        wr = [self.buf(out)]
        self.oplog.append((self.S.nops, q, "dma", [b.name for b in wr], [b.name for b in rd]))
        if slow:
            self.S.dma(q, lambda e: e.dma_start(out=o, in_=i, allow_slow_non_contiguous=True), reads=rd, writes=wr)
        else:
            self.S.dma(q, lambda e: e.dma_start(out=o, in_=i), reads=rd, writes=wr)

    def flush(self):
        self.S.barrier()
        with self.nc.Block() as blk:
            self.S.emit(blk)


class Model(KB):
    def declare_io(self):
        d = self.din
        self.x = d("x", [self.SEQ, D])
        self.meta = d("meta_tokens", [16, D])
        self.norm_mix = d("norm_mix", [2, D])
        self.w_in = d("w_in", [2, D, NCOLS])
        self.dn_conv_w = d("dn_conv_w", [2, 4, 768])
        self.dn_a_log = d("dn_a_log", [2, 4])
        self.dn_dt_bias = d("dn_dt_bias", [2, 4])
        self.dn_norm_w = d("dn_norm_w", [2, 64])
        self.df_lambda = d("df_lambda", [2, 4, 32])
        self.df_norm_w = d("df_norm_w", [2, 64])
        self.sw_sinks = d("sw_sinks", [2, 4])
        self.ssd_conv_w = d("ssd_conv_w", [2, 4, 768])
        self.ssd_conv_b = d("ssd_conv_b", [2, 768])
        self.ssd_a_log = d("ssd_a_log", [2, 4])
        self.ssd_dt_bias = d("ssd_dt_bias", [2, 4])
        self.ssd_d = d("ssd_d", [2, 4])
        self.ssd_norm_w = d("ssd_norm_w", [2, 256])
        self.w_out = d("w_out", [2, D, D])
        self.norm_ffn = d("norm_ffn", [2, D])
        self.ffn_w_gate = d("ffn_w_gate", [1, D, DFF])
        self.ffn_w_up = d("ffn_w_up", [1, D, DFF])
        self.ffn_w_down = d("ffn_w_down", [1, DFF, D])
        self.moe_router = d("moe_router", [1, D, NEXP])
        self.moe_w_gate = d("moe_w_gate", [1, NEXP, D, DFFE])
        self.moe_w_up = d("moe_w_up", [1, NEXP, D, DFFE])
        self.moe_w_down = d("moe_w_down", [1, NEXP, DFFE, D])
        self.norm_final = d("norm_final", [D])
        self.out = self.dout("out", [self.SEQ, D])
        T = self.T
        s = self.dscr
        self.H = s("H", [T, D])
        self.DN_TOK = s("DN_TOK", [T, 1024])
        self.TOK2 = s("TOK2", [T, 384])
        self.SSD_TOK = s("SSD_TOK", [T, 768])
        self.SSD_FT = s("SSD_FT", [4, 128, T])
        self.DF_QT = s("DF_QT", [2, 128, T], BF16)
        self.DF_KT = s("DF_KT", [2, 128, T], BF16)
        self.SW_QT = s("SW_QT", [2, 128, T], BF16)
        self.SW_KT = s("SW_KT", [128, T], BF16)
        self.MIX = s("MIX", [T, D])
        self.U2T = s("U2T", [8, 128, T], BF16)
        self.GATES = s("GATES", [NEXP, T])

    def build(self):
        nc = self.nc
        self.declare_io()
        with ExitStack() as gs:
            self.gs = gs
            self.S = Sched(nc, gs)
            self.consts()
            for l in range(self.depth):
                self.phase1(l)
                if "p1" in self.dbg:
                    break
                self.phase_sw(l)
                self.phase_df(l)
                self.phase_ssd(l)
                self.phase_dn(l)
                if "mix" in self.dbg:
                    break
                self.phase3(l)
            if not self.dbg:
                self.final()
        return nc

    def consts(self):
        gs = self.gs
        NT = self.NT
        sb = lambda n, s, dt=F32: self.sb(gs, n, s, dt)
        self.identf = sb("identf", [128, 128])
        self.identb = sb("identb", [128, 128], BF16)
        self.onesf = sb("onesf", [128, 128])
        self.Umat = sb("Umat", [128, 128])
        self.E127 = sb("E127", [128, 128])
        self.mstrict = sb("mstrict", [128, 128])
        self.mTincl = sb("mTincl", [128, 128])
        self.c01T = sb("c01T", [128, 128], BF16)
        self.p01T = sb("p01T", [128, 128], BF16)
        self.bind4 = sb("bind4", [128, 4])
        self.bind2 = sb("bind2", [128, 2])
        self.BETA = sb("BETA", [128, NT, 4])
        self.G = sb("G", [128, NT, 4])
        self.DT = sb("DT", [128, NT, 4])
        self.BGraw = sb("BGraw", [128, NT, 8])
        self.DTraw = sb("DTraw", [128, NT, 4])
        self.stats = sb("stats", [4, 8])
        self.mbias = sb("mbias", [128, 2])
        m = self.memset
        a = self.asel
        self.epsb = sb("epsb", [128, 1])
        m("pool", self.epsb[:], EPS)
        m("pool", self.identf[:], 1.0)
        a(self.identf[:], self.identf[:], [[-1, 128]], ALU.is_equal, 0.0, 0, 1)
        self.cp("dve", self.identb[:], self.identf[:])
        m("pool", self.onesf[:], 1.0)
        m("pool", self.Umat[:], 1.0)
        a(self.Umat[:], self.Umat[:], [[1, 128]], ALU.is_ge, 0.0, 0, -1)
        self.cp("dve", self.c01T[:], self.Umat[:])
        m("pool", self.E127[:], 1.0)
        a(self.E127[:], self.E127[:], [[0, 128]], ALU.is_equal, 0.0, -127, 1)
        m("pool", self.mstrict[:], 0.0)
        a(self.mstrict[:], self.mstrict[:], [[-1, 128]], ALU.is_gt, BIG, 0, 1)
        m("pool", self.mTincl[:], 0.0)
        a(self.mTincl[:], self.mTincl[:], [[1, 128]], ALU.is_ge, -BIG, 0, -1)
        m("pool", self.p01T[:], 1.0)
        a(self.p01T[:], self.p01T[:], [[-1, 128]], ALU.is_gt, 0.0, 0, 1)
        m("pool", self.bind4[:], 1.0)
        a(self.bind4[:], self.bind4[:], [[-32, 4]], ALU.is_ge, 0.0, 0, 1)
        a(self.bind4[:], self.bind4[:], [[32, 4]], ALU.is_ge, 0.0, 31, -1)
        m("pool", self.bind2[:], 1.0)
        a(self.bind2[:], self.bind2[:], [[-64, 2]], ALU.is_ge, 0.0, 0, 1)
        a(self.bind2[:], self.bind2[:], [[64, 2]], ALU.is_ge, 0.0, 63, -1)

    def groups(self):
        NT = self.NT
        g = []
        n = 0
        while n < NT:
            tg = min(4, NT - n)
            g.append((n, tg))
            n += tg
        return g

    def load_h_group(self, l, hs, n0, TG):
        if l == 0:
            j0 = 0
            if n0 == 0:
                self.memset("pool", hs[:, 0, :], 0.0)
                self.dma("sp", hs[112:128, 0, :], self.meta[:, :])
                j0 = 1
            if TG - j0 > 0:
                xt = self.x.rearrange("(n p) d -> p n d", p=128)
                self.dma("sp", hs[:, j0:TG, :], xt[:, n0 + j0 - 1:n0 + TG - 1, :])
        else:
            ht = self.H.rearrange("(n p) d -> p n d", p=128)
            self.dma("sp", hs[:, 0:TG, :], ht[:, n0:n0 + TG, :])

    def rmsnorm_T(self, st_tiles, hs, TG, normw, n0, uT, tp, zero_pad=True):
        junk, ssq, rstd, ub = st_tiles
        for j in range(TG):
            self.tt("pool", junk[:], hs[:, j, :], hs[:, j, :], ALU.mult)
            self.red("dve", ssq[:, j:j + 1], junk[:], ALU.add)
        self.act(rstd[:, 0:TG], ssq[:, 0:TG], AF.Sqrt, bias=self.epsb[:, 0:1], scale=1.0 / D)
        self.recip(rstd[:, 0:TG], rstd[:, 0:TG])
        for j in range(TG):
            self.stt("dve", ub[:, j, :], hs[:, j, :], rstd[:, j:j + 1], normw[:], ALU.mult, ALU.mult)
        if zero_pad and n0 == 0:
            self.memset("pool", ub[0:112, 0, :], 0.0)
        for j in range(TG):
            for c in range(8):
                self.tr(tp[:, c, :], ub[:, j, c * 128:(c + 1) * 128], self.identb[:])
            self.cp("act", uT[:, :, j * 128:(j + 1) * 128], tp[:, :, :])

    def phase1(self, l):
        NT = self.NT
        with ExitStack() as st:
            sb = lambda n, s, dt=F32: self.sb(st, n, s, dt)
            ps = lambda n, s, dt=F32: self.ps(st, n, s, dt)
            win = sb("win", [128, 8, NCOLS], BF16)
            for c in range(8):
                self.dma("pool", win[:, c, :], self.w_in[l, c * 128:(c + 1) * 128, :])
            wswq = sb("wswq", [128, 8, 256], BF16)
            for i_, h_ in enumerate((0, 2, 1, 3)):
                self.cp("pool", wswq[:, :, i_ * 64:(i_ + 1) * 64], win[:, :, C_SWQ + h_ * 64:C_SWQ + (h_ + 1) * 64])
            normw = sb("normw", [128, D])
            self.dma("sp", normw[:], self.norm_mix[l].partition_broadcast(128))
            dncw = sb("dncw", [128, 6, 4])
            for cc in range(6):
                self.dma("sp", dncw[:, cc, :], self.dn_conv_w[l, :, cc * 128:(cc + 1) * 128].rearrange("j p -> p j"), slow=True)
            sscw = sb("sscw", [128, 6, 4])
            for cc in range(6):
                self.dma("sp", sscw[:, cc, :], self.ssd_conv_w[l, :, cc * 128:(cc + 1) * 128].rearrange("j p -> p j"), slow=True)
            sscb = sb("sscb", [128, 6])
            self.dma("sp", sscb[:], self.ssd_conv_b[l].rearrange("(c p) -> p c", p=128), slow=True)
            hp = sb("hp", [128, 16])
            self.dma("sp", hp[:, 0:4], self.dn_a_log[l].partition_broadcast(128))
            self.dma("sp", hp[:, 4:8], self.dn_dt_bias[l].partition_broadcast(128))
            self.dma("sp", hp[:, 8:12], self.ssd_dt_bias[l].partition_broadcast(128))

            hs = sb("hs", [128, 4, D])
            junk = sb("junk", [128, D])
            ssq = sb("ssq", [128, 4])
            rstd = sb("rstd", [128, 4])
            ub = sb("ub", [128, 4, D], BF16)
            uT = sb("uT", [128, 8, 512], BF16)
            xin_dn = sb("xin_dn", [128, 6, 515])
            xin_ss = sb("xin_ss", [128, 6, 515])
            cacc = sb("cacc", [128, 512])
            cs = [sb("cs0", [128, 512]), sb("cs1", [128, 512])]
            dnst = sb("dnst", [128, 4, 1024])
            ssdst = sb("ssdst", [128, 4, 768])
            tok2st = sb("tok2st", [128, 4, 384])
            fst = [sb("fst0", [128, 512], BF16), sb("fst1", [128, 512], BF16)]
            sqf = sb("sqf", [128, 512])
            sqt = sb("sqt", [128, 512])
            ssn = sb("ssn", [128, 8])
            stmp = sb("stmp", [4, 1])

            tp = ps("tp", [128, 8, 128], BF16)
            pfm = [ps("pfm0", [128, 512]), ps("pfm1", [128, 512])]
            pA = ps("pA", [128, 512])
            pB = ps("pB", [128, 512])
            pC = ps("pC", [128, 512])
            t32 = ps("t32", [128, 4, 128])
            pstat = ps("pstat", [4, 512])

            self.memset("pool", xin_dn[:], 0.0)
            self.memset("pool", xin_ss[:], 0.0)
            self.memset("pool", self.stats[:], 0.0)
            fmi = [0]
            csi = [0]
            fsi = [0]

            for (n0, TG) in self.groups():
                TW = TG * 128
                self.load_h_group(l, hs, n0, TG)
                self.rmsnorm_T((junk, ssq, rstd, ub), hs, TG, normw, n0, uT, tp)

                def fm(lhs_fn):
                    pf = pfm[fmi[0] % 2]
                    fmi[0] += 1
                    for c in range(8):
                        self.mm(pf[:, 0:TW], lhs_fn(c), uT[:, c, 0:TW], start=(c == 0), stop=(c == 7))
                    return pf

                def conv_chunk(pf, xin, cc, cw, bias=None):
                    self.cp("act", xin[:, cc, 3:3 + TW], pf[:, 0:TW])
                    self.ts("dve", cacc[:, 0:TW], xin[:, cc, 0:TW], cw[:, cc, 0:1], ALU.mult)
                    for j in range(1, 4):
                        self.stt("dve", cacc[:, 0:TW], xin[:, cc, j:j + TW], cw[:, cc, j:j + 1], cacc[:, 0:TW], ALU.mult, ALU.add)
                    o = cs[csi[0] % 2]
                    csi[0] += 1
                    if bias is None:
                        self.act(o[:, 0:TW], cacc[:, 0:TW], AF.Silu)
                    else:
                        self.act(o[:, 0:TW], cacc[:, 0:TW], AF.Silu, bias=bias)
                    self.cp("pool", xin[:, cc, 0:3], xin[:, cc, TW:TW + 3])
                    return o

                def to_tok(o, dst, col0):
                    for j in range(TG):
                        self.tr(t32[:, j, :], o[:, j * 128:(j + 1) * 128], self.identf[:])
                    self.cp("dve", dst[:, 0:TG, col0:col0 + 128], t32[:, 0:TG, :])

                for cc in range(6):
                    pf = fm(lambda c, cc=cc: win[:, c, C_DNQ + cc * 128:C_DNQ + (cc + 1) * 128])
                    o = conv_chunk(pf, xin_dn, cc, dncw)
                    to_tok(o, dnst, cc * 128)
                for cc in range(6):
                    pf = fm(lambda c, cc=cc: win[:, c, C_SSX + cc * 128:C_SSX + (cc + 1) * 128])
                    o = conv_chunk(pf, xin_ss, cc, sscw, bias=sscb[:, cc:cc + 1])
                    if cc < 4:
                        to_tok(o, ssdst, cc * 128)
                    if cc >= 2:
                        self.dma("sp", self.SSD_FT[cc - 2, :, n0 * 128:n0 * 128 + TW], o[:, 0:TW])
                def qk_chunk(lhs_fn, scale, dst_ap, stat_col, bind, nb):
                    pf = fm(lhs_fn)
                    f = fst[fsi[0] % 2]
                    fsi[0] += 1
                    self.act(f[:, 0:TW], pf[:, 0:TW], AF.Identity, scale=scale)
                    self.act(sqf[:, 0:TW], pf[:, 0:TW], AF.Square, scale=scale)
                    self.mm(pstat[0:nb, 0:TW], bind[:, 0:nb], sqf[:, 0:TW])
                    self.red("dve", stmp[0:nb, 0:1], pstat[0:nb, 0:TW], ALU.max)
                    self.tt("dve", self.stats[0:nb, stat_col:stat_col + 1], self.stats[0:nb, stat_col:stat_col + 1], stmp[0:nb, 0:1], ALU.max)
                    self.dma("sp", dst_ap, f[:, 0:TW])

                sl = slice(n0 * 128, n0 * 128 + TW)
                for c2 in range(2):
                    qk_chunk(lambda c, c2=c2: win[:, c, C_DFQ + c2 * 128:C_DFQ + (c2 + 1) * 128], 32 ** -0.5, self.DF_QT[c2, :, sl], 0, self.bind4, 4)
                    qk_chunk(lambda c, c2=c2: win[:, c, C_DFK + c2 * 128:C_DFK + (c2 + 1) * 128], 1.0, self.DF_KT[c2, :, sl], 1, self.bind4, 4)
                for c2 in range(2):
                    qk_chunk(lambda c, c2=c2: wswq[:, c, c2 * 128:(c2 + 1) * 128], 64 ** -0.5, self.SW_QT[c2, :, sl], 2, self.bind2, 2)
                qk_chunk(lambda c: win[:, c, C_SWK:C_SWK + 128], 1.0, self.SW_KT[:, sl], 3, self.bind2, 2)

                for j in range(TG):
                    n = n0 + j
                    lt = lambda c: uT[:, c, j * 128:(j + 1) * 128]
                    for c in range(8):
                        self.mm(pA[:, 0:264], lt(c), win[:, c, C_DNZ:C_DNZ + 264], start=(c == 0), stop=(c == 7))
                    for c in range(8):
                        self.mm(pA[:, 264:268], lt(c), win[:, c, C_SSDT:C_SSDT + 4], start=(c == 0), stop=(c == 7))
                    for c in range(8):
                        self.mm(pB[:, 0:256], lt(c), win[:, c, C_DFV:C_DFV + 256], start=(c == 0), stop=(c == 7))
                    for c in range(8):
                        self.mm(pC[:, 0:384], lt(c), win[:, c, C_SWV:C_SWV + 384], start=(c == 0), stop=(c == 7))
                    self.cp("act", dnst[:, j, 768:1024], pA[:, 0:256])
                    self.cp("dve", self.BGraw[:, n, :], pA[:, 256:264])
                    self.cp("dve", self.DTraw[:, n, :], pA[:, 264:268])
                    self.cp("act", tok2st[:, j, 0:256], pB[:, 0:256])
                    self.cp("dve", tok2st[:, j, 256:384], pC[:, 0:128])
                    self.cp("act", ssdst[:, j, 512:768], pC[:, 128:384])
                for j in range(TG):
                    qk = dnst[:, j, 0:512].rearrange("p (h d) -> p h d", d=64)
                    self.tt("pool", sqt[:], dnst[:, j, 0:512], dnst[:, j, 0:512], ALU.mult)
                    self.red("dve", ssn[:], sqt[:].rearrange("p (h d) -> p h d", d=64), ALU.add)
                    self.act(ssn[:], ssn[:], AF.Sqrt, bias=self.epsb[:, 0:1])
                    self.recip(ssn[:], ssn[:])
                    self.ts("dve", ssn[:, 0:4], ssn[:, 0:4], 0.125, ALU.mult)
                    self.tt("dve", qk, qk, ssn[:].unsqueeze(2).to_broadcast([128, 8, 64]), ALU.mult)
                tsl = slice(n0, n0 + TG)
                self.dma("sp", self.DN_TOK.rearrange("(n p) c -> p n c", p=128)[:, tsl, :], dnst[:, 0:TG, :])
                self.dma("sp", self.SSD_TOK.rearrange("(n p) c -> p n c", p=128)[:, tsl, :], ssdst[:, 0:TG, :])
                self.dma("sp", self.TOK2.rearrange("(n p) c -> p n c", p=128)[:, tsl, :], tok2st[:, 0:TG, :])

            tmp = sb("gtmp", [128, NT, 4])
            tmp2 = sb("gtmp2", [128, NT, 4])
            negA = sb("negA", [128, 4])
            bc = lambda ap: ap.unsqueeze(1).to_broadcast([128, NT, 4])
            self.act(self.BETA[:], self.BGraw[:, :, 0:4], AF.Sigmoid)

            def softplus(dst, src_raw, bias_ap):
                self.tt("dve", tmp[:], src_raw, bc(bias_ap), ALU.add)
                self.act(tmp2[:], tmp[:], AF.Abs)
                self.act(tmp2[:], tmp2[:], AF.Exp, scale=-1.0)
                self.act(tmp2[:], tmp2[:], AF.Ln, bias=1.0)
                self.stt("dve", dst, tmp[:], 0.0, tmp2[:], ALU.max, ALU.add)

            softplus(self.G[:], self.BGraw[:, :, 4:8], hp[:, 4:8])
            self.act(negA[:], hp[:, 0:4], AF.Exp)
            self.ts("dve", negA[:], negA[:], -1.0, ALU.mult)
            self.tt("dve", self.G[:], self.G[:], bc(negA[:]), ALU.mult)
            softplus(self.DT[:], self.DTraw[:], hp[:, 8:12])
            self.memset("pool", self.G[0:112, 0, :], 0.0)
            self.memset("pool", self.BETA[0:112, 0, :], 0.0)
            self.memset("pool", self.DT[0:112, 0, :], 0.0)

            s4 = sb("s4", [4, 4])
            pt1 = pstat
            r14 = sb("r14", [1, 4])
            m11 = sb("m11", [1, 2])
            pbc = pA
            for i in range(4):
                self.tr(pt1[0:1, 0:4], self.stats[0:4, i:i + 1], self.identf[0:4, 0:4])
                self.red("dve", r14[0:1, i:i + 1], pt1[0:1, 0:4], ALU.max)
            self.tt("dve", m11[0:1, 0:1], r14[0:1, 0:1], r14[0:1, 1:2], ALU.mult)
            self.tt("dve", m11[0:1, 1:2], r14[0:1, 2:3], r14[0:1, 3:4], ALU.mult)
            self.act(m11[:], m11[:], AF.Sqrt)
            self.ts("dve", m11[:], m11[:], -1.0, ALU.mult)
            self.mm(pbc[:, 0:2], self.onesf[0:1, :], m11[0:1, 0:2])
            self.cp("dve", self.mbias[:], pbc[:, 0:2])
            self.flush()

    def phase_sw(self, l):
        NT, T = self.NT, self.T
        with ExitStack() as st:
            sb = lambda n, s, dt=F32: self.sb(st, n, s, dt)
            ps = lambda n, s, dt=F32: self.ps(st, n, s, dt)
            QT = sb("swQT", [128, 2, T], BF16)
            KT = sb("swKT", [128, T], BF16)
            for c in range(2):
                self.dma("sp", QT[:, c, :], self.SW_QT[c, :, :])
            self.dma("sp", KT[:], self.SW_KT[:, :])
            QTp = sb("swQTp", [128, 4, T], BF16)
            self.memset("pool", QTp[:], 0.0)
            for h in range(4):
                kv = h // 2
                self.cp("pool", QTp[64 * kv:64 * kv + 64, h, :], QT[64 * kv:64 * kv + 64, h % 2, :])
            Vst = sb("swVst", [128, NT, 128])
            self.dma("sp", Vst[:], self.TOK2.rearrange("(n p) c -> p n c", p=128)[:, :, 256:384])
            Vaug = sb("swVaug", [128, NT, 2, 66], BF16)
            self.cp("dve", Vaug[:, :, :, 0:64], Vst[:].rearrange("p n (g d) -> p n g d", d=64))
            self.memset("pool", Vaug[:, :, :, 64:66], 0.0)
            self.memset("pool", Vaug[:, :, :, 64:65], 1.0)
            self.memset("pool", Vaug[0:112, 0, :, :], 0.0)
            sk = sb("swsk", [128, 4])
            self.dma("sp", sk[:], self.sw_sinks[l].partition_broadcast(128))
            esink = sb("esink", [128, 4])
            self.act(esink[:], sk[:], AF.Exp, bias=self.mbias[:, 1:2])
            PTp = sb("PTp", [128, 4, 128], BF16)
            PTo = sb("PTo", [128, 4, 128], BF16)
            zt = sb("swz", [128, 4])
            ysw = [sb("ysw0", [128, 4, 64]), sb("ysw1", [128, 4, 64])]
            pSp = ps("pSp", [128, 4, 128])
            pSo = ps("pSo", [128, 4, 128])
            pO = [ps("pO0", [128, 512])[:, 0:264].rearrange("p (h d) -> p h d", d=66), ps("pO1", [128, 512])[:, 0:264].rearrange("p (h d) -> p h d", d=66)]
            mixt = self.MIX.rearrange("(n p) c -> p n c", p=128)
            for i in range(NT):
                qs = slice(i * 128, (i + 1) * 128)
                blks = ([(i - 1, pSp, PTp, self.p01T)] if i > 0 else []) + [(i, pSo, PTo, self.c01T)]
                for (j, pS, PT, msk) in blks:
                    ks = slice(j * 128, (j + 1) * 128)
                    for h in range(4):
                        self.mm(pS[:, h, :], KT[:, ks], QTp[:, h, qs])
                    self.act(PT[:], pS[:], AF.Exp, bias=self.mbias[:, 1:2])
                    self.tt("pool", PT[:], PT[:], msk[:].unsqueeze(1).to_broadcast([128, 4, 128]), ALU.mult)
                po = pO[i % 2]
                for h in range(4):
                    kv = h // 2
                    for bi, (j, pS, PT, msk) in enumerate(blks):
                        self.mm(po[:, h, :], PT[:, h, :], Vaug[:, j, kv, :], start=(bi == 0), stop=(bi == len(blks) - 1))
                self.tt("dve", zt[:], po[:, :, 64], esink[:], ALU.add)
                self.recip(zt[:], zt[:])
                y = ysw[i % 2]
                self.tt("dve", y[:], po[:, :, 0:64], zt[:].unsqueeze(2).to_broadcast([128, 4, 64]), ALU.mult)
                self.dma("sp", mixt[:, i, 512:768], y[:].rearrange("p h d -> p (h d)"))
            self.flush()

    def phase_df(self, l):
        NT, T = self.NT, self.T
        import math
        lam_init = 0.8 - 0.6 * math.exp(-0.3 * l)
        with ExitStack() as st:
            sb = lambda n, s, dt=F32: self.sb(st, n, s, dt)
            ps = lambda n, s, dt=F32: self.ps(st, n, s, dt)
            QT = sb("dfQT", [128, 2, T], BF16)
            KT = sb("dfKT", [128, 2, T], BF16)
            for c in range(2):
                self.dma("sp", QT[:, c, :], self.DF_QT[c, :, :])
                self.dma("sp", KT[:, c, :], self.DF_KT[c, :, :])
            QTp = sb("dfQTp", [128, 8, T], BF16)
            self.memset("pool", QTp[:], 0.0)
            for hm in range(8):
                pb = 32 * (hm % 4)
                self.cp("pool" if hm % 2 else "dve", QTp[pb:pb + 32, hm, :], QT[pb:pb + 32, hm // 4, :])
            Vst = sb("dfVst", [128, NT, 256])
            self.dma("sp", Vst[:], self.TOK2.rearrange("(n p) c -> p n c", p=128)[:, :, 0:256])
            Vaug = sb("dfVaug", [128, NT, 4, 66], BF16)
            self.cp("dve", Vaug[:, :, :, 0:64], Vst[:].rearrange("p n (g d) -> p n g d", d=64))
            self.memset("pool", Vaug[:, :, :, 64:66], 0.0)
            self.memset("pool", Vaug[:, :, :, 64:65], 1.0)
            self.memset("pool", Vaug[0:112, 0, :, :], 0.0)
            lt = sb("dflam", [128, 4, 32])
            self.dma("sp", lt[:].rearrange("p a b -> p (a b)"), self.df_lambda[l].rearrange("a b -> (a b)").partition_broadcast(128))
            lp = sb("dflp", [128, 2, 32])
            l2 = sb("dfl2", [128, 2])
            neglam = sb("neglam", [128, 1])
            self.tt("dve", lp[:, 0, :], lt[:, 0, :], lt[:, 1, :], ALU.mult)
            self.tt("dve", lp[:, 1, :], lt[:, 2, :], lt[:, 3, :], ALU.mult)
            self.red("dve", l2[:], lp[:], ALU.add)
            self.act(l2[:], l2[:], AF.Exp)
            self.tt("dve", neglam[:], l2[:, 1:2], l2[:, 0:1], ALU.subtract)
            self.ts("dve", neglam[:], neglam[:], -lam_init, ALU.add)
            nw = sb("dfnw", [128, 64])
            self.dma("sp", nw[:], self.df_norm_w[l].partition_broadcast(128))
            self.ts("dve", nw[:], nw[:], 1.0 - lam_init, ALU.mult)

            PTs = [sb(f"dfPT{k}", [128, 512], BF16) for k in range(4)]
            pSs = [ps(f"dfpS{k}", [128, 512]) for k in range(4)]
            pO = [[ps(f"dfpO{m}{k}", [128, 512])[:, 0:264].rearrange("p (h d) -> p h d", d=66) for k in range(2)] for m in range(2)]
            r0 = sb("dfr0", [128, 4])
            r1 = sb("dfr1", [128, 4])
            t0 = sb("dft0", [128, 4, 64])
            t1 = sb("dft1", [128, 4, 64])
            sq = sb("dfsq", [128, 4, 64])
            ss = sb("dfss", [128, 4])
            ydf = [sb("ydf0", [128, 4, 64]), sb("ydf1", [128, 4, 64])]
            mixt = self.MIX.rearrange("(n p) c -> p n c", p=128)
            it = 0
            LA = 3
            gstep = [0]
            for (b0, TG) in self.groups():
                for h in range(4):
                    po = [pO[0][it % 2], pO[1][it % 2]]
                    first = [True, True]
                    steps = [(j, m) for j in range(0, b0 + TG) for m in range(2)]
                    bufof = {}

                    def score(i):
                        j, m = steps[i]
                        k = gstep[0] % 4
                        gstep[0] += 1
                        bufof[i] = k
                        qlo = max(j, b0)
                        ncol = (b0 + TG - qlo) * 128
                        qcols = slice(qlo * 128, (b0 + TG) * 128)
                        ks = slice(j * 128, (j + 1) * 128)
                        hm = 2 * h + m
                        c = hm // 4
                        pst, ptt = pSs[k], PTs[k]
                        self.mm(pst[:, 0:ncol], KT[:, c, ks], QTp[:, hm, qcols])
                        self.act(ptt[:, 0:ncol], pst[:, 0:ncol], AF.Exp, bias=self.mbias[:, 0:1])
                        if j >= b0:
                            self.tt("pool", ptt[:, 0:128], ptt[:, 0:128], self.c01T[:], ALU.mult)

                    def pv(i):
                        j, m = steps[i]
                        ptt = PTs[bufof[i]]
                        qlo = max(j, b0)
                        for bi in range(qlo, b0 + TG):
                            col = (bi - qlo) * 128
                            self.mm(po[m][:, bi - b0, :], ptt[:, col:col + 128], Vaug[:, j, h, :], start=first[m], stop=False, acc=True)
                            first[m] = False

                    ns = len(steps)
                    for i in range(ns + LA):
                        if i < ns:
                            score(i)
                        if i - LA >= 0:
                            pv(i - LA)
                    if b0 == 0:
                        self.ts("dve", r0[:, 0:TG], po[0][:, 0:TG, 64], 1e-30, ALU.max)
                        self.ts("dve", r1[:, 0:TG], po[1][:, 0:TG, 64], 1e-30, ALU.max)
                        self.recip(r0[:, 0:TG], r0[:, 0:TG])
                        self.recip(r1[:, 0:TG], r1[:, 0:TG])
                    else:
                        self.recip(r0[:, 0:TG], po[0][:, 0:TG, 64])
                        self.recip(r1[:, 0:TG], po[1][:, 0:TG, 64])
                    self.ts("dve", r1[:, 0:TG], r1[:, 0:TG], neglam[:, 0:1], ALU.mult)
                    self.tt("dve", t0[:, 0:TG, :], po[0][:, 0:TG, 0:64], r0[:, 0:TG].unsqueeze(2).to_broadcast([128, TG, 64]), ALU.mult)
                    self.tt("dve", t1[:, 0:TG, :], po[1][:, 0:TG, 0:64], r1[:, 0:TG].unsqueeze(2).to_broadcast([128, TG, 64]), ALU.mult)
                    self.tt("pool", t0[:, 0:TG, :], t0[:, 0:TG, :], t1[:, 0:TG, :], ALU.add)
                    self.tt("pool", sq[:, 0:TG, :], t0[:, 0:TG, :], t0[:, 0:TG, :], ALU.mult)
                    self.red("dve", ss[:, 0:TG], sq[:, 0:TG, :], ALU.add)
                    self.act(ss[:, 0:TG], ss[:, 0:TG], AF.Sqrt, bias=self.epsb[:, 0:1], scale=1.0 / 64)
                    self.recip(ss[:, 0:TG], ss[:, 0:TG])
                    y = ydf[it % 2]
                    self.tt("dve", y[:, 0:TG, :], t0[:, 0:TG, :], ss[:, 0:TG].unsqueeze(2).to_broadcast([128, TG, 64]), ALU.mult)
                    self.tt("pool", y[:, 0:TG, :], y[:, 0:TG, :], nw[:].unsqueeze(1).to_broadcast([128, TG, 64]), ALU.mult)
                    self.dma("sp", mixt[:, b0:b0 + TG, 256 + h * 64:256 + (h + 1) * 64], y[:, 0:TG, :])
                    it += 1
            self.flush()

    def chunk_cumsum(self, st, src, name):
        NT = self.NT
        cum = self.sb(st, name + "cum", [128, NT, 4])
        last = self.sb(st, name + "last", [128, NT, 4])
        pc = self.ps(st, name + "pc", [128, 512])
        n4 = NT * 4
        self.mm(pc[:, 0:n4], self.Umat[:], src[:].rearrange("p n h -> p (n h)"))
        self.cp("dve", cum[:].rearrange("p n h -> p (n h)"), pc[:, 0:n4])
        self.mm(pc[:, 0:n4], self.E127[:], cum[:].rearrange("p n h -> p (n h)"))
        self.cp("dve", last[:].rearrange("p n h -> p (n h)"), pc[:, 0:n4])
        return cum, last, pc

    def phase_ssd(self, l):
        NT, T = self.NT, self.T
        with ExitStack() as st:
            sb = lambda n, s, dt=F32: self.sb(st, n, s, dt)
            ps = lambda n, s, dt=F32: self.ps(st, n, s, dt)
            hp = sb("sshp", [128, 8])
            self.dma("sp", hp[:, 0:4], self.ssd_a_log[l].partition_broadcast(128))
            self.dma("sp", hp[:, 4:8], self.ssd_d[l].partition_broadcast(128))
            nw = sb("ssnw", [128, 256])
            self.dma("sp", nw[:], self.ssd_norm_w[l].partition_broadcast(128))
            negA = sb("ssnegA", [128, 4])
            self.act(negA[:], hp[:, 0:4], AF.Exp)
            self.ts("dve", negA[:], negA[:], -1.0, ALU.mult)
            DA = sb("ssDA", [128, NT, 4])
            self.tt("dve", DA[:], self.DT[:], negA[:].unsqueeze(1).to_broadcast([128, NT, 4]), ALU.mult)
            ACUM, AL, pc = self.chunk_cumsum(st, DA, "ss")
            EAC = sb("ssEAC", [128, NT, 4])
            EDEC = sb("ssEDEC", [128, NT, 4])
            ECH = sb("ssECH", [128, NT, 4])
            self.act(EAC[:], ACUM[:], AF.Exp)
            self.tt("dve", EDEC[:], AL[:], ACUM[:], ALU.subtract)
            self.act(EDEC[:], EDEC[:], AF.Exp)
            self.act(ECH[:], AL[:], AF.Exp)
            hT = sb("sshT", [128, 4, 64])
            self.memset("pool", hT[:], 0.0)
            tok = sb("sstok", [128, 768])
            ft = sb("ssft", [128, 4, 128])
            diag = sb("ssdiag", [128, 4, 128])
            X = sb("ssX", [128, 4, 128])
            lmT = sb("sslmT", [128, 4, 128])
            WT = sb("ssWT", [128, 4, 128])
            xc = sb("ssxc", [128, 4, 64])
            xdec = sb("ssxdec", [128, 4, 64])
            t1 = sb("sst1", [128, 4, 64])
            y = sb("ssy", [128, 256])
            sz = sb("sssz", [128, 256])
            sq = sb("sssq", [128, 256])
            ss = sb("ssss", [128, 2])
            pBr = ps("sspBr", [128, 4, 128])
            pBC = ps("sspBC", [128, 4, 128])
            pY = ps("sspY", [128, 4, 128])
            pSt = ps("sspSt", [128, 4, 128])
            mixt = self.MIX.rearrange("(n p) c -> p n c", p=128)
            stok = self.SSD_TOK.rearrange("(n p) c -> p n c", p=128)
            bc64 = lambda ap: ap.unsqueeze(2).to_broadcast([128, 4, 64])
            bc128 = lambda ap: ap.unsqueeze(2).to_broadcast([128, 4, 128])
            for n in range(NT):
                self.dma("sp", tok[:], stok[:, n, :])
                self.dma("sp", ft[:], self.SSD_FT[:, :, n * 128:(n + 1) * 128].rearrange("c p t -> p c t"))
                x3 = tok[:, 0:256].rearrange("p (h d) -> p h d", d=64)
                self.tt("pool", diag[:], self.identf[:].unsqueeze(1).to_broadcast([128, 4, 128]), bc128(ACUM[:, n, :]), ALU.mult)
                for h in range(4):
                    self.mm(pBr[:, h, :], self.onesf[:], diag[:, h, :])
                self.tt("dve", X[:], pBr[:], bc128(ACUM[:, n, :]), ALU.subtract)
                self.tt("dve", X[:], X[:], self.mTincl[:].unsqueeze(1).to_broadcast([128, 4, 128]), ALU.min)
                self.act(lmT[:], X[:], AF.Exp)
                for g in range(2):
                    self.mm(pBC[:, g, :], ft[:, g, :], ft[:, 2 + g, :])
                for g in range(2):
                    self.tt("dve", WT[:, 2 * g:2 * g + 2, :], pBC[:, g, :].unsqueeze(1).to_broadcast([128, 2, 128]), lmT[:, 2 * g:2 * g + 2, :], ALU.mult)
                self.tt("pool", xc[:], x3, bc64(self.DT[:, n, :]), ALU.mult)
                for h in range(4):
                    self.mm(pY[:, h, 0:64], WT[:, h, :], xc[:, h, :])
                for h in range(4):
                    self.mm(pY[:, h, 64:128], ft[:, 2 + h // 2, :], hT[:, h, :])
                self.tt("dve", t1[:], pY[:, :, 64:128], bc64(EAC[:, n, :]), ALU.mult)
                self.tt("dve", t1[:], t1[:], pY[:, :, 0:64], ALU.add)
                y3 = y[:].rearrange("p (h d) -> p h d", d=64)
                self.tt("pool", y3, x3, bc64(hp[:, 4:8]), ALU.mult)
                self.tt("pool", y3, y3, t1[:], ALU.add)
                self.tt("pool", xdec[:], xc[:], bc64(EDEC[:, n, :]), ALU.mult)
                for h in range(4):
                    g = h // 2
                    self.mm(pSt[:, h, 0:64], tok[:, 256 + 128 * g:256 + 128 * (g + 1)], xdec[:, h, :])
                self.tt("pool", hT[:], hT[:], bc64(ECH[:, n, :]), ALU.mult)
                self.tt("dve", hT[:], hT[:], pSt[:, :, 0:64], ALU.add)
                self.act(sz[:], tok[:, 512:768], AF.Silu)
                self.tt("dve", y[:], y[:], sz[:], ALU.mult)
                self.tt("pool", sq[:], y[:], y[:], ALU.mult)
                self.red("dve", ss[:], sq[:].rearrange("p (g d) -> p g d", d=128), ALU.add)
                self.act(ss[:], ss[:], AF.Sqrt, bias=self.epsb[:, 0:1], scale=1.0 / 128)
                self.recip(ss[:], ss[:])
                yg = y[:].rearrange("p (g d) -> p g d", d=128)
                self.tt("dve", yg, yg, ss[:].unsqueeze(2).to_broadcast([128, 2, 128]), ALU.mult)
                self.tt("pool", y[:], y[:], nw[:], ALU.mult)
                self.dma("sp", mixt[:, n, 768:1024], y[:])
            self.flush()

    def phase_dn(self, l):
        NT, T = self.NT, self.T
        with ExitStack() as st:
            sb = lambda n, s, dt=F32: self.sb(st, n, s, dt)
            ps = lambda n, s, dt=F32: self.ps(st, n, s, dt)
            nw = sb("dnnw", [128, 64])
            self.dma("sp", nw[:], self.dn_norm_w[l].partition_broadcast(128))
            GC, GL, pc = self.chunk_cumsum(st, self.G, "dn")
            EGC = sb("dnEGC", [128, NT, 4])
            EKD = sb("dnEKD", [128, NT, 4])
            EGL = sb("dnEGL", [128, NT, 4])
            BGE = sb("dnBGE", [128, NT, 4])
            NEGB = sb("dnNEGB", [128, NT, 4])
            EGLS = sb("dnEGLS", [128, NT, 2])
            self.act(EGC[:], GC[:], AF.Exp)
            self.tt("dve", EKD[:], GL[:], GC[:], ALU.subtract)
            self.act(EKD[:], EKD[:], AF.Exp)
            self.act(EGL[:], GL[:], AF.Exp)
            self.tt("dve", BGE[:], self.BETA[:], EGC[:], ALU.mult)
            self.ts("dve", NEGB[:], self.BETA[:], -1.0, ALU.mult)
            self.cp("dve", EGLS[0:64, :, :], EGL[0:64, :, 0:4:2])
            self.cp("dve", EGLS[64:128, :, :], EGL[64:128, :, 1:4:2])
            Sst = sb("dnS", [128, 2, 64])
            self.memset("pool", Sst[:], 0.0)
            tok = sb("dntok", [128, 1024])
            qkT = sb("dnqkT", [128, 4, 128])
            qpad = sb("dnqpad", [128, 2, 2, 128])
            kpad = sb("dnkpad", [128, 2, 2, 128])
            self.memset("pool", qpad[:], 0.0)
            self.memset("pool", kpad[:], 0.0)
            diag = sb("dndiag", [128, 4, 128])
            X = sb("dnX", [128, 4, 128])
            dS = sb("dndS", [128, 4, 128])
            dT = sb("dndT", [128, 4, 128])
            attnT = sb("dnattnT", [128, 4, 128])
            Pb = [sb("dnP0", [128, 4, 128]), sb("dnP1", [128, 4, 128])]
            Qb = [sb("dnQ0", [128, 4, 128]), sb("dnQ1", [128, 4, 128])]
            W = sb("dnW", [128, 4, 128])
            vb = sb("dnvb", [128, 4, 64])
            kdec = sb("dnkdec", [128, 4, 64])
            Rt = sb("dnR", [128, 4, 64])
            vnew = sb("dnvnew", [128, 4, 64])
            o = sb("dno", [128, 4, 64])
            sq = sb("dnsq", [128, 4, 64])
            ss = sb("dnss", [128, 4])
            sz = sb("dnsz", [128, 256])
            B = [ps(f"dnB{i}", [128, 4, 128]) for i in range(6)]
            mixt = self.MIX.rearrange("(n p) c -> p n c", p=128)
            dtok = self.DN_TOK.rearrange("(n p) c -> p n c", p=128)
            bc64 = lambda ap: ap.unsqueeze(2).to_broadcast([128, 4, 64])
            bc128 = lambda ap: ap.unsqueeze(2).to_broadcast([128, 4, 128])
            mb = lambda m_: m_[:].unsqueeze(1).to_broadcast([128, 4, 128])
            hsl = lambda h: slice(64 * (h % 2), 64 * (h % 2) + 64)
            for n in range(NT):
                self.dma("sp", tok[:], dtok[:, n, :])
                q3 = tok[:, 0:256].rearrange("p (h d) -> p h d", d=64)
                k3 = tok[:, 256:512].rearrange("p (h d) -> p h d", d=64)
                v3 = tok[:, 512:768].rearrange("p (h d) -> p h d", d=64)
                pT = B[0]
                for i in range(4):
                    self.tr(pT[:, i, :], tok[:, i * 128:(i + 1) * 128], self.identf[:])
                self.cp("act", qkT[:], pT[:])
                self.cp("act", qpad[0:64, 0, :, :], pT[0:64, 0:2, :])
                self.cp("dve", qpad[64:128, 1, :, :], pT[64:128, 0:2, :])
                self.cp("act", kpad[0:64, 0, :, :], pT[0:64, 2:4, :])
                self.cp("dve", kpad[64:128, 1, :, :], pT[64:128, 2:4, :])
                qT = lambda h: qpad[:, h % 2, h // 2, :]
                kT = lambda h: kpad[:, h % 2, h // 2, :]
                pKK, pKQ, pBr = B[1], B[2], B[3]
                for h in range(4):
                    self.mm(pKK[:, h, :], kT(h), qkT[:, 2 + h // 2, :])
                for h in range(4):
                    self.mm(pKQ[:, h, :], kT(h), qkT[:, h // 2, :])
                self.tt("pool", diag[:], self.identf[:].unsqueeze(1).to_broadcast([128, 4, 128]), bc128(GC[:, n, :]), ALU.mult)
                for h in range(4):
                    self.mm(pBr[:, h, :], self.onesf[:], diag[:, h, :])
                self.tt("dve", X[:], pBr[:], bc128(GC[:, n, :]), ALU.subtract)
                self.tt("dve", dS[:], X[:], mb(self.mstrict), ALU.max)
                self.act(dS[:], dS[:], AF.Exp, scale=-1.0)
                self.tt("dve", dT[:], X[:], mb(self.mTincl), ALU.min)
                self.act(dT[:], dT[:], AF.Exp)
                P, Q = Pb[0], Qb[0]
                self.tt("dve", P[:], pKK[:], dS[:], ALU.mult)
                self.tt("pool", P[:], P[:], bc128(NEGB[:, n, :]), ALU.mult)
                self.tt("dve", attnT[:], pKQ[:], dT[:], ALU.mult)
                pQ = B[0]
                for h in range(4):
                    self.tr(pQ[:, h, :], P[:, h, :], self.identf[:])
                self.cp("act", Q[:], pQ[:])
                self.tt("pool", W[:], Q[:], self.identf[:].unsqueeze(1).to_broadcast([128, 4, 128]), ALU.add)
                pP, pW = B[1], B[2]
                for k in range(6):
                    P, Q = Pb[k % 2], Qb[k % 2]
                    Pn, Qn = Pb[(k + 1) % 2], Qb[(k + 1) % 2]
                    for h in range(4):
                        self.mm(pP[:, h, :], Q[:, h, :], P[:, h, :])
                    if k < 5:
                        for h in range(4):
                            self.mm(pQ[:, h, :], P[:, h, :], Q[:, h, :])
                    self.cp("act", Pn[:], pP[:])
                    if k < 5:
                        self.cp("dve", Qn[:], pQ[:])
                    for h in range(4):
                        self.mm(pW[:, h, :], Pn[:, h, :], W[:, h, :])
                    self.tt("dve", W[:], W[:], pW[:], ALU.add)
                self.tt("pool", vb[:], v3, bc64(self.BETA[:, n, :]), ALU.mult)
                self.tt("pool", kdec[:], k3, bc64(EKD[:, n, :]), ALU.mult)
                pA, pV, pSU = B[3], B[4], B[5]
                for h in range(4):
                    self.mm(pA[:, h, 0:64], kT(h), Sst[:, h // 2, :])
                for h in range(4):
                    self.mm(pA[:, h, 64:128], qT(h), Sst[:, h // 2, :])
                self.tt("dve", Rt[:], pA[:, :, 0:64], bc64(BGE[:, n, :]), ALU.mult)
                self.tt("dve", Rt[:], vb[:], Rt[:], ALU.subtract)
                for h in range(4):
                    self.mm(pV[:, h, 0:64], W[:, h, :], Rt[:, h, :])
                self.cp("act", vnew[:], pV[:, :, 0:64])
                for h in range(4):
                    self.mm(pV[:, h, 64:128], attnT[:, h, :], vnew[:, h, :])
                for pr_ in range(2):
                    self.mm(pSU[:, pr_, :], kdec[:, 2 * pr_:2 * pr_ + 2, :].rearrange("p h d -> p (h d)"),
                            vnew[:, 2 * pr_:2 * pr_ + 2, :].rearrange("p h d -> p (h d)"))
                self.tt("dve", o[:], pA[:, :, 64:128], bc64(EGC[:, n, :]), ALU.mult)
                self.tt("dve", o[:], o[:], pV[:, :, 64:128], ALU.add)
                self.tt("pool", Sst[:], Sst[:], EGLS[:, n, :].unsqueeze(2).to_broadcast([128, 2, 64]), ALU.mult)
                self.tt("dve", Sst[0:64, :, :], Sst[0:64, :, :], pSU[0:64, 0:2, 0:64], ALU.add)
                self.tt("dve", Sst[64:128, :, :], Sst[64:128, :, :], pSU[64:128, 0:2, 64:128], ALU.add)
                self.tt("pool", sq[:], o[:], o[:], ALU.mult)
                self.red("dve", ss[:], sq[:], ALU.add)
                self.act(ss[:], ss[:], AF.Sqrt, bias=self.epsb[:, 0:1], scale=1.0 / 64)
                self.recip(ss[:], ss[:])
                self.tt("dve", o[:], o[:], bc64(ss[:]), ALU.mult)
                self.tt("pool", o[:], o[:], nw[:].unsqueeze(1).to_broadcast([128, 4, 64]), ALU.mult)
                self.act(sz[:], tok[:, 768:1024], AF.Silu)
                self.tt("dve", o[:], o[:], sz[:].rearrange("p (h d) -> p h d", d=64), ALU.mult)
                self.dma("sp", mixt[:, n, 0:256], o[:].rearrange("p h d -> p (h d)"))
            self.flush()

    def phase3(self, l):
        self.phase3a(l)
        if l % 2 == 0:
            i = l // 2
            self.ffn(1, DFF, lambda e: self.ffn_w_gate[i], lambda e: self.ffn_w_up[i], lambda e: self.ffn_w_down[i], gated=False)
        else:
            i = l // 2
            self.ffn(NEXP, DFFE, lambda e: self.moe_w_gate[i, e], lambda e: self.moe_w_up[i, e], lambda e: self.moe_w_down[i, e], gated=True)

    def phase3a(self, l):
        NT, T = self.NT, self.T
        moe = (l % 2 == 1)
        with ExitStack() as st:
            sb = lambda n, s, dt=F32: self.sb(st, n, s, dt)
            ps = lambda n, s, dt=F32: self.ps(st, n, s, dt)
            wout = sb("wout", [128, 8, D], BF16)
            self.dma("pool", wout[:], self.w_out[l].rearrange("(c p) d -> p c d", p=128))
            normw = sb("normw2", [128, D])
            self.dma("sp", normw[:], self.norm_ffn[l].partition_broadcast(128))
            hs = sb("hs3", [128, 4, D])
            mx = sb("mx3", [128, 4, D])
            mxb = sb("mxb3", [128, 4, D], BF16)
            mixT = sb("mixT", [128, 8, 512], BF16)
            junk = sb("junk3", [128, D])
            ssq = sb("ssq3", [128, 4])
            rstd = sb("rstd3", [128, 4])
            ub = sb("ub3", [128, 4, D], BF16)
            uT = sb("uT3", [128, 8, 512], BF16)
            tp = ps("tp3", [128, 8, 128], BF16)
            pH = [ps("pH0", [128, 512]), ps("pH1", [128, 512])]
            if moe:
                rt = sb("router", [128, 8, NEXP])
                self.dma("sp", rt[:], self.moe_router[l // 2].rearrange("(c p) e -> p c e", p=128))
                uf = sb("uf3", [128, D])
                ufT = sb("ufT3", [128, 8, 128])
                ptf = ps("ptf3", [128, 4, 128])
                pL = ps("pL3", [128, 512])
                lg = sb("lg3", [128, NEXP])
                m1 = sb("m13", [128, 1])
                msk = sb("msk3", [128, NEXP])
                l2 = sb("l23", [128, NEXP])
                ex = sb("ex3", [128, NEXP])
                den = sb("den3", [128, 1])
                gT = sb("gT3", [NEXP, 512])
            mixt = self.MIX.rearrange("(n p) c -> p n c", p=128)
            ht = self.H.rearrange("(n p) d -> p n d", p=128)
            for (n0, TG) in self.groups():
                TW = TG * 128
                self.load_h_group(l, hs, n0, TG)
                self.dma("sp", mx[:, 0:TG, :], mixt[:, n0:n0 + TG, :])
                for j in range(TG):
                    self.cp("pool", mxb[:, j, :], mx[:, j, :])
                if n0 == 0:
                    self.memset("pool", mxb[0:112, 0, :], 0.0)
                for j in range(TG):
                    for c in range(8):
                        self.tr(tp[:, c, :], mxb[:, j, c * 128:(c + 1) * 128], self.identb[:])
                    self.cp("act", mixT[:, :, j * 128:(j + 1) * 128], tp[:, :, :])
                for j in range(TG):
                    for hf in range(2):
                        for c in range(8):
                            self.mm(pH[hf][:], mixT[:, c, j * 128:(j + 1) * 128], wout[:, c, hf * 512:(hf + 1) * 512], start=(c == 0), stop=(c == 7))
                        self.tt("dve", hs[:, j, hf * 512:(hf + 1) * 512], hs[:, j, hf * 512:(hf + 1) * 512], pH[hf][:], ALU.add)
                self.dma("sp", ht[:, n0:n0 + TG, :], hs[:, 0:TG, :])
                for j in range(TG):
                    self.tt("pool", junk[:], hs[:, j, :], hs[:, j, :], ALU.mult)
                    self.red("dve", ssq[:, j:j + 1], junk[:], ALU.add)
                self.act(rstd[:, 0:TG], ssq[:, 0:TG], AF.Sqrt, bias=self.epsb[:, 0:1], scale=1.0 / D)
                self.recip(rstd[:, 0:TG], rstd[:, 0:TG])
                for j in range(TG):
                    if moe:
                        self.stt("dve", uf[:], hs[:, j, :], rstd[:, j:j + 1], normw[:], ALU.mult, ALU.mult)
                        self.cp("pool", ub[:, j, :], uf[:])
                        for c4 in range(2):
                            for c in range(4):
                                self.tr(ptf[:, c, :], uf[:, (c4 * 4 + c) * 128:(c4 * 4 + c + 1) * 128], self.identf[:])
                            self.cp("act", ufT[:, c4 * 4:c4 * 4 + 4, :], ptf[:])
                        for c in range(8):
                            self.mm(pL[:, 0:NEXP], ufT[:, c, :], rt[:, c, :], start=(c == 0), stop=(c == 7))
                        self.cp("dve", lg[:], pL[:, 0:NEXP])
                        self.red("dve", m1[:], lg[:], ALU.max)
                        self.ts("dve", msk[:], lg[:], m1[:, 0:1], ALU.is_equal)
                        self.stt("dve", l2[:], msk[:], -BIG, lg[:], ALU.mult, ALU.add)
                        self.red("dve", den[:], l2[:], ALU.max)
                        self.ts("dve", msk[:], lg[:], den[:, 0:1], ALU.is_ge)
                        self.ts("dve", m1[:], m1[:], -1.0, ALU.mult)
                        self.act(ex[:], lg[:], AF.Exp, bias=m1[:, 0:1])
                        self.tt("dve", ex[:], ex[:], msk[:], ALU.mult)
                        self.red("dve", den[:], ex[:], ALU.add)
                        self.recip(den[:], den[:])
                        self.ts("dve", ex[:], ex[:], den[:, 0:1], ALU.mult)
                        self.tr(pL[0:NEXP, 128:256], ex[:], self.identf[:])
                        self.cp("dve", gT[:, j * 128:(j + 1) * 128], pL[0:NEXP, 128:256])
                    else:
                        self.stt("dve", ub[:, j, :], hs[:, j, :], rstd[:, j:j + 1], normw[:], ALU.mult, ALU.mult)
                for j in range(TG):
                    for c in range(8):
                        self.tr(tp[:, c, :], ub[:, j, c * 128:(c + 1) * 128], self.identb[:])
                    self.cp("act", uT[:, :, j * 128:(j + 1) * 128], tp[:, :, :])
                self.dma("sp", self.U2T[:, :, n0 * 128:n0 * 128 + TW].rearrange("c p t -> p c t"), uT[:, :, 0:TW])
                if moe:
                    self.dma("sp", self.GATES[:, n0 * 128:n0 * 128 + TW], gT[:, 0:TW])
            self.flush()

    def ffn(self, NE, FF, wg_of, wu_of, wd_of, gated):
        NT, T = self.NT, self.T
        nfc = FF // 128
        blocks = []
        f = 0
        while f < nfc:
            b = min(4, nfc - f)
            blocks.append((f, b))
            f += b
        halves = []
        n = 0
        HMAX = getattr(self, "HMAX", 17)
        while n < NT:
            k = min(HMAX, NT - n)
            halves.append((n, k))
            n += k
        with ExitStack() as st:
            sb = lambda n, s, dt=F32: self.sb(st, n, s, dt)
            ps = lambda n, s, dt=F32: self.ps(st, n, s, dt)
            hmax = max(k for _, k in halves)
            u2T = sb("fu2T", [128, 8, hmax * 128], BF16)
            yacc = sb("fyacc", [128, hmax, D])
            wg = [sb(f"fwg{i}", [128, 8, 512], BF16) for i in range(2)]
            wu = [sb(f"fwu{i}", [128, 8, 512], BF16) for i in range(2)]
            wd = [sb(f"fwd{i}", [128, 4, D], BF16) for i in range(2)]
            sg = [sb(f"fsg{i}", [128, 512]) for i in range(2)]
            actT = [sb(f"fact{i}", [128, 4, 512], BF16) for i in range(2)]
            hb = [sb(f"fhb{i}", [128, D]) for i in range(2)]
            if gated:
                gbc = sb("fgbc", [128, hmax * 128])
            pG = [ps(f"fpG{i}", [128, 512]) for i in range(2)]
            pU = [ps(f"fpU{i}", [128, 512]) for i in range(2)]
            pD = [[ps(f"fpD{i}{hf}", [128, 512]) for hf in range(2)] for i in range(2)]
            ht = self.H.rearrange("(n p) d -> p n d", p=128)
            wi = 0
            gi = 0
            ai = 0
            di = 0
            pending = [None]
            for (h0, hk) in halves:
                TH = hk * 128
                self.dma("sp", u2T[:, :, 0:TH], self.U2T[:, :, h0 * 128:h0 * 128 + TH].rearrange("c p t -> p c t"))
                self.memset("pool", yacc[:, 0:hk, :], 0.0)
                groups = []
                j = 0
                while j < hk:
                    tg = min(4, hk - j)
                    groups.append((j, tg))
                    j += tg
                for e in range(NE):
                    if gated:
                        self.dma("sp", gbc[:, 0:TH], self.GATES[e, h0 * 128:h0 * 128 + TH].partition_broadcast(128))
                    for (f0, fb) in blocks:
                        FB = fb * 128
                        g_, u_, d_ = wg[wi % 2], wu[wi % 2], wd[wi % 2]
                        wi += 1
                        self.dma("pool", g_[:, :, 0:FB], wg_of(e)[:, f0 * 128:f0 * 128 + FB].rearrange("(c p) f -> p c f", p=128))
                        self.dma("pool", u_[:, :, 0:FB], wu_of(e)[:, f0 * 128:f0 * 128 + FB].rearrange("(c p) f -> p c f", p=128))
                        self.dma("pool", d_[:, 0:fb, :], wd_of(e)[f0 * 128:f0 * 128 + FB, :].rearrange("(c p) d -> p c d", p=128))
                        for (j0, tg) in groups:
                            TW = tg * 128
                            cs_ = slice(j0 * 128, j0 * 128 + TW)
                            a_ = actT[ai % 2]
                            ai += 1
                            for fc in range(fb):
                                pg, pu, s_ = pG[gi % 2], pU[gi % 2], sg[gi % 2]
                                gi += 1
                                for c in range(8):
                                    self.mm(pg[:, 0:TW], g_[:, c, fc * 128:(fc + 1) * 128], u2T[:, c, cs_], start=(c == 0), stop=(c == 7))
                                for c in range(8):
                                    self.mm(pu[:, 0:TW], u_[:, c, fc * 128:(fc + 1) * 128], u2T[:, c, cs_], start=(c == 0), stop=(c == 7))
                                self.act(s_[:, 0:TW], pg[:, 0:TW], AF.Silu)
                                if gated:
                                    self.tt("dve", s_[:, 0:TW], s_[:, 0:TW], pu[:, 0:TW], ALU.mult)
                                    self.tt("pool", a_[:, fc, 0:TW], s_[:, 0:TW], gbc[:, cs_], ALU.mult)
                                else:
                                    self.tt("dve", a_[:, fc, 0:TW], s_[:, 0:TW], pu[:, 0:TW], ALU.mult)
                            if pending[0] is not None:
                                pending[0]()

                            def down(a_=a_, d_=d_, fb=fb, j0=j0, tg=tg):
                                nonlocal di
                                for j in range(tg):
                                    pd = pD[di % 2]
                                    di += 1
                                    for hf in range(2):
                                        for fc in range(fb):
                                            self.mm(pd[hf][:], a_[:, fc, j * 128:(j + 1) * 128], d_[:, fc, hf * 512:(hf + 1) * 512], start=(fc == 0), stop=(fc == fb - 1))
                                        ya = yacc[:, j0 + j, hf * 512:(hf + 1) * 512]
                                        self.tt("dve", ya, ya, pd[hf][:], ALU.add)
                            pending[0] = down
                if pending[0] is not None:
                    pending[0]()
                    pending[0] = None
                for j in range(hk):
                    b_ = hb[j % 2]
                    self.dma("sp", b_[:], ht[:, h0 + j, :])
                    self.tt("pool", b_[:], b_[:], yacc[:, j, :], ALU.add)
                    self.dma("sp", ht[:, h0 + j, :], b_[:])
            self.flush()

    def final(self):
        NT = self.NT
        with ExitStack() as st:
            sb = lambda n, s, dt=F32: self.sb(st, n, s, dt)
            normw = sb("normwf", [128, D])
            self.dma("sp", normw[:], self.norm_final.partition_broadcast(128))
            hs = [sb("hsf0", [128, D]), sb("hsf1", [128, D])]
            junk = sb("junkf", [128, D])
            ssq = sb("ssqf", [128, 2])
            ht = self.H.rearrange("(n p) d -> p n d", p=128)
            ot = self.out.rearrange("(n p) d -> p n d", p=128)
            for n in range(1, NT):
                h = hs[n % 2]
                s_ = ssq[:, (n % 2):(n % 2) + 1]
                self.dma("sp", h[:], ht[:, n, :])
                self.tt("pool", junk[:], h[:], h[:], ALU.mult)
                self.red("dve", s_, junk[:], ALU.add)
                self.act(s_, s_, AF.Sqrt, bias=self.epsb[:, 0:1], scale=1.0 / D)
                self.recip(s_, s_)
                self.stt("dve", h[:], h[:], s_, normw[:], ALU.mult, ALU.mult)
                self.dma("sp", ot[:, n - 1, :], h[:])
            self.flush()


_PARAMS = ["meta_tokens", "norm_mix", "w_in", "dn_conv_w", "dn_a_log", "dn_dt_bias", "dn_norm_w", "df_lambda",
           "df_norm_w", "sw_sinks", "ssd_conv_w", "ssd_conv_b", "ssd_a_log", "ssd_dt_bias", "ssd_d", "ssd_norm_w",
           "w_out", "norm_ffn", "ffn_w_gate", "ffn_w_up", "ffn_w_down", "moe_router", "moe_w_gate", "moe_w_up",
           "moe_w_down", "norm_final"]


def kernel(**inputs):
    from concourse.bass_utils import run_bass_kernel_spmd
    x = np.asarray(inputs["x"], dtype=np.float32)
    B, SEQ, _ = x.shape
    NT = SEQ // 128 + 1
    m = Model(NT, depth=2)
    nc = m.build()
    params = {k: np.ascontiguousarray(np.asarray(inputs[k], dtype=np.float32)) for k in _PARAMS}
    in_maps = []
    for b in range(B):
        d = dict(params)
        d["x"] = np.ascontiguousarray(x[b])
        in_maps.append(d)
    res = run_bass_kernel_spmd(nc, in_maps, core_ids=list(range(B)))
    return np.stack([np.asarray(r["out"], dtype=np.float32) for r in res.results], axis=0)
```

```python
import numpy as np
from contextlib import ExitStack
import concourse.bass as bass
import concourse.mybir as mybir

F32 = mybir.dt.float32
BF16 = mybir.dt.bfloat16
AF = mybir.ActivationFunctionType
ALU = mybir.AluOpType
AX = mybir.AxisListType

ENGS = ("pe", "act", "dve", "pool", "sp")
N_DMA_SEMS = 6


class Buf:
    __slots__ = ("name", "w", "r")

    def __init__(self, name):
        self.name = name
        self.w = None
        self.r = {}


class Sched:
    def __init__(self, nc, stack):
        self.nc = nc
        self.sem = {e: stack.enter_context(nc.semaphore("s_" + e)) for e in ENGS}
        self.dsem = {}
        for q in ("sp", "act", "pool"):
            for i in range(N_DMA_SEMS):
                self.dsem[(q, i)] = stack.enter_context(nc.semaphore(f"d_{q}{i}"))
        self.cnt = {e: 0 for e in ENGS}
        self.dcnt = {k: 0 for k in self.dsem}
        self.drr = {"sp": 0, "act": 0, "pool": 0}
        self.seen = {e: {} for e in ENGS}
        self.ops = {e: [] for e in ENGS}
        self.nops = 0
        import os
        self.limit = int(os.environ.get("MK_LIMIT", "1000000000"))

    def _semh(self, key):
        return self.sem[key] if isinstance(key, str) else self.dsem[key]

    def _need(self, eng, tok, waits):
        if tok is None:
            return
        key, val, teng = tok
        if eng == "pe" and teng == "pe":
            return
        if self.seen[eng].get(key, 0) >= val:
            return
        self.seen[eng][key] = val
        waits[key] = max(waits.get(key, 0), val)

    def _deps(self, eng, reads, writes, waits):
        for b in reads:
            self._need(eng, b.w, waits)
        for b in writes:
            self._need(eng, b.w, waits)
            for k, (v, te) in b.r.items():
                self._need(eng, (k, v, te), waits)

    def _commit(self, tok, reads, writes):
        for b in reads:
            o = b.r.get(tok[0])
            if o is None or o[0] < tok[1]:
                b.r[tok[0]] = (tok[1], tok[2])
        for b in writes:
            b.w = tok
            b.r = {}

    def op(self, eng, fn, reads=(), writes=()):
        if self.nops >= self.limit:
            return None
        waits = {}
        self._deps(eng, reads, writes, waits)
        self.cnt[eng] += 1
        tok = (eng, self.cnt[eng], eng)
        self.ops[eng].append((fn, waits, (eng, 1)))
        self._commit(tok, reads, writes)
        self.nops += 1
        return tok

    def dma(self, q, fn, reads=(), writes=()):
        if self.nops >= self.limit:
            return None
        waits = {}
        self._deps(q, reads, writes, waits)
        i = self.drr[q]
        self.drr[q] = (i + 1) % N_DMA_SEMS
        key = (q, i)
        if self.dcnt[key] > 0:
            self._need(q, (key, self.dcnt[key], "dma"), waits)
        self.dcnt[key] += 16
        tok = (key, self.dcnt[key], "dma")
        self.ops[q].append((fn, waits, (key, 16)))
        self._commit(tok, reads, writes)
        self.nops += 1
        return tok

    def barrier(self):
        for e in ENGS:
            waits = {}
            for k in ENGS:
                if k == "pe" and e == "pe":
                    continue
                if self.cnt[k] > 0 and self.seen[e].get(k, 0) < self.cnt[k]:
                    self.seen[e][k] = self.cnt[k]
                    waits[k] = self.cnt[k]
            for k, v in self.dcnt.items():
                if v > 0 and self.seen[e].get(k, 0) < v:
                    self.seen[e][k] = v
                    waits[k] = v
            if waits:
                self.ops[e].append((None, waits, None))

    def emit(self, block):
        names = {"pe": "tensor", "act": "scalar", "dve": "vector", "pool": "gpsimd", "sp": "sync"}
        for e in ENGS:
            ops = self.ops[e]
            self.ops[e] = []

            def body(engine, ops=ops):
                for fn, waits, inc in ops:
                    for k, v in waits.items():
                        engine.wait_ge(self._semh(k), v)
                    if fn is not None:
                        fn(engine).then_inc(self._semh(inc[0]), inc[1])
            getattr(block, names[e])(body)


D = 1024
NCOLS = 3340
C_DNQ, C_DNK, C_DNV, C_DNZ, C_DNB, C_DNA = 0, 256, 512, 768, 1024, 1028
C_DFQ, C_DFK, C_DFV = 1032, 1288, 1544
C_SWQ, C_SWK, C_SWV = 1800, 2056, 2184
C_SSZ, C_SSX, C_SSB, C_SSC, C_SSDT = 2312, 2568, 2824, 3080, 3336
EPS = 1e-6
DFF = 2816
NEXP = 8
DFFE = 3584
BIG = 1e30


class KB:
    def __init__(self, NT, depth=2, dbg=()):
        self.NT = NT
        self.T = NT * 128
        self.SEQ = self.T - 128
        self.depth = depth
        self.dbg = dbg
        self.nc = bass.Bass("TRN2", target_bir_lowering=False)
        self.bufs = {}
        self.uid = 0
        self.oplog = []
        self.psum_names = set()

    def din(self, name, shape):
        return self.nc.dram_tensor(name, list(shape), F32, kind="ExternalInput").ap()

    def dout(self, name, shape):
        return self.nc.dram_tensor(name, list(shape), F32, kind="ExternalOutput").ap()

    def dscr(self, name, shape, dt=F32):
        return self.nc.dram_tensor(name, list(shape), dt, kind="Internal").ap()

    def sb(self, st, name, shape, dt=F32):
        self.uid += 1
        return st.enter_context(self.nc.sbuf_tensor(f"{name}_{self.uid}", list(shape), dt))

    def ps(self, st, name, shape, dt=F32):
        self.uid += 1
        nm = f"{name}_{self.uid}"
        self.psum_names.add(nm)
        return st.enter_context(self.nc.psum_tensor(nm, list(shape), dt))

    def buf(self, x):
        if isinstance(x, tuple):
            key = x[1]
        else:
            key = x.name
        b = self.bufs.get(key)
        if b is None:
            b = self.bufs[key] = Buf(str(key))
        return b

    @staticmethod
    def ap(x):
        return x[0] if isinstance(x, tuple) else x

    def _op(self, eng, fn, outs, ins):
        rd = [self.buf(i) for i in ins if i is not None and not isinstance(i, (int, float))]
        wr = [self.buf(o) for o in outs]
        wr += [b for b in rd if b.name in self.psum_names and b not in wr]
        import sys
        self.oplog.append((self.S.nops, eng, sys._getframe(1).f_code.co_name, [b.name for b in wr], [b.name for b in rd]))
        self.S.op(eng, fn, reads=rd, writes=wr)

    def mm(self, out, lhsT, rhs, start=True, stop=True, acc=False, **kw):
        o, l, r = self.ap(out), self.ap(lhsT), self.ap(rhs)
        self._op("pe", lambda e: e.matmul(o, lhsT=l, rhs=r, start=start, stop=stop, skip_group_check=True, **kw),
                 [out], [lhsT, rhs] + ([out] if (acc or not start) else []))

    def tr(self, out, in_, ident):
        o, i, d = self.ap(out), self.ap(in_), self.ap(ident)
        self._op("pe", lambda e: e.transpose(o, i, d), [out], [in_, ident])

    def act(self, out, in_, func, bias=0.0, scale=1.0, accum_out=None, eng="act"):
        o, i = self.ap(out), self.ap(in_)
        b = self.ap(bias) if not isinstance(bias, (int, float)) else bias
        s = self.ap(scale) if not isinstance(scale, (int, float)) else scale
        if accum_out is not None:
            a = self.ap(accum_out)
            self._op("act", lambda e: e.activation(o, i, func, bias=b, scale=s, accum_out=a), [out, accum_out], [in_, bias, scale, accum_out])
        else:
            self._op("act", lambda e: e.activation(o, i, func, bias=b, scale=s), [out], [in_, bias, scale])

    def tt(self, eng, out, in0, in1, op):
        o, a, b = self.ap(out), self.ap(in0), self.ap(in1)
        self._op(eng, lambda e: e.tensor_tensor(o, a, b, op), [out], [in0, in1])

    def ts(self, eng, out, in0, s1, op0, s2=None, op1=None):
        o, a = self.ap(out), self.ap(in0)
        v1 = self.ap(s1) if not isinstance(s1, (int, float)) else s1
        v2 = self.ap(s2) if (s2 is not None and not isinstance(s2, (int, float))) else s2
        if op1 is None:
            self._op(eng, lambda e: e.tensor_scalar(o, a, v1, None, op0), [out], [in0, s1])
        else:
            self._op(eng, lambda e: e.tensor_scalar(o, a, v1, v2, op0, op1), [out], [in0, s1, s2])

    def stt(self, eng, out, in0, scalar, in1, op0, op1):
        o, a, b = self.ap(out), self.ap(in0), self.ap(in1)
        sc = self.ap(scalar) if not isinstance(scalar, (int, float)) else scalar
        self._op(eng, lambda e: e.scalar_tensor_tensor(o, a, sc, b, op0, op1), [out], [in0, scalar, in1])

    def cp(self, eng, out, in_):
        o, i = self.ap(out), self.ap(in_)
        if eng == "act":
            self._op(eng, lambda e: e.copy(o, i), [out], [in_])
        else:
            self._op(eng, lambda e: e.tensor_copy(o, i), [out], [in_])

    def memset(self, eng, out, val):
        o = self.ap(out)
        self._op(eng, lambda e: e.memset(o, val), [out], [])

    def red(self, eng, out, in_, op, axis=AX.X):
        o, i = self.ap(out), self.ap(in_)
        self._op(eng, lambda e: e.tensor_reduce(o, i, axis, op), [out], [in_])

    def recip(self, out, in_):
        o, i = self.ap(out), self.ap(in_)
        self._op("dve", lambda e: e.reciprocal(o, i), [out], [in_])

    def asel(self, out, in_, pattern, cmp, fill, base, cm):
        o, i = self.ap(out), self.ap(in_)
        self._op("pool", lambda e: e.affine_select(o, i, pattern=pattern, compare_op=cmp, fill=fill, base=base, channel_multiplier=cm), [out], [in_])

    def dma(self, q, out, in_, slow=False):
        o, i = self.ap(out), self.ap(in_)
        rd = [self.buf(in_)]
        wr = [self.buf(out)]
        self.oplog.append((self.S.nops, q, "dma", [b.name for b in wr], [b.name for b in rd]))
        if slow:
            self.S.dma(q, lambda e: e.dma_start(out=o, in_=i, allow_slow_non_contiguous=True), reads=rd, writes=wr)
        else:
            self.S.dma(q, lambda e: e.dma_start(out=o, in_=i), reads=rd, writes=wr)

    def flush(self):
        self.S.barrier()
        with self.nc.Block() as blk:
            self.S.emit(blk)


class Model(KB):
    def declare_io(self):
        d = self.din
        self.x = d("x", [self.SEQ, D])
        self.meta = d("meta_tokens", [16, D])
        self.norm_mix = d("norm_mix", [2, D])
        self.w_in = d("w_in", [2, D, NCOLS])
        self.dn_conv_w = d("dn_conv_w", [2, 4, 768])
        self.dn_a_log = d("dn_a_log", [2, 4])
        self.dn_dt_bias = d("dn_dt_bias", [2, 4])
        self.dn_norm_w = d("dn_norm_w", [2, 64])
        self.df_lambda = d("df_lambda", [2, 4, 32])
        self.df_norm_w = d("df_norm_w", [2, 64])
        self.sw_sinks = d("sw_sinks", [2, 4])
        self.ssd_conv_w = d("ssd_conv_w", [2, 4, 768])
        self.ssd_conv_b = d("ssd_conv_b", [2, 768])
        self.ssd_a_log = d("ssd_a_log", [2, 4])
        self.ssd_dt_bias = d("ssd_dt_bias", [2, 4])
        self.ssd_d = d("ssd_d", [2, 4])
        self.ssd_norm_w = d("ssd_norm_w", [2, 256])
        self.w_out = d("w_out", [2, D, D])
        self.norm_ffn = d("norm_ffn", [2, D])
        self.ffn_w_gate = d("ffn_w_gate", [1, D, DFF])
        self.ffn_w_up = d("ffn_w_up", [1, D, DFF])
        self.ffn_w_down = d("ffn_w_down", [1, DFF, D])
        self.moe_router = d("moe_router", [1, D, NEXP])
        self.moe_w_gate = d("moe_w_gate", [1, NEXP, D, DFFE])
        self.moe_w_up = d("moe_w_up", [1, NEXP, D, DFFE])
        self.moe_w_down = d("moe_w_down", [1, NEXP, DFFE, D])
        self.norm_final = d("norm_final", [D])
        self.out = self.dout("out", [self.SEQ, D])
        T = self.T
        s = self.dscr
        self.H = s("H", [T, D])
        self.DN_TOK = s("DN_TOK", [T, 1024])
        self.TOK2 = s("TOK2", [T, 384])
        self.SSD_TOK = s("SSD_TOK", [T, 768])
        self.SSD_FT = s("SSD_FT", [4, 128, T])
        self.DF_QT = s("DF_QT", [2, 128, T], BF16)
        self.DF_KT = s("DF_KT", [2, 128, T], BF16)
        self.SW_QT = s("SW_QT", [2, 128, T], BF16)
        self.SW_KT = s("SW_KT", [128, T], BF16)
        self.MIX = s("MIX", [T, D])
        self.U2T = s("U2T", [8, 128, T], BF16)
        self.GATES = s("GATES", [NEXP, T])

    def build(self):
        nc = self.nc
        self.declare_io()
        with ExitStack() as gs:
            self.gs = gs
            self.S = Sched(nc, gs)
            self.consts()
            for l in range(self.depth):
                self.phase1(l)
                if "p1" in self.dbg:
                    break
                self.phase_sw(l)
                self.phase_df(l)
                self.phase_ssd_dn(l)
                if "mix" in self.dbg:
                    break
                self.phase3(l)
            if not self.dbg:
                self.final()
        return nc

    def consts(self):
        gs = self.gs
        NT = self.NT
        sb = lambda n, s, dt=F32: self.sb(gs, n, s, dt)
        self.identf = sb("identf", [128, 128])
        self.identb = sb("identb", [128, 128], BF16)
        self.onesf = sb("onesf", [128, 128])
        self.Umat = sb("Umat", [128, 128])
        self.E127 = sb("E127", [128, 128])
        self.mstrict = sb("mstrict", [128, 128])
        self.mTincl = sb("mTincl", [128, 128])
        self.c01T = sb("c01T", [128, 128], BF16)
        self.p01T = sb("p01T", [128, 128], BF16)
        self.bind4 = sb("bind4", [128, 4])
        self.bind2 = sb("bind2", [128, 2])
        self.BETA = sb("BETA", [128, NT, 4])
        self.G = sb("G", [128, NT, 4])
        self.DT = sb("DT", [128, NT, 4])
        self.BGraw = sb("BGraw", [128, NT, 8])
        self.DTraw = sb("DTraw", [128, NT, 4])
        self.stats = sb("stats", [4, 8])
        self.mbias = sb("mbias", [128, 2])
        m = self.memset
        a = self.asel
        self.epsb = sb("epsb", [128, 1])
        m("pool", self.epsb[:], EPS)
        m("pool", self.identf[:], 1.0)
        a(self.identf[:], self.identf[:], [[-1, 128]], ALU.is_equal, 0.0, 0, 1)
        self.cp("dve", self.identb[:], self.identf[:])
        m("pool", self.onesf[:], 1.0)
        m("pool", self.Umat[:], 1.0)
        a(self.Umat[:], self.Umat[:], [[1, 128]], ALU.is_ge, 0.0, 0, -1)
        self.cp("dve", self.c01T[:], self.Umat[:])
        m("pool", self.E127[:], 1.0)
        a(self.E127[:], self.E127[:], [[0, 128]], ALU.is_equal, 0.0, -127, 1)
        m("pool", self.mstrict[:], 0.0)
        a(self.mstrict[:], self.mstrict[:], [[-1, 128]], ALU.is_gt, BIG, 0, 1)
        m("pool", self.mTincl[:], 0.0)
        a(self.mTincl[:], self.mTincl[:], [[1, 128]], ALU.is_ge, -BIG, 0, -1)
        m("pool", self.p01T[:], 1.0)
        a(self.p01T[:], self.p01T[:], [[-1, 128]], ALU.is_gt, 0.0, 0, 1)
        m("pool", self.bind4[:], 1.0)
        a(self.bind4[:], self.bind4[:], [[-32, 4]], ALU.is_ge, 0.0, 0, 1)
        a(self.bind4[:], self.bind4[:], [[32, 4]], ALU.is_ge, 0.0, 31, -1)
        m("pool", self.bind2[:], 1.0)
        a(self.bind2[:], self.bind2[:], [[-64, 2]], ALU.is_ge, 0.0, 0, 1)
        a(self.bind2[:], self.bind2[:], [[64, 2]], ALU.is_ge, 0.0, 63, -1)

    def groups(self):
        NT = self.NT
        g = []
        n = 0
        while n < NT:
            tg = min(4, NT - n)
            g.append((n, tg))
            n += tg
        return g

    def load_h_group(self, l, hs, n0, TG):
        if l == 0:
            j0 = 0
            if n0 == 0:
                self.memset("pool", hs[:, 0, :], 0.0)
                self.dma("sp", hs[112:128, 0, :], self.meta[:, :])
                j0 = 1
            if TG - j0 > 0:
                xt = self.x.rearrange("(n p) d -> p n d", p=128)
                self.dma("sp", hs[:, j0:TG, :], xt[:, n0 + j0 - 1:n0 + TG - 1, :])
        else:
            ht = self.H.rearrange("(n p) d -> p n d", p=128)
            self.dma("sp", hs[:, 0:TG, :], ht[:, n0:n0 + TG, :])

    def rmsnorm_T(self, st_tiles, hs, TG, normw, n0, uT, tp, zero_pad=True):
        junk, ssq, rstd, ub = st_tiles
        for j in range(TG):
            self.tt("pool", junk[:], hs[:, j, :], hs[:, j, :], ALU.mult)
            self.red("dve", ssq[:, j:j + 1], junk[:], ALU.add)
        self.act(rstd[:, 0:TG], ssq[:, 0:TG], AF.Sqrt, bias=self.epsb[:, 0:1], scale=1.0 / D)
        self.recip(rstd[:, 0:TG], rstd[:, 0:TG])
        for j in range(TG):
            self.stt("dve", ub[:, j, :], hs[:, j, :], rstd[:, j:j + 1], normw[:], ALU.mult, ALU.mult)
        if zero_pad and n0 == 0:
            self.memset("pool", ub[0:112, 0, :], 0.0)
        for j in range(TG):
            for c in range(8):
                self.tr(tp[:, c, :], ub[:, j, c * 128:(c + 1) * 128], self.identb[:])
            self.cp("act", uT[:, :, j * 128:(j + 1) * 128], tp[:, :, :])

    def phase1(self, l):
        NT = self.NT
        with ExitStack() as st:
            sb = lambda n, s, dt=F32: self.sb(st, n, s, dt)
            ps = lambda n, s, dt=F32: self.ps(st, n, s, dt)
            win = sb("win", [128, 8, NCOLS], BF16)
            for c in range(8):
                self.dma("pool", win[:, c, :], self.w_in[l, c * 128:(c + 1) * 128, :])
            wswq = sb("wswq", [128, 8, 256], BF16)
            for i_, h_ in enumerate((0, 2, 1, 3)):
                self.cp("pool", wswq[:, :, i_ * 64:(i_ + 1) * 64], win[:, :, C_SWQ + h_ * 64:C_SWQ + (h_ + 1) * 64])
            normw = sb("normw", [128, D])
            self.dma("sp", normw[:], self.norm_mix[l].partition_broadcast(128))
            dncw = sb("dncw", [128, 6, 4])
            for cc in range(6):
                self.dma("sp", dncw[:, cc, :], self.dn_conv_w[l, :, cc * 128:(cc + 1) * 128].rearrange("j p -> p j"), slow=True)
            sscw = sb("sscw", [128, 6, 4])
            for cc in range(6):
                self.dma("sp", sscw[:, cc, :], self.ssd_conv_w[l, :, cc * 128:(cc + 1) * 128].rearrange("j p -> p j"), slow=True)
            sscb = sb("sscb", [128, 6])
            self.dma("sp", sscb[:], self.ssd_conv_b[l].rearrange("(c p) -> p c", p=128), slow=True)
            hp = sb("hp", [128, 16])
            self.dma("sp", hp[:, 0:4], self.dn_a_log[l].partition_broadcast(128))
            self.dma("sp", hp[:, 4:8], self.dn_dt_bias[l].partition_broadcast(128))
            self.dma("sp", hp[:, 8:12], self.ssd_dt_bias[l].partition_broadcast(128))

            hs_one = sb("hsA", [128, 4, D])
            hs_l = [hs_one, hs_one]
            junk = sb("junk", [128, D])
            ssq_l = [sb("ssqA", [128, 4]), sb("ssqB", [128, 4])]
            rstd_l = [sb("rstdA", [128, 4]), sb("rstdB", [128, 4])]
            ub_l = [sb("ubA", [128, 4, D], BF16), sb("ubB", [128, 4, D], BF16)]
            uT_l = [sb("uTA", [128, 8, 512], BF16), sb("uTB", [128, 8, 512], BF16)]
            xin_dn = sb("xin_dn", [128, 6, 515])
            xin_ss = sb("xin_ss", [128, 6, 515])
            cacc_l = [sb("caccA", [128, 512]), sb("caccB", [128, 512])]
            cai = [0]
            cs = [sb("cs0", [128, 512]), sb("cs1", [128, 512])]
            dnst = sb("dnst", [128, 4, 1024])
            ssdst = sb("ssdst", [128, 4, 768])
            tok2st = sb("tok2st", [128, 4, 384])
            fst = [sb("fst0", [128, 512], BF16), sb("fst1", [128, 512], BF16)]
            sqf = sb("sqf", [128, 512])
            sqt = sb("sqt", [128, 512])
            ssn = sb("ssn", [128, 8])
            stmp = sb("stmp", [4, 1])

            tp = ps("tp", [128, 8, 128], BF16)
            pfm = [ps("pfm0", [128, 512]), ps("pfm1", [128, 512])]
            pA = ps("pA", [128, 512])
            pB = ps("pB", [128, 512])
            pC = ps("pC", [128, 512])
            t32 = ps("t32", [128, 4, 128])
            pstat = ps("pstat", [4, 512])

            self.memset("pool", xin_dn[:], 0.0)
            self.memset("pool", xin_ss[:], 0.0)
            self.memset("pool", self.stats[:], 0.0)
            fmi = [0]
            csi = [0]
            fsi = [0]

            for gidx, (n0, TG) in enumerate(self.groups()):
                TW = TG * 128
                hs, ssq, rstd, ub, uT = hs_l[gidx % 2], ssq_l[gidx % 2], rstd_l[gidx % 2], ub_l[gidx % 2], uT_l[gidx % 2]
                self.load_h_group(l, hs, n0, TG)
                self.rmsnorm_T((junk, ssq, rstd, ub), hs, TG, normw, n0, uT, tp)

                def fm(lhs_fn):
                    pf = pfm[fmi[0] % 2]
                    fmi[0] += 1
                    for c in range(8):
                        self.mm(pf[:, 0:TW], lhs_fn(c), uT[:, c, 0:TW], start=(c == 0), stop=(c == 7))
                    return pf

                def conv_chunk(pf, xin, cc, cw, bias=None):
                    cacc = cacc_l[cai[0] % 2]
                    cai[0] += 1
                    self.cp("act", xin[:, cc, 3:3 + TW], pf[:, 0:TW])
                    self.ts("dve", cacc[:, 0:TW], xin[:, cc, 0:TW], cw[:, cc, 0:1], ALU.mult)
                    for j in range(1, 4):
                        self.stt("dve", cacc[:, 0:TW], xin[:, cc, j:j + TW], cw[:, cc, j:j + 1], cacc[:, 0:TW], ALU.mult, ALU.add)
                    o = cs[csi[0] % 2]
                    csi[0] += 1
                    if bias is None:
                        self.act(o[:, 0:TW], cacc[:, 0:TW], AF.Silu)
                    else:
                        self.act(o[:, 0:TW], cacc[:, 0:TW], AF.Silu, bias=bias)
                    self.cp("pool", xin[:, cc, 0:3], xin[:, cc, TW:TW + 3])
                    return o

                def to_tok(o, dst, col0):
                    for j in range(TG):
                        self.tr(t32[:, j, :], o[:, j * 128:(j + 1) * 128], self.identf[:])
                    self.cp("dve", dst[:, 0:TG, col0:col0 + 128], t32[:, 0:TG, :])

                for cc in range(6):
                    pf = fm(lambda c, cc=cc: win[:, c, C_DNQ + cc * 128:C_DNQ + (cc + 1) * 128])
                    o = conv_chunk(pf, xin_dn, cc, dncw)
                    to_tok(o, dnst, cc * 128)
                for cc in range(6):
                    pf = fm(lambda c, cc=cc: win[:, c, C_SSX + cc * 128:C_SSX + (cc + 1) * 128])
                    o = conv_chunk(pf, xin_ss, cc, sscw, bias=sscb[:, cc:cc + 1])
                    if cc < 4:
                        to_tok(o, ssdst, cc * 128)
                    if cc >= 2:
                        self.dma("sp", self.SSD_FT[cc - 2, :, n0 * 128:n0 * 128 + TW], o[:, 0:TW])
                def qk_chunk(lhs_fn, scale, dst_ap, stat_col, bind, nb):
                    pf = fm(lhs_fn)
                    f = fst[fsi[0] % 2]
                    fsi[0] += 1
                    self.act(f[:, 0:TW], pf[:, 0:TW], AF.Identity, scale=scale)
                    self.act(sqf[:, 0:TW], pf[:, 0:TW], AF.Square, scale=scale)
                    self.mm(pstat[0:nb, 0:TW], bind[:, 0:nb], sqf[:, 0:TW])
                    self.red("dve", stmp[0:nb, 0:1], pstat[0:nb, 0:TW], ALU.max)
                    self.tt("dve", self.stats[0:nb, stat_col:stat_col + 1], self.stats[0:nb, stat_col:stat_col + 1], stmp[0:nb, 0:1], ALU.max)
                    self.dma("sp", dst_ap, f[:, 0:TW])

                sl = slice(n0 * 128, n0 * 128 + TW)
                for c2 in range(2):
                    qk_chunk(lambda c, c2=c2: win[:, c, C_DFQ + c2 * 128:C_DFQ + (c2 + 1) * 128], 32 ** -0.5, self.DF_QT[c2, :, sl], 0, self.bind4, 4)
                    qk_chunk(lambda c, c2=c2: win[:, c, C_DFK + c2 * 128:C_DFK + (c2 + 1) * 128], 1.0, self.DF_KT[c2, :, sl], 1, self.bind4, 4)
                for c2 in range(2):
                    qk_chunk(lambda c, c2=c2: wswq[:, c, c2 * 128:(c2 + 1) * 128], 64 ** -0.5, self.SW_QT[c2, :, sl], 2, self.bind2, 2)
                qk_chunk(lambda c: win[:, c, C_SWK:C_SWK + 128], 1.0, self.SW_KT[:, sl], 3, self.bind2, 2)

                for j in range(TG):
                    n = n0 + j
                    lt = lambda c: uT[:, c, j * 128:(j + 1) * 128]
                    for c in range(8):
                        self.mm(pA[:, 0:264], lt(c), win[:, c, C_DNZ:C_DNZ + 264], start=(c == 0), stop=(c == 7))
                    for c in range(8):
                        self.mm(pA[:, 264:268], lt(c), win[:, c, C_SSDT:C_SSDT + 4], start=(c == 0), stop=(c == 7))
                    for c in range(8):
                        self.mm(pB[:, 0:256], lt(c), win[:, c, C_DFV:C_DFV + 256], start=(c == 0), stop=(c == 7))
                    for c in range(8):
                        self.mm(pC[:, 0:384], lt(c), win[:, c, C_SWV:C_SWV + 384], start=(c == 0), stop=(c == 7))
                    self.cp("act", dnst[:, j, 768:1024], pA[:, 0:256])
                    self.cp("dve", self.BGraw[:, n, :], pA[:, 256:264])
                    self.cp("dve", self.DTraw[:, n, :], pA[:, 264:268])
                    self.cp("act", tok2st[:, j, 0:256], pB[:, 0:256])
                    self.cp("dve", tok2st[:, j, 256:384], pC[:, 0:128])
                    self.cp("act", ssdst[:, j, 512:768], pC[:, 128:384])
                for j in range(TG):
                    qk = dnst[:, j, 0:512].rearrange("p (h d) -> p h d", d=64)
                    self.tt("pool", sqt[:], dnst[:, j, 0:512], dnst[:, j, 0:512], ALU.mult)
                    self.red("dve", ssn[:], sqt[:].rearrange("p (h d) -> p h d", d=64), ALU.add)
                    self.act(ssn[:], ssn[:], AF.Sqrt, bias=self.epsb[:, 0:1])
                    self.recip(ssn[:], ssn[:])
                    self.ts("dve", ssn[:, 0:4], ssn[:, 0:4], 0.125, ALU.mult)
                    self.tt("dve", qk, qk, ssn[:].unsqueeze(2).to_broadcast([128, 8, 64]), ALU.mult)
                tsl = slice(n0, n0 + TG)
                self.dma("sp", self.DN_TOK.rearrange("(n p) c -> p n c", p=128)[:, tsl, :], dnst[:, 0:TG, :])
                self.dma("sp", self.SSD_TOK.rearrange("(n p) c -> p n c", p=128)[:, tsl, :], ssdst[:, 0:TG, :])
                self.dma("sp", self.TOK2.rearrange("(n p) c -> p n c", p=128)[:, tsl, :], tok2st[:, 0:TG, :])

            tmp = sb("gtmp", [128, NT, 4])
            tmp2 = sb("gtmp2", [128, NT, 4])
            negA = sb("negA", [128, 4])
            bc = lambda ap: ap.unsqueeze(1).to_broadcast([128, NT, 4])
            self.act(self.BETA[:], self.BGraw[:, :, 0:4], AF.Sigmoid)

            def softplus(dst, src_raw, bias_ap):
                self.tt("dve", tmp[:], src_raw, bc(bias_ap), ALU.add)
                self.act(tmp2[:], tmp[:], AF.Abs)
                self.act(tmp2[:], tmp2[:], AF.Exp, scale=-1.0)
                self.act(tmp2[:], tmp2[:], AF.Ln, bias=1.0)
                self.stt("dve", dst, tmp[:], 0.0, tmp2[:], ALU.max, ALU.add)

            softplus(self.G[:], self.BGraw[:, :, 4:8], hp[:, 4:8])
            self.act(negA[:], hp[:, 0:4], AF.Exp)
            self.ts("dve", negA[:], negA[:], -1.0, ALU.mult)
            self.tt("dve", self.G[:], self.G[:], bc(negA[:]), ALU.mult)
            softplus(self.DT[:], self.DTraw[:], hp[:, 8:12])
            self.memset("pool", self.G[0:112, 0, :], 0.0)
            self.memset("pool", self.BETA[0:112, 0, :], 0.0)
            self.memset("pool", self.DT[0:112, 0, :], 0.0)

            s4 = sb("s4", [4, 4])
            pt1 = pstat
            r14 = sb("r14", [1, 4])
            m11 = sb("m11", [1, 2])
            pbc = pA
            for i in range(4):
                self.tr(pt1[0:1, 0:4], self.stats[0:4, i:i + 1], self.identf[0:4, 0:4])
                self.red("dve", r14[0:1, i:i + 1], pt1[0:1, 0:4], ALU.max)
            self.tt("dve", m11[0:1, 0:1], r14[0:1, 0:1], r14[0:1, 1:2], ALU.mult)
            self.tt("dve", m11[0:1, 1:2], r14[0:1, 2:3], r14[0:1, 3:4], ALU.mult)
            self.act(m11[:], m11[:], AF.Sqrt)
            self.ts("dve", m11[:], m11[:], -1.0, ALU.mult)
            self.mm(pbc[:, 0:2], self.onesf[0:1, :], m11[0:1, 0:2])
            self.cp("dve", self.mbias[:], pbc[:, 0:2])
            self.flush()

    def phase_sw(self, l):
        NT, T = self.NT, self.T
        with ExitStack() as st:
            sb = lambda n, s, dt=F32: self.sb(st, n, s, dt)
            ps = lambda n, s, dt=F32: self.ps(st, n, s, dt)
            QT = sb("swQT", [128, 2, T], BF16)
            KT = sb("swKT", [128, T], BF16)
            for c in range(2):
                self.dma("sp", QT[:, c, :], self.SW_QT[c, :, :])
            self.dma("sp", KT[:], self.SW_KT[:, :])
            QTp = sb("swQTp", [128, 4, T], BF16)
            self.memset("pool", QTp[:], 0.0)
            for h in range(4):
                kv = h // 2
                self.cp("pool", QTp[64 * kv:64 * kv + 64, h, :], QT[64 * kv:64 * kv + 64, h % 2, :])
            Vst = sb("swVst", [128, NT, 128])
            self.dma("sp", Vst[:], self.TOK2.rearrange("(n p) c -> p n c", p=128)[:, :, 256:384])
            Vaug = sb("swVaug", [128, NT, 2, 66], BF16)
            self.cp("dve", Vaug[:, :, :, 0:64], Vst[:].rearrange("p n (g d) -> p n g d", d=64))
            self.memset("pool", Vaug[:, :, :, 64:66], 0.0)
            self.memset("pool", Vaug[:, :, :, 64:65], 1.0)
            self.memset("pool", Vaug[0:112, 0, :, :], 0.0)
            sk = sb("swsk", [128, 4])
            self.dma("sp", sk[:], self.sw_sinks[l].partition_broadcast(128))
            esink = sb("esink", [128, 4])
            self.act(esink[:], sk[:], AF.Exp, bias=self.mbias[:, 1:2])
            PTp = sb("PTp", [128, 4, 128], BF16)
            PTo = sb("PTo", [128, 4, 128], BF16)
            zt = sb("swz", [128, 4])
            ysw = [sb("ysw0", [128, 4, 64]), sb("ysw1", [128, 4, 64])]
            pSp = ps("pSp", [128, 4, 128])
            pSo = ps("pSo", [128, 4, 128])
            pO = [ps("pO0", [128, 512])[:, 0:264].rearrange("p (h d) -> p h d", d=66), ps("pO1", [128, 512])[:, 0:264].rearrange("p (h d) -> p h d", d=66)]
            mixt = self.MIX.rearrange("(n p) c -> p n c", p=128)
            for i in range(NT):
                qs = slice(i * 128, (i + 1) * 128)
                blks = ([(i - 1, pSp, PTp, self.p01T)] if i > 0 else []) + [(i, pSo, PTo, self.c01T)]
                for (j, pS, PT, msk) in blks:
                    ks = slice(j * 128, (j + 1) * 128)
                    for h in range(4):
                        self.mm(pS[:, h, :], KT[:, ks], QTp[:, h, qs])
                    self.act(PT[:], pS[:], AF.Exp, bias=self.mbias[:, 1:2])
                    self.tt("pool", PT[:], PT[:], msk[:].unsqueeze(1).to_broadcast([128, 4, 128]), ALU.mult)
                po = pO[i % 2]
                for h in range(4):
                    kv = h // 2
                    for bi, (j, pS, PT, msk) in enumerate(blks):
                        self.mm(po[:, h, :], PT[:, h, :], Vaug[:, j, kv, :], start=(bi == 0), stop=(bi == len(blks) - 1))
                self.tt("dve", zt[:], po[:, :, 64], esink[:], ALU.add)
                self.recip(zt[:], zt[:])
                y = ysw[i % 2]
                self.tt("dve", y[:], po[:, :, 0:64], zt[:].unsqueeze(2).to_broadcast([128, 4, 64]), ALU.mult)
                self.dma("sp", mixt[:, i, 512:768], y[:].rearrange("p h d -> p (h d)"))
            self.flush()

    def phase_df(self, l):
        NT, T = self.NT, self.T
        import math
        lam_init = 0.8 - 0.6 * math.exp(-0.3 * l)
        with ExitStack() as st:
            sb = lambda n, s, dt=F32: self.sb(st, n, s, dt)
            ps = lambda n, s, dt=F32: self.ps(st, n, s, dt)
            QT = sb("dfQT", [128, 2, T], BF16)
            KT = sb("dfKT", [128, 2, T], BF16)
            for c in range(2):
                self.dma("sp", QT[:, c, :], self.DF_QT[c, :, :])
                self.dma("sp", KT[:, c, :], self.DF_KT[c, :, :])
            QTp = sb("dfQTp", [128, 8, T], BF16)
            self.memset("pool", QTp[:], 0.0)
            for hm in range(8):
                pb = 32 * (hm % 4)
                self.cp("pool" if hm % 2 else "dve", QTp[pb:pb + 32, hm, :], QT[pb:pb + 32, hm // 4, :])
            Vst = sb("dfVst", [128, NT, 256])
            self.dma("sp", Vst[:], self.TOK2.rearrange("(n p) c -> p n c", p=128)[:, :, 0:256])
            Vaug = sb("dfVaug", [128, NT, 4, 66], BF16)
            self.cp("dve", Vaug[:, :, :, 0:64], Vst[:].rearrange("p n (g d) -> p n g d", d=64))
            self.memset("pool", Vaug[:, :, :, 64:66], 0.0)
            self.memset("pool", Vaug[:, :, :, 64:65], 1.0)
            self.memset("pool", Vaug[0:112, 0, :, :], 0.0)
            lt = sb("dflam", [128, 4, 32])
            self.dma("sp", lt[:].rearrange("p a b -> p (a b)"), self.df_lambda[l].rearrange("a b -> (a b)").partition_broadcast(128))
            lp = sb("dflp", [128, 2, 32])
            l2 = sb("dfl2", [128, 2])
            neglam = sb("neglam", [128, 1])
            self.tt("dve", lp[:, 0, :], lt[:, 0, :], lt[:, 1, :], ALU.mult)
            self.tt("dve", lp[:, 1, :], lt[:, 2, :], lt[:, 3, :], ALU.mult)
            self.red("dve", l2[:], lp[:], ALU.add)
            self.act(l2[:], l2[:], AF.Exp)
            self.tt("dve", neglam[:], l2[:, 1:2], l2[:, 0:1], ALU.subtract)
            self.ts("dve", neglam[:], neglam[:], -lam_init, ALU.add)
            nw = sb("dfnw", [128, 64])
            self.dma("sp", nw[:], self.df_norm_w[l].partition_broadcast(128))
            self.ts("dve", nw[:], nw[:], 1.0 - lam_init, ALU.mult)

            PTs = [sb(f"dfPT{k}", [128, 512], BF16) for k in range(4)]
            pSs = [ps(f"dfpS{k}", [128, 512]) for k in range(4)]
            pO = [[ps(f"dfpO{m}{k}", [128, 512])[:, 0:264].rearrange("p (h d) -> p h d", d=66) for k in range(2)] for m in range(2)]
            r0 = sb("dfr0", [128, 4])
            r1 = sb("dfr1", [128, 4])
            t0 = sb("dft0", [128, 4, 64])
            t1 = sb("dft1", [128, 4, 64])
            sq = sb("dfsq", [128, 4, 64])
            ss = sb("dfss", [128, 4])
            ydf = [sb("ydf0", [128, 4, 64]), sb("ydf1", [128, 4, 64])]
            mixt = self.MIX.rearrange("(n p) c -> p n c", p=128)
            it = 0
            LA = 3
            gstep = [0]
            for (b0, TG) in self.groups():
                for h in range(4):
                    po = [pO[0][it % 2], pO[1][it % 2]]
                    first = [True, True]
                    steps = [(j, m) for j in range(0, b0 + TG) for m in range(2)]
                    bufof = {}

                    def score(i):
                        j, m = steps[i]
                        k = gstep[0] % 4
                        gstep[0] += 1
                        bufof[i] = k
                        qlo = max(j, b0)
                        ncol = (b0 + TG - qlo) * 128
                        qcols = slice(qlo * 128, (b0 + TG) * 128)
                        ks = slice(j * 128, (j + 1) * 128)
                        hm = 2 * h + m
                        c = hm // 4
                        pst, ptt = pSs[k], PTs[k]
                        self.mm(pst[:, 0:ncol], KT[:, c, ks], QTp[:, hm, qcols])
                        self.act(ptt[:, 0:ncol], pst[:, 0:ncol], AF.Exp, bias=self.mbias[:, 0:1])
                        if j >= b0:
                            self.tt("pool", ptt[:, 0:128], ptt[:, 0:128], self.c01T[:], ALU.mult)

                    def pv(i):
                        j, m = steps[i]
                        ptt = PTs[bufof[i]]
                        qlo = max(j, b0)
                        for bi in range(qlo, b0 + TG):
                            col = (bi - qlo) * 128
                            self.mm(po[m][:, bi - b0, :], ptt[:, col:col + 128], Vaug[:, j, h, :], start=first[m], stop=False, acc=True)
                            first[m] = False

                    ns = len(steps)
                    for i in range(ns + LA):
                        if i < ns:
                            score(i)
                        if i - LA >= 0:
                            pv(i - LA)
                    if b0 == 0:
                        self.ts("dve", r0[:, 0:TG], po[0][:, 0:TG, 64], 1e-30, ALU.max)
                        self.ts("dve", r1[:, 0:TG], po[1][:, 0:TG, 64], 1e-30, ALU.max)
                        self.recip(r0[:, 0:TG], r0[:, 0:TG])
                        self.recip(r1[:, 0:TG], r1[:, 0:TG])
                    else:
                        self.recip(r0[:, 0:TG], po[0][:, 0:TG, 64])
                        self.recip(r1[:, 0:TG], po[1][:, 0:TG, 64])
                    self.ts("dve", r1[:, 0:TG], r1[:, 0:TG], neglam[:, 0:1], ALU.mult)
                    self.tt("dve", t0[:, 0:TG, :], po[0][:, 0:TG, 0:64], r0[:, 0:TG].unsqueeze(2).to_broadcast([128, TG, 64]), ALU.mult)
                    self.tt("dve", t1[:, 0:TG, :], po[1][:, 0:TG, 0:64], r1[:, 0:TG].unsqueeze(2).to_broadcast([128, TG, 64]), ALU.mult)
                    self.tt("pool", t0[:, 0:TG, :], t0[:, 0:TG, :], t1[:, 0:TG, :], ALU.add)
                    self.tt("pool", sq[:, 0:TG, :], t0[:, 0:TG, :], t0[:, 0:TG, :], ALU.mult)
                    self.red("dve", ss[:, 0:TG], sq[:, 0:TG, :], ALU.add)
                    self.act(ss[:, 0:TG], ss[:, 0:TG], AF.Sqrt, bias=self.epsb[:, 0:1], scale=1.0 / 64)
                    self.recip(ss[:, 0:TG], ss[:, 0:TG])
                    y = ydf[it % 2]
                    self.tt("dve", y[:, 0:TG, :], t0[:, 0:TG, :], ss[:, 0:TG].unsqueeze(2).to_broadcast([128, TG, 64]), ALU.mult)
                    self.tt("pool", y[:, 0:TG, :], y[:, 0:TG, :], nw[:].unsqueeze(1).to_broadcast([128, TG, 64]), ALU.mult)
                    self.dma("sp", mixt[:, b0:b0 + TG, 256 + h * 64:256 + (h + 1) * 64], y[:, 0:TG, :])
                    it += 1
            self.flush()

    def chunk_cumsum(self, st, src, name, pc_in=None):
        NT = self.NT
        cum = self.sb(st, name + "cum", [128, NT, 4])
        last = self.sb(st, name + "last", [128, NT, 4])
        pc = pc_in if pc_in is not None else self.ps(st, name + "pc", [128, 512])
        n4 = NT * 4
        self.mm(pc[:, 0:n4], self.Umat[:], src[:].rearrange("p n h -> p (n h)"))
        self.cp("dve", cum[:].rearrange("p n h -> p (n h)"), pc[:, 0:n4])
        self.mm(pc[:, 0:n4], self.E127[:], cum[:].rearrange("p n h -> p (n h)"))
        self.cp("dve", last[:].rearrange("p n h -> p (n h)"), pc[:, 0:n4])
        return cum, last, pc

    def phase_ssd(self, l):
        with ExitStack() as st:
            for _ in self.gen_ssd(l, st):
                pass
            self.flush()

    def phase_dn(self, l):
        with ExitStack() as st:
            for _ in self.gen_dn(l, st):
                pass
            self.flush()

    def phase_ssd_dn(self, l):
        with ExitStack() as st:
            active = [self.gen_dn(l, st), self.gen_ssd(l, st)]
            while active:
                for g in list(active):
                    try:
                        next(g)
                    except StopIteration:
                        active.remove(g)
            self.flush()

    def gen_ssd(self, l, st):
        NT, T = self.NT, self.T
        if True:
            sb = lambda n, s, dt=F32: self.sb(st, n, s, dt)
            ps = lambda n, s, dt=F32: self.ps(st, n, s, dt)
            hp = sb("sshp", [128, 8])
            self.dma("sp", hp[:, 0:4], self.ssd_a_log[l].partition_broadcast(128))
            self.dma("sp", hp[:, 4:8], self.ssd_d[l].partition_broadcast(128))
            nw = sb("ssnw", [128, 256])
            self.dma("sp", nw[:], self.ssd_norm_w[l].partition_broadcast(128))
            negA = sb("ssnegA", [128, 4])
            self.act(negA[:], hp[:, 0:4], AF.Exp)
            self.ts("dve", negA[:], negA[:], -1.0, ALU.mult)
            DA = sb("ssDA", [128, NT, 4])
            self.tt("dve", DA[:], self.DT[:], negA[:].unsqueeze(1).to_broadcast([128, NT, 4]), ALU.mult)
            pY_pre = ps("sspY", [128, 4, 128])
            ACUM, AL, pc = self.chunk_cumsum(st, DA, "ss", pc_in=pY_pre[:].rearrange("p h d -> p (h d)"))
            EAC = sb("ssEAC", [128, NT, 4])
            EDEC = sb("ssEDEC", [128, NT, 4])
            ECH = sb("ssECH", [128, NT, 4])
            self.act(EAC[:], ACUM[:], AF.Exp)
            self.tt("dve", EDEC[:], AL[:], ACUM[:], ALU.subtract)
            self.act(EDEC[:], EDEC[:], AF.Exp)
            self.act(ECH[:], AL[:], AF.Exp)
            hT = sb("sshT", [128, 4, 64])
            self.memset("pool", hT[:], 0.0)
            tok = sb("sstok", [128, 768])
            ft = sb("ssft", [128, 4, 128])
            diag = sb("ssdiag", [128, 4, 128])
            X = sb("ssX", [128, 4, 128])
            lmT = sb("sslmT", [128, 4, 128])
            WT = sb("ssWT", [128, 4, 128])
            xc = sb("ssxc", [128, 4, 64])
            xdec = sb("ssxdec", [128, 4, 64])
            t1 = sb("sst1", [128, 4, 64])
            y = sb("ssy", [128, 256])
            sz = sb("sssz", [128, 256])
            sq = sb("sssq", [128, 256])
            ss = sb("ssss", [128, 2])
            pBr = ps("sspA", [128, 4, 128])
            pBC = pBr
            pY = pY_pre
            pSt = pBr
            mixt = self.MIX.rearrange("(n p) c -> p n c", p=128)
            stok = self.SSD_TOK.rearrange("(n p) c -> p n c", p=128)
            bc64 = lambda ap: ap.unsqueeze(2).to_broadcast([128, 4, 64])
            bc128 = lambda ap: ap.unsqueeze(2).to_broadcast([128, 4, 128])
            for n in range(NT):
                self.dma("sp", tok[:], stok[:, n, :])
                self.dma("sp", ft[:], self.SSD_FT[:, :, n * 128:(n + 1) * 128].rearrange("c p t -> p c t"))
                x3 = tok[:, 0:256].rearrange("p (h d) -> p h d", d=64)
                self.tt("pool", diag[:], self.identf[:].unsqueeze(1).to_broadcast([128, 4, 128]), bc128(ACUM[:, n, :]), ALU.mult)
                for h in range(4):
                    self.mm(pBr[:, h, :], self.onesf[:], diag[:, h, :])
                self.tt("dve", X[:], pBr[:], bc128(ACUM[:, n, :]), ALU.subtract)
                self.tt("dve", X[:], X[:], self.mTincl[:].unsqueeze(1).to_broadcast([128, 4, 128]), ALU.min)
                self.act(lmT[:], X[:], AF.Exp)
                yield
                for g in range(2):
                    self.mm(pBC[:, g, :], ft[:, g, :], ft[:, 2 + g, :])
                for g in range(2):
                    self.tt("dve", WT[:, 2 * g:2 * g + 2, :], pBC[:, g, :].unsqueeze(1).to_broadcast([128, 2, 128]), lmT[:, 2 * g:2 * g + 2, :], ALU.mult)
                self.tt("pool", xc[:], x3, bc64(self.DT[:, n, :]), ALU.mult)
                for h in range(4):
                    self.mm(pY[:, h, 0:64], WT[:, h, :], xc[:, h, :])
                for h in range(4):
                    self.mm(pY[:, h, 64:128], ft[:, 2 + h // 2, :], hT[:, h, :])
                self.tt("dve", t1[:], pY[:, :, 64:128], bc64(EAC[:, n, :]), ALU.mult)
                self.tt("dve", t1[:], t1[:], pY[:, :, 0:64], ALU.add)
                y3 = y[:].rearrange("p (h d) -> p h d", d=64)
                self.tt("pool", y3, x3, bc64(hp[:, 4:8]), ALU.mult)
                self.tt("pool", y3, y3, t1[:], ALU.add)
                yield
                self.tt("pool", xdec[:], xc[:], bc64(EDEC[:, n, :]), ALU.mult)
                for h in range(4):
                    g = h // 2
                    self.mm(pSt[:, h, 0:64], tok[:, 256 + 128 * g:256 + 128 * (g + 1)], xdec[:, h, :])
                self.tt("pool", hT[:], hT[:], bc64(ECH[:, n, :]), ALU.mult)
                self.tt("dve", hT[:], hT[:], pSt[:, :, 0:64], ALU.add)
                self.act(sz[:], tok[:, 512:768], AF.Silu)
                self.tt("dve", y[:], y[:], sz[:], ALU.mult)
                self.tt("pool", sq[:], y[:], y[:], ALU.mult)
                self.red("dve", ss[:], sq[:].rearrange("p (g d) -> p g d", d=128), ALU.add)
                self.act(ss[:], ss[:], AF.Sqrt, bias=self.epsb[:, 0:1], scale=1.0 / 128)
                self.recip(ss[:], ss[:])
                yg = y[:].rearrange("p (g d) -> p g d", d=128)
                self.tt("dve", yg, yg, ss[:].unsqueeze(2).to_broadcast([128, 2, 128]), ALU.mult)
                self.tt("pool", y[:], y[:], nw[:], ALU.mult)
                self.dma("sp", mixt[:, n, 768:1024], y[:])
                yield

    def gen_dn(self, l, st):
        NT, T = self.NT, self.T
        if True:
            sb = lambda n, s, dt=F32: self.sb(st, n, s, dt)
            ps = lambda n, s, dt=F32: self.ps(st, n, s, dt)
            nw = sb("dnnw", [128, 64])
            self.dma("sp", nw[:], self.dn_norm_w[l].partition_broadcast(128))
            B = [ps(f"dnB{i}", [128, 4, 128]) for i in range(6)]
            GC, GL, pc = self.chunk_cumsum(st, self.G, "dn", pc_in=B[5][:].rearrange("p h d -> p (h d)"))
            EGC = sb("dnEGC", [128, NT, 4])
            EKD = sb("dnEKD", [128, NT, 4])
            EGL = sb("dnEGL", [128, NT, 4])
            BGE = sb("dnBGE", [128, NT, 4])
            NEGB = sb("dnNEGB", [128, NT, 4])
            EGLS = sb("dnEGLS", [128, NT, 2])
            self.act(EGC[:], GC[:], AF.Exp)
            self.tt("dve", EKD[:], GL[:], GC[:], ALU.subtract)
            self.act(EKD[:], EKD[:], AF.Exp)
            self.act(EGL[:], GL[:], AF.Exp)
            self.tt("dve", BGE[:], self.BETA[:], EGC[:], ALU.mult)
            self.ts("dve", NEGB[:], self.BETA[:], -1.0, ALU.mult)
            self.cp("dve", EGLS[0:64, :, :], EGL[0:64, :, 0:4:2])
            self.cp("dve", EGLS[64:128, :, :], EGL[64:128, :, 1:4:2])
            Sst = sb("dnS", [128, 2, 64])
            self.memset("pool", Sst[:], 0.0)
            tok = sb("dntok", [128, 1024])
            qkT = sb("dnqkT", [128, 4, 128])
            qpad = sb("dnqpad", [128, 2, 2, 128])
            kpad = sb("dnkpad", [128, 2, 2, 128])
            self.memset("pool", qpad[:], 0.0)
            self.memset("pool", kpad[:], 0.0)
            diag = sb("dndiag", [128, 4, 128])
            X = sb("dnX", [128, 4, 128])
            dS = sb("dndS", [128, 4, 128])
            dT = sb("dndT", [128, 4, 128])
            attnT = sb("dnattnT", [128, 4, 128])
            Pb = [sb("dnP0", [128, 4, 128]), sb("dnP1", [128, 4, 128])]
            Qb = [sb("dnQ0", [128, 4, 128]), sb("dnQ1", [128, 4, 128])]
            W = sb("dnW", [128, 4, 128])
            vb = sb("dnvb", [128, 4, 64])
            kdec = sb("dnkdec", [128, 4, 64])
            Rt = sb("dnR", [128, 4, 64])
            vnew = sb("dnvnew", [128, 4, 64])
            o = sb("dno", [128, 4, 64])
            sq = sb("dnsq", [128, 4, 64])
            ss = sb("dnss", [128, 4])
            sz = sb("dnsz", [128, 256])
            mixt = self.MIX.rearrange("(n p) c -> p n c", p=128)
            dtok = self.DN_TOK.rearrange("(n p) c -> p n c", p=128)
            bc64 = lambda ap: ap.unsqueeze(2).to_broadcast([128, 4, 64])
            bc128 = lambda ap: ap.unsqueeze(2).to_broadcast([128, 4, 128])
            mb = lambda m_: m_[:].unsqueeze(1).to_broadcast([128, 4, 128])
            hsl = lambda h: slice(64 * (h % 2), 64 * (h % 2) + 64)
            for n in range(NT):
                self.dma("sp", tok[:], dtok[:, n, :])
                q3 = tok[:, 0:256].rearrange("p (h d) -> p h d", d=64)
                k3 = tok[:, 256:512].rearrange("p (h d) -> p h d", d=64)
                v3 = tok[:, 512:768].rearrange("p (h d) -> p h d", d=64)
                pT = B[0]
                for i in range(4):
                    self.tr(pT[:, i, :], tok[:, i * 128:(i + 1) * 128], self.identf[:])
                self.cp("act", qkT[:], pT[:])
                self.cp("act", qpad[0:64, 0, :, :], pT[0:64, 0:2, :])
                self.cp("dve", qpad[64:128, 1, :, :], pT[64:128, 0:2, :])
                self.cp("act", kpad[0:64, 0, :, :], pT[0:64, 2:4, :])
                self.cp("dve", kpad[64:128, 1, :, :], pT[64:128, 2:4, :])
                qT = lambda h: qpad[:, h % 2, h // 2, :]
                kT = lambda h: kpad[:, h % 2, h // 2, :]
                pKK, pKQ, pBr = B[1], B[2], B[3]
                for h in range(4):
                    self.mm(pKK[:, h, :], kT(h), qkT[:, 2 + h // 2, :])
                for h in range(4):
                    self.mm(pKQ[:, h, :], kT(h), qkT[:, h // 2, :])
                self.tt("pool", diag[:], self.identf[:].unsqueeze(1).to_broadcast([128, 4, 128]), bc128(GC[:, n, :]), ALU.mult)
                for h in range(4):
                    self.mm(pBr[:, h, :], self.onesf[:], diag[:, h, :])
                self.tt("dve", X[:], pBr[:], bc128(GC[:, n, :]), ALU.subtract)
                self.tt("dve", dS[:], X[:], mb(self.mstrict), ALU.max)
                self.act(dS[:], dS[:], AF.Exp, scale=-1.0)
                self.tt("dve", dT[:], X[:], mb(self.mTincl), ALU.min)
                self.act(dT[:], dT[:], AF.Exp)
                P, Q = Pb[0], Qb[0]
                self.tt("dve", P[:], pKK[:], dS[:], ALU.mult)
                self.tt("pool", P[:], P[:], bc128(NEGB[:, n, :]), ALU.mult)
                self.tt("dve", attnT[:], pKQ[:], dT[:], ALU.mult)
                yield
                pQ = B[0]
                for h in range(4):
                    self.tr(pQ[:, h, :], P[:, h, :], self.identf[:])
                self.cp("act", Q[:], pQ[:])
                self.tt("pool", W[:], Q[:], self.identf[:].unsqueeze(1).to_broadcast([128, 4, 128]), ALU.add)
                pP, pW = B[1], B[2]
                for k in range(6):
                    P, Q = Pb[k % 2], Qb[k % 2]
                    Pn, Qn = Pb[(k + 1) % 2], Qb[(k + 1) % 2]
                    for h in range(4):
                        self.mm(pP[:, h, :], Q[:, h, :], P[:, h, :])
                    if k < 5:
                        for h in range(4):
                            self.mm(pQ[:, h, :], P[:, h, :], Q[:, h, :])
                    self.cp("act", Pn[:], pP[:])
                    if k < 5:
                        self.cp("dve", Qn[:], pQ[:])
                    for h in range(4):
                        self.mm(pW[:, h, :], Pn[:, h, :], W[:, h, :])
                    self.tt("dve", W[:], W[:], pW[:], ALU.add)
                    yield
                self.tt("pool", vb[:], v3, bc64(self.BETA[:, n, :]), ALU.mult)
                self.tt("pool", kdec[:], k3, bc64(EKD[:, n, :]), ALU.mult)
                pA, pV, pSU = B[3], B[4], B[5]
                for h in range(4):
                    self.mm(pA[:, h, 0:64], kT(h), Sst[:, h // 2, :])
                for h in range(4):
                    self.mm(pA[:, h, 64:128], qT(h), Sst[:, h // 2, :])
                self.tt("dve", Rt[:], pA[:, :, 0:64], bc64(BGE[:, n, :]), ALU.mult)
                self.tt("dve", Rt[:], vb[:], Rt[:], ALU.subtract)
                for h in range(4):
                    self.mm(pV[:, h, 0:64], W[:, h, :], Rt[:, h, :])
                self.cp("act", vnew[:], pV[:, :, 0:64])
                for h in range(4):
                    self.mm(pV[:, h, 64:128], attnT[:, h, :], vnew[:, h, :])
                for pr_ in range(2):
                    self.mm(pSU[:, pr_, :], kdec[:, 2 * pr_:2 * pr_ + 2, :].rearrange("p h d -> p (h d)"),
                            vnew[:, 2 * pr_:2 * pr_ + 2, :].rearrange("p h d -> p (h d)"))
                yield
                self.tt("dve", o[:], pA[:, :, 64:128], bc64(EGC[:, n, :]), ALU.mult)
                self.tt("dve", o[:], o[:], pV[:, :, 64:128], ALU.add)
                self.tt("pool", Sst[:], Sst[:], EGLS[:, n, :].unsqueeze(2).to_broadcast([128, 2, 64]), ALU.mult)
                self.tt("dve", Sst[0:64, :, :], Sst[0:64, :, :], pSU[0:64, 0:2, 0:64], ALU.add)
                self.tt("dve", Sst[64:128, :, :], Sst[64:128, :, :], pSU[64:128, 0:2, 64:128], ALU.add)
                self.tt("pool", sq[:], o[:], o[:], ALU.mult)
                self.red("dve", ss[:], sq[:], ALU.add)
                self.act(ss[:], ss[:], AF.Sqrt, bias=self.epsb[:, 0:1], scale=1.0 / 64)
                self.recip(ss[:], ss[:])
                self.tt("dve", o[:], o[:], bc64(ss[:]), ALU.mult)
                self.tt("pool", o[:], o[:], nw[:].unsqueeze(1).to_broadcast([128, 4, 64]), ALU.mult)
                self.act(sz[:], tok[:, 768:1024], AF.Silu)
                self.tt("dve", o[:], o[:], sz[:].rearrange("p (h d) -> p h d", d=64), ALU.mult)
                self.dma("sp", mixt[:, n, 0:256], o[:].rearrange("p h d -> p (h d)"))
                yield

    def phase3(self, l):
        self.phase3a(l)
        if l % 2 == 0:
            i = l // 2
            self.ffn(1, DFF, lambda e: self.ffn_w_gate[i], lambda e: self.ffn_w_up[i], lambda e: self.ffn_w_down[i], gated=False)
        else:
            i = l // 2
            self.ffn(NEXP, DFFE, lambda e: self.moe_w_gate[i, e], lambda e: self.moe_w_up[i, e], lambda e: self.moe_w_down[i, e], gated=True)

    def phase3a(self, l):
        NT, T = self.NT, self.T
        moe = (l % 2 == 1)
        with ExitStack() as st:
            sb = lambda n, s, dt=F32: self.sb(st, n, s, dt)
            ps = lambda n, s, dt=F32: self.ps(st, n, s, dt)
            wout = sb("wout", [128, 8, D], BF16)
            self.dma("pool", wout[:], self.w_out[l].rearrange("(c p) d -> p c d", p=128))
            normw = sb("normw2", [128, D])
            self.dma("sp", normw[:], self.norm_ffn[l].partition_broadcast(128))
            hs = sb("hs3", [128, 4, D])
            mx = sb("mx3", [128, 4, D])
            mxb = sb("mxb3", [128, 4, D], BF16)
            mixT = sb("mixT", [128, 8, 512], BF16)
            junk = sb("junk3", [128, D])
            ssq = sb("ssq3", [128, 4])
            rstd = sb("rstd3", [128, 4])
            ub = sb("ub3", [128, 4, D], BF16)
            uT = sb("uT3", [128, 8, 512], BF16)
            tp = ps("tp3", [128, 8, 128], BF16)
            pH = [ps("pH0", [128, 512]), ps("pH1", [128, 512])]
            if moe:
                rt = sb("router", [128, 8, NEXP])
                self.dma("sp", rt[:], self.moe_router[l // 2].rearrange("(c p) e -> p c e", p=128))
                uf = sb("uf3", [128, D])
                ufT = sb("ufT3", [128, 8, 128])
                ptf = ps("ptf3", [128, 4, 128])
                pL = ps("pL3", [128, 512])
                lg = sb("lg3", [128, NEXP])
                m1 = sb("m13", [128, 1])
                msk = sb("msk3", [128, NEXP])
                l2 = sb("l23", [128, NEXP])
                ex = sb("ex3", [128, NEXP])
                den = sb("den3", [128, 1])
                gT = sb("gT3", [NEXP, 512])
            mixt = self.MIX.rearrange("(n p) c -> p n c", p=128)
            ht = self.H.rearrange("(n p) d -> p n d", p=128)
            for (n0, TG) in self.groups():
                TW = TG * 128
                self.load_h_group(l, hs, n0, TG)
                self.dma("sp", mx[:, 0:TG, :], mixt[:, n0:n0 + TG, :])
                for j in range(TG):
                    self.cp("pool", mxb[:, j, :], mx[:, j, :])
                if n0 == 0:
                    self.memset("pool", mxb[0:112, 0, :], 0.0)
                for j in range(TG):
                    for c in range(8):
                        self.tr(tp[:, c, :], mxb[:, j, c * 128:(c + 1) * 128], self.identb[:])
                    self.cp("act", mixT[:, :, j * 128:(j + 1) * 128], tp[:, :, :])
                for j in range(TG):
                    for hf in range(2):
                        for c in range(8):
                            self.mm(pH[hf][:], mixT[:, c, j * 128:(j + 1) * 128], wout[:, c, hf * 512:(hf + 1) * 512], start=(c == 0), stop=(c == 7))
                        self.tt("dve", hs[:, j, hf * 512:(hf + 1) * 512], hs[:, j, hf * 512:(hf + 1) * 512], pH[hf][:], ALU.add)
                self.dma("sp", ht[:, n0:n0 + TG, :], hs[:, 0:TG, :])
                for j in range(TG):
                    self.tt("pool", junk[:], hs[:, j, :], hs[:, j, :], ALU.mult)
                    self.red("dve", ssq[:, j:j + 1], junk[:], ALU.add)
                self.act(rstd[:, 0:TG], ssq[:, 0:TG], AF.Sqrt, bias=self.epsb[:, 0:1], scale=1.0 / D)
                self.recip(rstd[:, 0:TG], rstd[:, 0:TG])
                for j in range(TG):
                    if moe:
                        self.stt("dve", uf[:], hs[:, j, :], rstd[:, j:j + 1], normw[:], ALU.mult, ALU.mult)
                        self.cp("pool", ub[:, j, :], uf[:])
                        for c4 in range(2):
                            for c in range(4):
                                self.tr(ptf[:, c, :], uf[:, (c4 * 4 + c) * 128:(c4 * 4 + c + 1) * 128], self.identf[:])
                            self.cp("act", ufT[:, c4 * 4:c4 * 4 + 4, :], ptf[:])
                        for c in range(8):
                            self.mm(pL[:, 0:NEXP], ufT[:, c, :], rt[:, c, :], start=(c == 0), stop=(c == 7))
                        self.cp("dve", lg[:], pL[:, 0:NEXP])
                        self.red("dve", m1[:], lg[:], ALU.max)
                        self.ts("dve", msk[:], lg[:], m1[:, 0:1], ALU.is_equal)
                        self.stt("dve", l2[:], msk[:], -BIG, lg[:], ALU.mult, ALU.add)
                        self.red("dve", den[:], l2[:], ALU.max)
                        self.ts("dve", msk[:], lg[:], den[:, 0:1], ALU.is_ge)
                        self.ts("dve", m1[:], m1[:], -1.0, ALU.mult)
                        self.act(ex[:], lg[:], AF.Exp, bias=m1[:, 0:1])
                        self.tt("dve", ex[:], ex[:], msk[:], ALU.mult)
                        self.red("dve", den[:], ex[:], ALU.add)
                        self.recip(den[:], den[:])
                        self.ts("dve", ex[:], ex[:], den[:, 0:1], ALU.mult)
                        self.tr(pL[0:NEXP, 128:256], ex[:], self.identf[:])
                        self.cp("dve", gT[:, j * 128:(j + 1) * 128], pL[0:NEXP, 128:256])
                    else:
                        self.stt("dve", ub[:, j, :], hs[:, j, :], rstd[:, j:j + 1], normw[:], ALU.mult, ALU.mult)
                for j in range(TG):
                    for c in range(8):
                        self.tr(tp[:, c, :], ub[:, j, c * 128:(c + 1) * 128], self.identb[:])
                    self.cp("act", uT[:, :, j * 128:(j + 1) * 128], tp[:, :, :])
                self.dma("sp", self.U2T[:, :, n0 * 128:n0 * 128 + TW].rearrange("c p t -> p c t"), uT[:, :, 0:TW])
                if moe:
                    self.dma("sp", self.GATES[:, n0 * 128:n0 * 128 + TW], gT[:, 0:TW])
            self.flush()

    def ffn(self, NE, FF, wg_of, wu_of, wd_of, gated):
        NT, T = self.NT, self.T
        nfc = FF // 128
        blocks = []
        f = 0
        while f < nfc:
            b = min(4, nfc - f)
            blocks.append((f, b))
            f += b
        halves = []
        n = 0
        HMAX = getattr(self, "HMAX", 17)
        while n < NT:
            k = min(HMAX, NT - n)
            halves.append((n, k))
            n += k
        with ExitStack() as st:
            sb = lambda n, s, dt=F32: self.sb(st, n, s, dt)
            ps = lambda n, s, dt=F32: self.ps(st, n, s, dt)
            hmax = max(k for _, k in halves)
            u2T = sb("fu2T", [128, 8, hmax * 128], BF16)
            yacc = sb("fyacc", [128, hmax, D])
            wg = [sb(f"fwg{i}", [128, 8, 512], BF16) for i in range(2)]
            wu = [sb(f"fwu{i}", [128, 8, 512], BF16) for i in range(2)]
            wd = [sb(f"fwd{i}", [128, 4, D], BF16) for i in range(2)]
            sg = [sb(f"fsg{i}", [128, 512]) for i in range(2)]
            actT = [sb(f"fact{i}", [128, 4, 512], BF16) for i in range(2)]
            hb = [sb(f"fhb{i}", [128, D]) for i in range(2)]
            if gated:
                gbc = sb("fgbc", [128, hmax * 128])
            pG = [ps(f"fpG{i}", [128, 512]) for i in range(2)]
            pU = [ps(f"fpU{i}", [128, 512]) for i in range(2)]
            pD = [[ps(f"fpD{i}{hf}", [128, 512]) for hf in range(2)] for i in range(2)]
            ht = self.H.rearrange("(n p) d -> p n d", p=128)
            wi = 0
            gi = 0
            ai = 0
            di = 0
            pending = [None]
            for (h0, hk) in halves:
                TH = hk * 128
                self.dma("sp", u2T[:, :, 0:TH], self.U2T[:, :, h0 * 128:h0 * 128 + TH].rearrange("c p t -> p c t"))
                self.memset("pool", yacc[:, 0:hk, :], 0.0)
                groups = []
                j = 0
                while j < hk:
                    tg = min(4, hk - j)
                    groups.append((j, tg))
                    j += tg
                for e in range(NE):
                    if gated:
                        self.dma("sp", gbc[:, 0:TH], self.GATES[e, h0 * 128:h0 * 128 + TH].partition_broadcast(128))
                    for (f0, fb) in blocks:
                        FB = fb * 128
                        g_, u_, d_ = wg[wi % 2], wu[wi % 2], wd[wi % 2]
                        wi += 1
                        self.dma("pool", g_[:, :, 0:FB], wg_of(e)[:, f0 * 128:f0 * 128 + FB].rearrange("(c p) f -> p c f", p=128))
                        self.dma("pool", u_[:, :, 0:FB], wu_of(e)[:, f0 * 128:f0 * 128 + FB].rearrange("(c p) f -> p c f", p=128))
                        self.dma("pool", d_[:, 0:fb, :], wd_of(e)[f0 * 128:f0 * 128 + FB, :].rearrange("(c p) d -> p c d", p=128))
                        for (j0, tg) in groups:
                            TW = tg * 128
                            cs_ = slice(j0 * 128, j0 * 128 + TW)
                            a_ = actT[ai % 2]
                            ai += 1
                            for fc in range(fb):
                                pg, pu, s_ = pG[gi % 2], pU[gi % 2], sg[gi % 2]
                                gi += 1
                                for c in range(8):
                                    self.mm(pg[:, 0:TW], g_[:, c, fc * 128:(fc + 1) * 128], u2T[:, c, cs_], start=(c == 0), stop=(c == 7))
                                for c in range(8):
                                    self.mm(pu[:, 0:TW], u_[:, c, fc * 128:(fc + 1) * 128], u2T[:, c, cs_], start=(c == 0), stop=(c == 7))
                                self.act(s_[:, 0:TW], pg[:, 0:TW], AF.Silu)
                                if gated:
                                    self.tt("dve", s_[:, 0:TW], s_[:, 0:TW], pu[:, 0:TW], ALU.mult)
                                    self.tt("pool", a_[:, fc, 0:TW], s_[:, 0:TW], gbc[:, cs_], ALU.mult)
                                else:
                                    self.tt("dve", a_[:, fc, 0:TW], s_[:, 0:TW], pu[:, 0:TW], ALU.mult)
                            if pending[0] is not None:
                                pending[0]()

                            def down(a_=a_, d_=d_, fb=fb, j0=j0, tg=tg):
                                nonlocal di
                                for j in range(tg):
                                    pd = pD[di % 2]
                                    di += 1
                                    for hf in range(2):
                                        for fc in range(fb):
                                            self.mm(pd[hf][:], a_[:, fc, j * 128:(j + 1) * 128], d_[:, fc, hf * 512:(hf + 1) * 512], start=(fc == 0), stop=(fc == fb - 1))
                                        ya = yacc[:, j0 + j, hf * 512:(hf + 1) * 512]
                                        self.tt("dve", ya, ya, pd[hf][:], ALU.add)
                            pending[0] = down
                if pending[0] is not None:
                    pending[0]()
                    pending[0] = None
                for j in range(hk):
                    b_ = hb[j % 2]
                    self.dma("sp", b_[:], ht[:, h0 + j, :])
                    self.tt("pool", b_[:], b_[:], yacc[:, j, :], ALU.add)
                    self.dma("sp", ht[:, h0 + j, :], b_[:])
            self.flush()

    def final(self):
        NT = self.NT
        with ExitStack() as st:
            sb = lambda n, s, dt=F32: self.sb(st, n, s, dt)
            normw = sb("normwf", [128, D])
            self.dma("sp", normw[:], self.norm_final.partition_broadcast(128))
            hs = [sb("hsf0", [128, D]), sb("hsf1", [128, D])]
            junk = sb("junkf", [128, D])
            ssq = sb("ssqf", [128, 2])
            ht = self.H.rearrange("(n p) d -> p n d", p=128)
            ot = self.out.rearrange("(n p) d -> p n d", p=128)
            for n in range(1, NT):
                h = hs[n % 2]
                s_ = ssq[:, (n % 2):(n % 2) + 1]
                self.dma("sp", h[:], ht[:, n, :])
                self.tt("pool", junk[:], h[:], h[:], ALU.mult)
                self.red("dve", s_, junk[:], ALU.add)
                self.act(s_, s_, AF.Sqrt, bias=self.epsb[:, 0:1], scale=1.0 / D)
                self.recip(s_, s_)
                self.stt("dve", h[:], h[:], s_, normw[:], ALU.mult, ALU.mult)
                self.dma("sp", ot[:, n - 1, :], h[:])
            self.flush()


_PARAMS = ["meta_tokens", "norm_mix", "w_in", "dn_conv_w", "dn_a_log", "dn_dt_bias", "dn_norm_w", "df_lambda",
           "df_norm_w", "sw_sinks", "ssd_conv_w", "ssd_conv_b", "ssd_a_log", "ssd_dt_bias", "ssd_d", "ssd_norm_w",
           "w_out", "norm_ffn", "ffn_w_gate", "ffn_w_up", "ffn_w_down", "moe_router", "moe_w_gate", "moe_w_up",
           "moe_w_down", "norm_final"]


def kernel(**inputs):
    from concourse.bass_utils import run_bass_kernel_spmd
    x = np.asarray(inputs["x"], dtype=np.float32)
    B, SEQ, _ = x.shape
    NT = SEQ // 128 + 1
    m = Model(NT, depth=2)
    nc = m.build()
    params = {k: np.ascontiguousarray(np.asarray(inputs[k], dtype=np.float32)) for k in _PARAMS}
    in_maps = []
    for b in range(B):
        d = dict(params)
        d["x"] = np.ascontiguousarray(x[b])
        in_maps.append(d)
    res = run_bass_kernel_spmd(nc, in_maps, core_ids=list(range(B)))
    return np.stack([np.asarray(r["out"], dtype=np.float32) for r in res.results], axis=0)
```
